# Optimizing a Trainium2 kernel written in Bass

```python
import jax, jax.numpy as jnp
from jax import lax
import numpy as np

D_MODEL = 1024
BATCH = 4
SEQ = 8192
DEPTH = 2

GRID_W = 64
CTX_LEN = 256
N_Q_HEADS = 8
N_KV_HEADS = 2
GQA_GROUP = N_Q_HEADS // N_KV_HEADS
HEAD_DIM = 64
WINDOW = 128
ATT_BLOCK = 128
ROPE_BASE = 10000.0
Q_DIM = N_Q_HEADS * HEAD_DIM
KV_DIM = N_KV_HEADS * HEAD_DIM
SG_GROUPS = 8
SG_HEAD = 64
SG_WIDTH = SG_GROUPS * SG_HEAD
SG_CHUNK = 128
EVEN_IN = Q_DIM + 2 * KV_DIM + 2 * SG_WIDTH
EVEN_MIX = Q_DIM + SG_WIDTH
CONV_WIDTH = D_MODEL
CONV_K = 3
N_GROUPS = 4
EXPERTS_PER_GROUP = 8
N_EXPERTS = N_GROUPS * EXPERTS_PER_GROUP
TOP_K_IN_GROUP = 2
EXPERT_HIDDEN = D_MODEL // 2
MOE_BLOCK = 128
N_EVEN = (DEPTH + 1) // 2
N_ODD = DEPTH // 2
EPS = 1e-6
NEG_INF = -1e30

kernel_name = 'hybrid_diffusion_swa_sgmlp_shortconv_hmoe'


def rms_norm(x, g):
    xf = x.astype(jnp.float32)
    y = xf * lax.rsqrt(jnp.mean(xf * xf, axis=-1, keepdims=True) + EPS)
    return (y * g.astype(jnp.float32)).astype(x.dtype)


def layer_norm(x, g):
    xf = x.astype(jnp.float32)
    mu = jnp.mean(xf, axis=-1, keepdims=True)
    xc = xf - mu
    y = xc * lax.rsqrt(jnp.mean(xc * xc, axis=-1, keepdims=True) + EPS)
    return (y * g.astype(jnp.float32)).astype(x.dtype)


def modulate(h, shift, scale):
    return h * (1 + scale) + shift


def axial_rope_tables(rows, dtype):
    quarter = HEAD_DIM // 4
    row_ids = jnp.repeat(jnp.arange(rows, dtype=jnp.float32), GRID_W)
    col_ids = jnp.tile(jnp.arange(GRID_W, dtype=jnp.float32), rows)
    inv = ROPE_BASE ** (-jnp.arange(quarter, dtype=jnp.float32) / quarter)
    ang_r = row_ids[:, None] * inv
    ang_c = col_ids[:, None] * inv
    return (jnp.cos(ang_r)[:, None, :].astype(dtype), jnp.sin(ang_r)[:, None, :].astype(dtype),
            jnp.cos(ang_c)[:, None, :].astype(dtype), jnp.sin(ang_c)[:, None, :].astype(dtype))


def apply_axial_rope(t, rope):
    cos_r, sin_r, cos_c, sin_c = rope
    half = HEAD_DIM // 2
    quarter = HEAD_DIM // 4

    def rot(u, cos, sin):
        u1, u2 = u[..., :quarter], u[..., quarter:]
        return jnp.concatenate([u1 * cos - u2 * sin, u2 * cos + u1 * sin], axis=-1)

    return jnp.concatenate([rot(t[..., :half], cos_r, sin_r), rot(t[..., half:], cos_c, sin_c)], axis=-1)


def windowed_attention(q, k, v, kc, vc, sink):
    b, s = q.shape[:2]
    nb = s // ATT_BLOCK
    scale = HEAD_DIM ** -0.5
    f32 = jnp.float32
    qb = q.reshape(b, nb, ATT_BLOCK, N_KV_HEADS, GQA_GROUP, HEAD_DIM)

    def band(t):
        tp = jnp.pad(t, ((0, 0), (ATT_BLOCK, ATT_BLOCK), (0, 0), (0, 0)))
        tp = tp.reshape(b, nb + 2, ATT_BLOCK, N_KV_HEADS, HEAD_DIM)
        return jnp.concatenate([tp[:, :-2], tp[:, 1:-1], tp[:, 2:]], axis=2)

    kb, vb = band(k), band(v)
    s_loc = jnp.einsum('bnqhgd,bnkhd->bnhgqk', qb, kb, preferred_element_type=f32) * scale
    s_ctx = jnp.einsum('bnqhgd,bchd->bnhgqc', qb, kc, preferred_element_type=f32) * scale
    qi = jnp.arange(ATT_BLOCK)[:, None]
    kj = jnp.arange(3 * ATT_BLOCK)[None, :]
    in_win = jnp.abs(kj - qi - ATT_BLOCK) <= WINDOW
    kpos = (jnp.arange(nb)[:, None] - 1) * ATT_BLOCK + jnp.arange(3 * ATT_BLOCK)[None, :]
    mask = in_win[None] & ((kpos >= 0) & (kpos < s))[:, None, :]
    s_loc = jnp.where(mask[None, :, None, None], s_loc, NEG_INF)
    sink_l = jnp.broadcast_to(sink.astype(f32).reshape(1, 1, N_KV_HEADS, GQA_GROUP, 1, 1), s_ctx.shape[:-1] + (1,))
    p = jax.nn.softmax(jnp.concatenate([sink_l, s_ctx, s_loc], axis=-1), axis=-1)
    n_ctx = kc.shape[1]
    p_ctx = p[..., 1:1 + n_ctx].astype(vc.dtype)
    p_loc = p[..., 1 + n_ctx:].astype(vb.dtype)
    o = (jnp.einsum('bnhgqc,bchd->bnqhgd', p_ctx, vc)
         + jnp.einsum('bnhgqk,bnkhd->bnqhgd', p_loc, vb))
    return o.reshape(b, s, Q_DIM)


def context_attention(qc, kc, vc, sink):
    b, n = qc.shape[:2]
    f32 = jnp.float32
    qh = qc.reshape(b, n, N_KV_HEADS, GQA_GROUP, HEAD_DIM)
    sc = jnp.einsum('bqhgd,bkhd->bhgqk', qh, kc, preferred_element_type=f32) * (HEAD_DIM ** -0.5)
    sink_c = jnp.broadcast_to(sink.astype(f32).reshape(1, N_KV_HEADS, GQA_GROUP, 1, 1), sc.shape[:-1] + (1,))
    p = jax.nn.softmax(jnp.concatenate([sink_c, sc], axis=-1), axis=-1)
    o = jnp.einsum('bhgqk,bkhd->bqhgd', p[..., 1:].astype(vc.dtype), vc)
    return o.reshape(b, n, Q_DIM)


def spatial_gating(u, z, g_sgu, w_sp, b_sp):
    b, n = u.shape[:2]
    u = jax.nn.gelu(u, approximate=False)
    z = layer_norm(jax.nn.gelu(z, approximate=False), g_sgu)
    zc = z.reshape(b, n // SG_CHUNK, SG_CHUNK, SG_GROUPS, SG_HEAD)
    sgate = jnp.einsum('hij,bnjhc->bnihc', w_sp, zc) + b_sp.T[:, :, None]
    return (u.reshape(zc.shape) * sgate).reshape(b, n, SG_WIDTH)


def even_mixer(h, hc, w_in, sink, g_sgu, w_sp, b_sp, w_out, rope, need_ctx):
    b, s, _ = h.shape
    n_ctx = hc.shape[1]
    q, k, v, u, z = jnp.split(h @ w_in, [Q_DIM, Q_DIM + KV_DIM, Q_DIM + 2 * KV_DIM, Q_DIM + 2 * KV_DIM + SG_WIDTH], axis=-1)
    q = apply_axial_rope(q.reshape(b, s, N_Q_HEADS, HEAD_DIM), rope)
    k = apply_axial_rope(k.reshape(b, s, N_KV_HEADS, HEAD_DIM), rope)
    v = v.reshape(b, s, N_KV_HEADS, HEAD_DIM)
    kc, vc = jnp.split(hc @ w_in[:, Q_DIM:Q_DIM + 2 * KV_DIM], 2, axis=-1)
    kc = kc.reshape(b, n_ctx, N_KV_HEADS, HEAD_DIM)
    vc = vc.reshape(b, n_ctx, N_KV_HEADS, HEAD_DIM)
    att = windowed_attention(q, k, v, kc, vc, sink)
    sg = spatial_gating(u, z, g_sgu, w_sp, b_sp)
    out_lat = jnp.concatenate([att, sg], axis=-1) @ w_out
    if not need_ctx:
        return out_lat, None
    qc = hc @ w_in[:, :Q_DIM]
    uc, zc = jnp.split(hc @ w_in[:, Q_DIM + 2 * KV_DIM:], 2, axis=-1)
    att_c = context_attention(qc, kc, vc, sink)
    sg_c = spatial_gating(uc, zc, g_sgu, w_sp, b_sp)
    return out_lat, jnp.concatenate([att_c, sg_c], axis=-1) @ w_out


def short_conv_mixer(h, w_in, conv_w, w_out):
    bg, cg, xt = jnp.split(h @ w_in, 3, axis=-1)
    y = cg * xt
    yp = jnp.pad(y, ((0, 0), (1, 1), (0, 0)))
    conv = yp[:, :-2] * conv_w[0] + yp[:, 1:-1] * conv_w[1] + yp[:, 2:] * conv_w[2]
    return (bg * conv) @ w_out


def moe_ffn(h, w_rg, b_rg, w_re, b_re, w_gate, w_up, w_down):
    n, d = h.shape
    f32 = jnp.float32
    g_logits = jnp.einsum('nd,dg->ng', h, w_rg, preferred_element_type=f32) + b_rg.astype(f32)
    g_idx = jnp.argmax(g_logits, axis=-1).astype(jnp.int32)
    g_w = jnp.take_along_axis(jax.nn.softmax(g_logits, axis=-1), g_idx[:, None], axis=-1)
    e_logits = (jnp.einsum('nd,de->ne', h, w_re, preferred_element_type=f32) + b_re.astype(f32))
    e_logits = e_logits.reshape(n, N_GROUPS, EXPERTS_PER_GROUP)
    e_sel = jnp.take_along_axis(e_logits, g_idx[:, None, None], axis=1)[:, 0]
    top_v, top_i = lax.top_k(e_sel, TOP_K_IN_GROUP)
    gate = g_w * jax.nn.softmax(top_v, axis=-1)
    expert = g_idx[:, None] * EXPERTS_PER_GROUP + top_i.astype(jnp.int32)
    n_assign = n * TOP_K_IN_GROUP
    flat_e = expert.reshape(-1)
    flat_tok = jnp.repeat(jnp.arange(n, dtype=jnp.int32), TOP_K_IN_GROUP)
    flat_w = gate.reshape(-1).astype(h.dtype)
    order = jnp.argsort(flat_e)
    sorted_e = flat_e[order]
    counts = jnp.bincount(flat_e, length=N_EXPERTS)
    padded = (counts + MOE_BLOCK - 1) // MOE_BLOCK * MOE_BLOCK
    pad_end = jnp.cumsum(padded)
    pad_start = pad_end - padded
    raw_start = jnp.cumsum(counts) - counts
    dest = pad_start[sorted_e] + jnp.arange(n_assign, dtype=pad_start.dtype) - raw_start[sorted_e]
    n_blocks = -(-n_assign // MOE_BLOCK) + N_EXPERTS
    n_rows = n_blocks * MOE_BLOCK
    row_tok = jnp.zeros((n_rows,), jnp.int32).at[dest].set(flat_tok[order])
    row_w = jnp.zeros((n_rows,), h.dtype).at[dest].set(flat_w[order])
    block_e = jnp.clip(jnp.searchsorted(pad_end, jnp.arange(n_blocks, dtype=pad_end.dtype) * MOE_BLOCK, side='right'), 0, N_EXPERTS - 1)
    xb = h[row_tok].reshape(n_blocks, MOE_BLOCK, d)

    def expert_block(args):
        xblk, e = args
        hid = jax.nn.silu(xblk @ w_gate[e]) * (xblk @ w_up[e])
        return hid @ w_down[e]

    yb = lax.map(expert_block, (xb, block_e)).reshape(n_rows, d)
    return jnp.zeros_like(h).at[row_tok].add(yb * row_w[:, None])


def setup_inputs(seed: int = 0) -> dict:
    key = jax.random.key(seed)
    ks = jax.random.split(key, 25)
    D = D_MODEL

    def nrm(k, shape, s):
        return jax.random.normal(k, shape, jnp.float32) * s

    return {
        'x': nrm(ks[0], (BATCH, SEQ, D), 1.0),
        'c': nrm(ks[1], (BATCH, D), 1.0),
        'ctx': nrm(ks[2], (BATCH, CTX_LEN, D), 1.0),
        'c_ctx': nrm(ks[3], (D,), 1.0),
        'w_ada': nrm(ks[4], (DEPTH, D, 6 * D), 0.5 * D ** -0.5),
        'b_ada': nrm(ks[5], (DEPTH, 6 * D), 0.02),
        'g_norm1': 1.0 + nrm(ks[6], (DEPTH, D), 0.1),
        'g_norm2': 1.0 + nrm(ks[7], (DEPTH, D), 0.1),
        'g_final': 1.0 + nrm(ks[8], (D,), 0.1),
        'w_in_even': nrm(ks[9], (N_EVEN, D, EVEN_IN), D ** -0.5),
        'attn_sink': nrm(ks[10], (N_EVEN, N_Q_HEADS), 1.0),
        'g_sgu': 1.0 + nrm(ks[11], (N_EVEN, SG_WIDTH), 0.1),
        'w_spatial': nrm(ks[12], (N_EVEN, SG_GROUPS, SG_CHUNK, SG_CHUNK), SG_CHUNK ** -0.5),
        'b_spatial': 1.0 + nrm(ks[13], (N_EVEN, SG_GROUPS, SG_CHUNK), 0.1),
        'w_out_even': nrm(ks[14], (N_EVEN, EVEN_MIX, D), EVEN_MIX ** -0.5),
        'w_in_odd': nrm(ks[15], (N_ODD, D, 3 * CONV_WIDTH), D ** -0.5),
        'conv_w': nrm(ks[16], (N_ODD, CONV_K, CONV_WIDTH), CONV_K ** -0.5),
        'w_out_odd': nrm(ks[17], (N_ODD, CONV_WIDTH, D), CONV_WIDTH ** -0.5),
        'w_router_group': nrm(ks[18], (DEPTH, D, N_GROUPS), D ** -0.5),
        'b_router_group': nrm(ks[19], (DEPTH, N_GROUPS), 0.01),
        'w_router_expert': nrm(ks[20], (DEPTH, D, N_EXPERTS), D ** -0.5),
        'b_router_expert': nrm(ks[21], (DEPTH, N_EXPERTS), 0.01),
        'w_gate': nrm(ks[22], (DEPTH, N_EXPERTS, D, EXPERT_HIDDEN), D ** -0.5),
        'w_up': nrm(ks[23], (DEPTH, N_EXPERTS, D, EXPERT_HIDDEN), D ** -0.5),
        'w_down': nrm(ks[24], (DEPTH, N_EXPERTS, EXPERT_HIDDEN, D), EXPERT_HIDDEN ** -0.5),
    }


def reference(x, c, ctx, c_ctx, w_ada, b_ada, g_norm1, g_norm2, g_final, w_in_even, attn_sink, g_sgu,
              w_spatial, b_spatial, w_out_even, w_in_odd, conv_w, w_out_odd, w_router_group,
              b_router_group, w_router_expert, b_router_expert, w_gate, w_up, w_down):
    b, s, d = x.shape
    rows = s // GRID_W
    rope = axial_rope_tables(rows, x.dtype)
    silu_c = jax.nn.silu(c)
    silu_cc = jax.nn.silu(c_ctx)
    lat, cx = x, ctx
    for l in range(DEPTH):
        need_ctx = any(j % 2 == 0 for j in range(l + 1, DEPTH))
        m = jnp.split(silu_c @ w_ada[l] + b_ada[l], 6, axis=-1)
        sh1, sc1, g1, sh2, sc2, g2 = [t[:, None, :] for t in m]
        h = modulate(rms_norm(lat, g_norm1[l]), sh1, sc1)
        mix_c = None
        if l % 2 == 0 or need_ctx:
            csh1, csc1, cg1, csh2, csc2, cg2 = jnp.split(silu_cc @ w_ada[l] + b_ada[l], 6, axis=-1)
            hc = modulate(rms_norm(cx, g_norm1[l]), csh1, csc1)
        i = l // 2
        if l % 2 == 0:
            mix, mix_c = even_mixer(h, hc, w_in_even[i], attn_sink[i], g_sgu[i], w_spatial[i],
                                    b_spatial[i], w_out_even[i], rope, need_ctx)
        else:
            mix = short_conv_mixer(h, w_in_odd[i], conv_w[i], w_out_odd[i])
            if need_ctx:
                mix_c = short_conv_mixer(hc, w_in_odd[i], conv_w[i], w_out_odd[i])
        lat = lat + g1 * mix
        h2 = modulate(rms_norm(lat, g_norm2[l]), sh2, sc2)
        lat = lat + g2 * moe_ffn(h2.reshape(-1, d), w_router_group[l], b_router_group[l], w_router_expert[l],
                                 b_router_expert[l], w_gate[l], w_up[l], w_down[l]).reshape(b, s, d)
        if need_ctx:
            cx = cx + cg1 * mix_c
            hc2 = modulate(rms_norm(cx, g_norm2[l]), csh2, csc2)
            cx = cx + cg2 * moe_ffn(hc2.reshape(-1, d), w_router_group[l], b_router_group[l], w_router_expert[l],
                                    b_router_expert[l], w_gate[l], w_up[l], w_down[l]).reshape(cx.shape)
    return rms_norm(lat, g_final)
```

```python
import numpy as np
from contextlib import ExitStack
import concourse.bass as bass
import concourse.mybir as mybir
from concourse.bass_utils import run_bass_kernel_spmd

F32 = mybir.dt.float32
F32R = mybir.dt.float32r
BF16 = mybir.dt.bfloat16
I32 = mybir.dt.int32
AF = mybir.ActivationFunctionType
ALU = mybir.AluOpType
AX = mybir.AxisListType

NB = 34
NF = 33
NO = 32
D = 1024
NE = 32
EPS = 1e-6
NSS0 = NF + NE
NSS1 = NO + NE
NS0 = 2 * NSS0
NS1 = 2 * NSS1
OOB = 1.0e6


_UC = [0]
_JR = {}
_SEMPOOL = {"free": [], "es": None}


def U(n):
    _UC[0] += 1
    return f"{n}_u{_UC[0]}"


class Sched:
    ENG = ("sync", "act", "pool", "pe", "dve")

    def __init__(self, nc, name):
        self.nc = nc
        self.name = name
        self.ops = []
        self.last_w = {}
        self.readers = {}

    def add(self, eng, fn, r=(), w=(), dma=None):
        idx = len(self.ops)
        deps = set()
        for x in r:
            j = self.last_w.get(x)
            if j is not None:
                deps.add(j)
        for x in w:
            j = self.last_w.get(x)
            if j is not None:
                deps.add(j)
            deps.update(self.readers.get(x, ()))
        deps.discard(idx)
        self.ops.append(dict(eng=eng, fn=fn, deps=deps, dma=dma))
        for x in r:
            self.readers.setdefault(x, []).append(idx)
        for x in w:
            self.last_w[x] = idx
            self.readers[x] = []
        return idx

    def emit(self):
        nc = self.nc
        ops = self.ops
        has_dep = [False] * len(ops)
        for o in ops:
            for j in o["deps"]:
                has_dep[j] = True
        semkeys = []
        seen = set()
        for o in ops:
            k = (("dmasw" if o["eng"] == "pool" else "dma"), o["dma"]) if o["dma"] is not None else ("eng", o["eng"])
            o["semkey"] = k
            if k not in seen:
                seen.add(k)
                semkeys.append(k)
        pool = _SEMPOOL
        semobj = {}
        counts = {}
        for k in semkeys:
            fl = pool.setdefault("free_" + k[0], [])
            if fl:
                so, c0 = fl.pop()
            else:
                so, c0 = pool["es"].enter_context(nc.semaphore(U("sem"))), 0
            semobj[k] = so
            counts[k] = c0
        start = dict(counts)
        for i, o in enumerate(ops):
            k = o["semkey"]
            if o["dma"] is not None:
                counts[k] += 16
                o["semval"] = counts[k]
                o["inc"] = 16
            elif has_dep[i]:
                counts[k] += 1
                o["semval"] = counts[k]
                o["inc"] = 1
            else:
                o["semval"] = None
                o["inc"] = 0
        waited = {e: dict(start) for e in self.ENG}
        for o in ops:
            need = {}
            for j in o["deps"]:
                d = ops[j]
                if d["eng"] == "pe" and o["eng"] == "pe" and d["dma"] is None:
                    continue
                k = d["semkey"]
                v = d["semval"]
                if v is None:
                    continue
                if need.get(k, 0) < v:
                    need[k] = v
            wl = []
            for k, v in need.items():
                if waited[o["eng"]].get(k, 0) >= v:
                    continue
                waited[o["eng"]][k] = v
                wl.append((k, v))
            o["waits"] = wl
        final_waits = [(k, counts[k]) for k in semkeys if k[0] in ("dma", "dmasw") and counts[k] > start[k]]
        with ExitStack() as es:
            sems = semobj
            blk = es.enter_context(nc.Block())

            def run(engname):
                def body(eng):
                    for o in ops:
                        if o["eng"] != engname:
                            continue
                        for k, v in o["waits"]:
                            eng.wait_ge(sems[k], v)
                        ins = o["fn"](eng)
                        if o["inc"]:
                            ins.then_inc(sems[o["semkey"]], o["inc"])
                return body

            present = {o["eng"] for o in ops}

            def run_sync(eng):
                run("sync")(eng)
                for k, v in final_waits:
                    eng.wait_ge(sems[k], v)

            blk.sync(run_sync)
            if False:
                blk.sync(run("sync"))
            if "act" in present:
                blk.scalar(run("act"))
            if "pool" in present:
                blk.gpsimd(run("pool"))
            if "pe" in present:
                blk.tensor(run("pe"))
            if "dve" in present:
                blk.vector(run("dve"))
        nc.all_engine_barrier()
        for k in semkeys:
            pool["free_" + k[0]].append((semobj[k], counts[k]))
        return len(ops)


class Ctx:
    def __init__(self, nc, ext_in, ext_out):
        self.nc = nc
        self.ext_in = ext_in
        self.ext_out = ext_out
        self.d = {}

    def dram(self, name, shape, dtype=F32):
        kind = "ExternalInput" if name in self.ext_in else ("ExternalOutput" if name in self.ext_out else "Internal")
        ap = self.nc.dram_tensor(name, list(shape), dtype, kind=kind).ap()
        self.d[name] = ap
        return ap


INPUT_SHAPES = {
    "xs": (NB * 128, D), "ctxs": (256, D), "cc": (128, 8, 2),
    "w_ada": (2, D, 6 * D), "b_ada": (2, 6 * D), "gn1": (2, D), "gn2": (2, D), "gfin": (1, D),
    "w_in_e": (D, 1792), "sink": (1, 8), "g_sgu": (1, 512), "w_spT": (128, 8, 128), "b_spc": (128, 8),
    "w_out_e": (D, D), "w_in_o": (D, 3 * D), "conv_c": (128, 3, 8), "w_out_o": (D, D),
    "wr": (2, D, 36), "br": (2, 36),
    "w_gate": (2 * NE * 128, 4096), "w_up": (2 * NE * 128, 4096), "w_down": (2 * NE * 128, 4096),
    "ropeC": (NB * 128, 64), "ropeS": (NB * 128, 64), "maskP": (128, 512), "maskN": (128, 512),
    "widx": (128, 1), "ident": (128, 128), "utri": (128, 128), "tokf": (128, NF), "thr": (1, NSS0), "rifill": (128, NS0 * 4),
}


def _rms_rstd(s, ss, col, xtile, xres, junk, tag):
    s.add("act", lambda e: e.activation(out=junk[:], in_=xtile, func=AF.Square, accum_out=ss[:, col:col + 1]),
          r=[xres], w=[_JR.get(id(junk), "junk" + tag), ("ss" + tag, col)])
    s.add("act", lambda e: e.activation(out=ss[:, col + 1:col + 2], in_=ss[:, col:col + 1], func=AF.Sqrt,
                                        scale=1.0 / D, bias=EPS),
          r=[("ss" + tag, col)], w=[("rs" + tag, col)])
    s.add("dve", lambda e: e.reciprocal(out=ss[:, col + 1:col + 2], in_=ss[:, col + 1:col + 2]),
          r=[("rs" + tag, col)], w=[("rs" + tag, col)])


def phase_mod(C):
    nc = C.nc
    Dm = C.d
    with ExitStack() as es:
        T = lambda n, sh, dt: es.enter_context(nc.sbuf_tensor(U(n), sh, dt))
        P = lambda n, sh, dt: es.enter_context(nc.psum_tensor(U(n), sh, dt))
        cc = T("m_cc", [128, 8, 2], F32)
        sl = T("m_sl", [128, 8, 2], F32)
        wa = [T(f"m_wa{i}", [128, 8, 512], F32) for i in range(2)]
        bb = [T(f"m_bb{l}", [2, 6 * D], F32) for l in range(2)]
        mr = [T(f"m_mr{l}", [2, 6 * D], F32) for l in range(2)]
        ps = [P(f"m_ps{i}", [2, 512], F32) for i in range(2)]
        s = Sched(nc, "mod")
        s.add("sync", lambda e: e.dma_start(out=cc[:], in_=Dm["cc"]), w=["cc"], dma="cc")
        s.add("act", lambda e: e.activation(out=sl[:], in_=cc[:], func=AF.Silu), r=["cc"], w=["sl"])
        for l in range(2):
            s.add("sync", lambda e, l=l: e.dma_start(out=bb[l][:], in_=Dm["b_ada"][l:l + 1, :].broadcast_to([2, 6 * D])),
                  w=[("bb", l)], dma=("bb", l))
        it = 0
        import os as _os
        for l in range(2):
            for j in range(int(_os.environ.get("MODJ", "12"))):
                par = it % 2
                it += 1
                s.add("sync", lambda e, l=l, j=j, par=par: e.dma_start(
                    out=wa[par][:], in_=Dm["w_ada"][l, :, j * 512:(j + 1) * 512].rearrange("(k p) n -> p k n", p=128)),
                    w=[("wa", par)], dma=("wa", par))
                for k in range(8):
                    s.add("pe", lambda e, k=k, par=par: e.matmul(out=ps[par][:], lhsT=sl[:, k, :], rhs=wa[par][:, k, :],
                                                                  start=(k == 0), stop=(k == 7)),
                          r=["sl", ("wa", par)], w=[("ps", par)])
                s.add("dve", lambda e, l=l, j=j, par=par: e.tensor_tensor(
                    out=mr[l][:, j * 512:(j + 1) * 512], in0=ps[par][:], in1=bb[l][:, j * 512:(j + 1) * 512], op=ALU.add),
                    r=[("ps", par), ("bb", l)], w=[("mr", l)])
            s.add("sync", lambda e, l=l: e.dma_start(out=Dm["modrow"][l], in_=mr[l][:]), r=[("mr", l)], w=[("modrow", l)],
                  dma=("modst", l))
        s.add("sync", lambda e: e.nop(), r=[("modrow", 0), ("modrow", 1)])
        s.emit()


def _load_modrows(s, C, T, l, names, sidx=0):
    out = {}
    for nm, pi in names.items():
        t = T(f"mr_{l}_{sidx}_{nm}", [128, D], F32)
        s.add("sync", lambda e, t=t, pi=pi: e.dma_start(
            out=t[:], in_=C.d["modrow"][l, sidx:sidx + 1, pi * D:(pi + 1) * D].broadcast_to([128, D])),
            w=[("mrow", l, sidx, nm)], dma=("mrow", l, sidx, nm))
        out[nm] = t
    return out


def _make_G(s, T, C, l, which, scrow, res_sc, tag):
    if getattr(s, "gn_tile", None) is None:
        s.gn_tile = T("gn_shared", [128, D], F32)
    g = s.gn_tile
    s.add("sync", lambda e: e.dma_start(out=g[:], in_=C.d[which][l:l + 1, :].broadcast_to([128, D])),
          w=["gnS"], dma="gnS")
    s.add("dve", lambda e: e.scalar_tensor_tensor(out=scrow[:], in0=scrow[:], scalar=1.0, in1=g[:], op0=ALU.add, op1=ALU.mult),
          r=[res_sc, "gnS"], w=[res_sc])


def _load_w_bf16(s, T, name, dst, src_ap, ncols, stg, tag):
    j = 0
    c0 = 0
    while c0 < ncols:
        cw = min(256, ncols - c0)
        par = (j % 2) if stg[0] is not stg[1] else 0
        s.add("sync", lambda e, c0=c0, cw=cw, par=par: e.dma_start(
            out=stg[par][:, :, 0:cw], in_=src_ap[:, c0:c0 + cw].rearrange("(k p) n -> p k n", p=128)),
            w=[("stg", par)], dma=("stg", par))
        s.add("pool", lambda e, c0=c0, cw=cw, par=par: e.tensor_copy(out=dst[:, :, c0:c0 + cw], in_=stg[par][:, :, 0:cw]),
              r=[("stg", par)], w=[(tag, j)])
        c0 += cw
        j += 1
    return [(tag, jj) for jj in range(j)]


def _tail(s, n, lat, latres, tl, l):
    nc = tl["nc"]
    ss, junk = tl["ss2"], tl["junk"]
    _rms_rstd(s, ss, 2 * n, lat[:], latres, junk, "t")
    h2 = tl["h2"][0]
    hres = ("h2", 0)
    s.add("dve", lambda e: e.scalar_tensor_tensor(out=h2[:], in0=lat[:], scalar=ss[:, 2 * n + 1:2 * n + 2], in1=tl["G2"][:],
                                                  op0=ALU.mult, op1=ALU.mult),
          r=[latres, ("rst", 2 * n), "G2"], w=[hres])
    s.add("dve", lambda e: e.tensor_tensor(out=h2[:], in0=h2[:], in1=tl["SH2"][:], op=ALU.add), r=[hres, "SH2"], w=[hres])
    h2b = tl["h2b"]
    s.add("act", lambda e: e.activation(out=h2b[:], in_=h2[:], func=AF.Copy), r=[hres], w=["h2b"])
    s.add("sync", lambda e: e.dma_start(out=tl["H2b"][n * 128:(n + 1) * 128, :], in_=h2b[:]), r=["h2b"], w=[("H2", n)],
          dma=("h2st", 0))
    pT = tl["pT32"]
    for k in range(8):
        s.add("pe", lambda e, k=k: e.transpose(out=pT[k // 4][:, k % 4, :], in_=h2[:, k * 128:(k + 1) * 128], identity=tl["identf"][:]),
              r=[hres, "identf"], w=[("pT32", k // 4)])
    h2T = tl["h2T"]
    s.add("act", lambda e: e.activation(out=h2T[:, 0:4, :], in_=pT[0][:], func=AF.Copy), r=[("pT32", 0)], w=["h2Ta"])
    s.add("act", lambda e: e.activation(out=h2T[:, 4:8, :], in_=pT[1][:], func=AF.Copy), r=[("pT32", 1)], w=["h2Tb"])
    pR = tl["pR"]
    for k in range(8):
        s.add("pe", lambda e, k=k: e.matmul(out=pR[:, 0:36], lhsT=h2T[:, k, :], rhs=tl["wr"][:, k, :], start=(k == 0), stop=(k == 7)),
              r=["h2Ta", "h2Tb", "wr"], w=["pR"])
    lg = tl["lg_all"]
    s.add("dve", lambda e: e.tensor_tensor(out=lg[:, n, :], in0=pR[:, 0:36], in1=tl["brow"][:], op=ALU.add), r=["pR", "brow"], w=[("lg", n)])


def _route_all(s, tl, NT, T):
    L = tl["lg_all"]
    G = L[:, 0:NT, 0:4]
    E4 = L[:, 0:NT, 4:36].rearrange("p n (g j) -> p n g j", g=4)
    OHf, OHb, gates = tl["OHf"], tl["OHb"], tl["gates"]
    gmax = T("r_gmax", [128, NF], F32)
    ohg = T("r_ohg", [128, NF, 4], F32)
    gsh = T("r_gsh", [128, NF, 4], F32)
    sumg = T("r_sumg", [128, NF], F32)
    tmp4 = T("r_tmp4", [128, NF, 4, 8], F32)
    esel = T("r_esel", [128, NF, 8], F32)
    class _V:
        def __init__(self, i):
            self.i = i

        def __getitem__(self, idx):
            return tmp4[:, :, self.i, :][idx]
    esel2, oh1, oh2 = _V(0), _V(1), _V(2)
    m1 = T("r_m1", [128, NF], F32)
    m2 = T("r_m2", [128, NF], F32)
    e21 = T("r_e21", [128, NF], F32)
    w1 = T("r_w1", [128, NF], F32)
    w2 = T("r_w2", [128, NF], F32)
    allg = [("lg", n) for n in range(NT)]
    b3 = lambda t, k: t[:, 0:NT].unsqueeze(2).broadcast_to([128, NT, k])
    s.add("dve", lambda e: e.reduce_max(out=gmax[:, 0:NT], in_=G, axis=AX.X), r=allg, w=["gmax"])
    s.add("dve", lambda e: e.tensor_tensor(out=ohg[:, 0:NT, :], in0=G, in1=b3(gmax, 4), op=ALU.is_ge), r=allg + ["gmax"], w=["ohg"])
    s.add("dve", lambda e: e.tensor_tensor(out=gsh[:, 0:NT, :], in0=G, in1=b3(gmax, 4), op=ALU.subtract), r=allg + ["gmax"], w=["gsh"])
    s.add("act", lambda e: e.activation(out=gsh[:, 0:NT, :], in_=gsh[:, 0:NT, :], func=AF.Exp), r=["gsh"], w=["gsh"])
    s.add("dve", lambda e: e.reduce_sum(out=sumg[:, 0:NT], in_=gsh[:, 0:NT, :], axis=AX.X), r=["gsh"], w=["sumg"])
    s.add("dve", lambda e: e.reciprocal(out=sumg[:, 0:NT], in_=sumg[:, 0:NT]), r=["sumg"], w=["sumg"])
    s.add("dve", lambda e: e.tensor_tensor(out=tmp4[:, 0:NT], in0=E4, in1=ohg[:, 0:NT, :].unsqueeze(3).broadcast_to([128, NT, 4, 8]), op=ALU.mult),
          r=allg + ["ohg"], w=["tmp4"])
    s.add("dve", lambda e: e.reduce_sum(out=esel[:, 0:NT, :], in_=tmp4[:, 0:NT].rearrange("p n g j -> p n j g"), axis=AX.X), r=["tmp4"], w=["esel"])
    s.add("dve", lambda e: e.reduce_max(out=m1[:, 0:NT], in_=esel[:, 0:NT, :], axis=AX.X), r=["esel"], w=["m1"])
    s.add("dve", lambda e: e.tensor_tensor(out=oh1[:, 0:NT, :], in0=esel[:, 0:NT, :], in1=b3(m1, 8), op=ALU.is_ge), r=["esel", "m1"], w=["oh1"])
    s.add("dve", lambda e: e.scalar_tensor_tensor(out=esel2[:, 0:NT, :], in0=oh1[:, 0:NT, :], scalar=-1.0e30, in1=esel[:, 0:NT, :], op0=ALU.mult, op1=ALU.add),
          r=["oh1", "esel"], w=["esel2"])
    s.add("dve", lambda e: e.reduce_max(out=m2[:, 0:NT], in_=esel2[:, 0:NT, :], axis=AX.X), r=["esel2"], w=["m2"])
    s.add("dve", lambda e: e.tensor_tensor(out=oh2[:, 0:NT, :], in0=esel2[:, 0:NT, :], in1=b3(m2, 8), op=ALU.is_ge), r=["esel2", "m2"], w=["oh2"])
    s.add("dve", lambda e: e.tensor_tensor(out=e21[:, 0:NT], in0=m2[:, 0:NT], in1=m1[:, 0:NT], op=ALU.subtract), r=["m1", "m2"], w=["e21"])
    s.add("act", lambda e: e.activation(out=e21[:, 0:NT], in_=e21[:, 0:NT], func=AF.Exp), r=["e21"], w=["e21"])
    s.add("dve", lambda e: e.tensor_scalar(out=w1[:, 0:NT], in0=e21[:, 0:NT], scalar1=1.0, scalar2=None, op0=ALU.add), r=["e21"], w=["w1"])
    s.add("dve", lambda e: e.reciprocal(out=w1[:, 0:NT], in_=w1[:, 0:NT]), r=["w1"], w=["w1"])
    s.add("dve", lambda e: e.tensor_tensor(out=w2[:, 0:NT], in0=e21[:, 0:NT], in1=w1[:, 0:NT], op=ALU.mult), r=["e21", "w1"], w=["w2"])
    s.add("dve", lambda e: e.tensor_tensor(out=gates[:, 0:NT, 0], in0=w1[:, 0:NT], in1=sumg[:, 0:NT], op=ALU.mult), r=["w1", "sumg"], w=["gate0"])
    s.add("dve", lambda e: e.tensor_tensor(out=gates[:, 0:NT, 1], in0=w2[:, 0:NT], in1=sumg[:, 0:NT], op=ALU.mult), r=["w2", "sumg"], w=["gate1"])
    for k, oh, nm in ((0, oh1, "oh1"), (1, oh2, "oh2")):
        s.add("dve", lambda e, k=k, oh=oh: e.tensor_tensor(
            out=OHf[:, 0:NT, k, :].rearrange("p n (g j) -> p n g j", g=4),
            in0=ohg[:, 0:NT, :].unsqueeze(3).broadcast_to([128, NT, 4, 8]), in1=oh[:, 0:NT, :].unsqueeze(2).broadcast_to([128, NT, 4, 8]), op=ALU.mult),
            r=["ohg", nm], w=[("OHf", k)])
    s.add("dve", lambda e: e.tensor_tensor(out=OHb[:, 0:NT, :], in0=OHf[:, 0:NT, 0, :], in1=OHf[:, 0:NT, 1, :], op=ALU.add),
          r=[("OHf", 0), ("OHf", 1)], w=["OHb"])
    s.add("sync", lambda e: e.nop(), r=["OHb", "gate0", "gate1"])


def _tail_alloc(C, T, P, s, l, NT, mrow):
    nc = C.nc
    tl = dict(nc=nc)
    tl["ss2"] = T("t_ss2", [128, 2 * NF], F32)
    tl["h2"] = [T("t_h2_0", [128, D], F32)] * 2
    tl["h2T"] = T("t_h2T", [128, 8, 128], F32)
    tl["pT32"] = [P(f"t_pT32_{i}", [128, 4, 128], F32) for i in range(2)]
    tl["pR"] = P("t_pR", [128, 512], F32)
    tl["lg_all"] = T("t_lg_all", [128, NF, 36], F32)
    tl["sc"] = T("t_sc", [128, 16], F32)
    tl["identf"] = T("t_identf", [128, 128], F32)
    tl["wr"] = T("t_wr", [128, 8, 36], F32)
    tl["brow"] = T("t_brow", [128, 36], F32)
    tl["G2"] = mrow["sc2"]
    tl["SH2"] = mrow["sh2"]
    tl["H2b"] = C.d["H2b"]
    tl["h2b"] = T("t_h2b", [128, D], BF16)
    tl["junk"] = tl["h2b"]
    _JR[id(tl["h2b"])] = "h2b"
    s.add("dve", lambda e: e.memset(tl["ss2"][:], 0.0), w=[("sst", c) for c in range(2 * NF)])
    s.add("sync", lambda e: e.dma_start(out=tl["identf"][:], in_=C.d["ident"]), w=["identf"], dma="identf")
    s.add("sync", lambda e: e.dma_start(out=tl["wr"][:], in_=C.d["wr"][l].rearrange("(k p) n -> p k n", p=128)), w=["wr"], dma="wr")
    s.add("sync", lambda e: e.dma_start(out=tl["brow"][:], in_=C.d["br"][l:l + 1, :].broadcast_to([128, 36])), w=["brow"], dma="brow")
    return tl


def phase_l0(C, pers):
    nc = C.nc
    Dm = C.d
    OHf, OHb, gates = pers["OHf"], pers["OHb"], pers["gates"]
    with ExitStack() as es0:
        T0 = lambda n, sh, dt: es0.enter_context(nc.sbuf_tensor(U(n), sh, dt))
        qT_all = T0("qT_all", [128, NB, 512], BF16)
        kT_all = T0("kT_all", [128, NB * 128], BF16)
        Vaug = T0("Vaug", [128, NB, 2, 65], BF16)
        sg_all = T0("sg_all", [128, NB, 512], BF16)
        kcT = T0("kcT", [128, 256], BF16)
        Vcaug = T0("Vcaug", [128, 2, 2, 65], BF16)
        identb = T0("identb", [128, 128], BF16)
        with ExitStack() as es:
            T = lambda n, sh, dt: es.enter_context(nc.sbuf_tensor(U(n), sh, dt))
            P = lambda n, sh, dt: es.enter_context(nc.psum_tensor(U(n), sh, dt))
            s = Sched(nc, "l0a")
            Gt = T("a_Gt", [128, D], F32)
            St = T("a_St", [128, D], F32)
            gnt = T("a_gnt", [128, D], F32)
            s.add("sync", lambda e: e.dma_start(out=gnt[:], in_=Dm["gn1"][0:1, :].broadcast_to([128, D])), w=["gn"], dma="gn")

            def load_rows(sidx):
                s.add("sync", lambda e: e.dma_start(out=St[:], in_=Dm["modrow"][0, sidx:sidx + 1, 0:D].broadcast_to([128, D])), w=["SHrow"], dma="SHrow")
                s.add("sync", lambda e: e.dma_start(out=Gt[:], in_=Dm["modrow"][0, sidx:sidx + 1, D:2 * D].broadcast_to([128, D])), w=["Grow"], dma="Grow")
                s.add("dve", lambda e: e.scalar_tensor_tensor(out=Gt[:], in0=Gt[:], scalar=1.0, in1=gnt[:], op0=ALU.add, op1=ALU.mult),
                      r=["Grow", "gn"], w=["Grow"])
            stg = [T("a_stg0", [128, 8, 256], F32)] * 2
            w_in = T("a_win", [128, 8, 1792], BF16)
            wres = _load_w_bf16(s, T, "w_in_e", w_in, Dm["w_in_e"], 1792, stg, "win")
            identf = T("a_identf", [128, 128], F32)
            s.add("sync", lambda e: e.dma_start(out=identf[:], in_=Dm["ident"]), w=["identf"], dma="identf")
            s.add("dve", lambda e: e.tensor_copy(out=identb[:], in_=identf[:]), r=["identf"], w=["identb"])
            wspf = T("a_wspf", [128, 8, 128], F32)
            wspb = T("a_wspb", [128, 8, 128], BF16)
            s.add("sync", lambda e: e.dma_start(out=wspf[:], in_=Dm["w_spT"]), w=["wspf"], dma="wspf")
            s.add("dve", lambda e: e.tensor_copy(out=wspb[:], in_=wspf[:]), r=["wspf"], w=["wspb"])
            bsp = T("a_bsp", [128, 8], F32)
            s.add("sync", lambda e: e.dma_start(out=bsp[:], in_=Dm["b_spc"]), w=["bsp"], dma="bsp")
            gsgu = T("a_gsgu", [128, 512], F32)
            s.add("sync", lambda e: e.dma_start(out=gsgu[:], in_=Dm["g_sgu"].broadcast_to([128, 512])), w=["gsgu"], dma="gsgu")
            rCS = [T(f"a_rCS{i}", [128, 2, 64], F32) for i in range(2)]
            xr = [T(f"a_x{i}", [128, D], F32) for i in range(2)]
            junk = T("a_junk", [128, D], BF16)
            ss = T("a_ss", [128, 2 * (NB + 2)], F32)
            hf = T("a_hf", [128, D], F32)
            hb = T("a_hb", [128, D], BF16)
            hT = T("a_hT", [128, 8, 128], BF16)
            qkf2 = [T(f"a_qkf{i}", [128, 640], F32) for i in range(2)]
            qk1 = T("a_qk1", [128, 640], F32)
            qk2 = T("a_qk2", [128, 640], F32)
            qkb = T("a_qkb", [128, 640], BF16)
            gu2 = [T(f"a_gu{i}", [128, 512], F32) for i in range(2)]
            gz2 = [T(f"a_gz{i}", [128, 512], F32) for i in range(2)]
            zb = T("a_zb", [128, 512], BF16)
            st6 = T("a_st6", [128, 6], F32)
            mv = T("a_mv", [128, 4], F32)
            t1 = T("a_t1", [128, 512], F32)
            pT = P("a_pT", [128, 8, 128], BF16)
            psq = P("a_psq", [128, 512], F32)
            pskv = P("a_pskv", [128, 512], F32)
            psu = P("a_psu", [128, 512], F32)
            psz = P("a_psz", [128, 512], F32)
            pssp = P("a_pssp", [128, 512], F32)
            s.add("dve", lambda e: e.memset(ss[:], 0.0), w=[("ssa", c) for c in range(2 * (NB + 2))])
            s.add("dve", lambda e: e.memset(Vaug[:], 1.0), w=[("V", n) for n in range(NB)])
            s.add("dve", lambda e: e.memset(Vcaug[:], 1.0), w=[("Vc", j) for j in range(2)])

            def blockA(n, is_ctx, stage=0):
                par = n % 2
                qkf, gu, gz = qkf2[par], gu2[par], gz2[par]
                if stage == 2:
                    return blockA2(n)
                col = 2 * n
                src = Dm["ctxs"] if is_ctx else Dm["xs"]
                bi = n - NB if is_ctx else n
                s.add("sync", lambda e: e.dma_start(out=xr[par][:], in_=src[bi * 128:(bi + 1) * 128, :]), w=[("x", par)], dma=("x", par))
                _rms_rstd(s, ss, col, xr[par][:], ("x", par), junk, "a")
                gres = "Grow"
                sres = "SHrow"
                s.add("dve", lambda e: e.scalar_tensor_tensor(out=hf[:], in0=xr[par][:], scalar=ss[:, col + 1:col + 2], in1=Gt[:],
                                                              op0=ALU.mult, op1=ALU.mult),
                      r=[("x", par), ("rsa", col), gres], w=["hf"])
                s.add("pool", lambda e: e.tensor_tensor(out=hb[:], in0=hf[:], in1=St[:], op=ALU.add), r=["hf", sres], w=["hb"])
                for k in range(8):
                    s.add("pe", lambda e, k=k: e.transpose(out=pT[:, k, :], in_=hb[:, k * 128:(k + 1) * 128], identity=identb[:]),
                          r=["hb", "identb"], w=["pT"])
                s.add("act", lambda e: e.activation(out=hT[:], in_=pT[:], func=AF.Copy), r=["pT"], w=["hT"])
                groups = [(pskv, "pskv", 512, 256)] if is_ctx else [(psq, "psq", 0, 512), (pskv, "pskv", 512, 256), (psu, "psu", 768, 512), (psz, "psz", 1280, 512)]
                for (pt, pn, c0, cw) in groups:
                    for k in range(8):
                        s.add("pe", lambda e, pt=pt, c0=c0, cw=cw, k=k: e.matmul(out=pt[:, 0:cw], lhsT=hT[:, k, :], rhs=w_in[:, k, c0:c0 + cw],
                                                                                 start=(k == 0), stop=(k == 7)),
                              r=["hT"] + wres, w=[pn])
                if is_ctx:
                    j = bi
                    s.add("act", lambda e: e.activation(out=qkb[:, 0:128], in_=pskv[:, 0:128], func=AF.Copy), r=["pskv"], w=["qkb"])
                    s.add("act", lambda e: e.activation(out=Vcaug[:, j, :, 0:64], in_=pskv[:, 128:256].rearrange("p (h d) -> p h d", h=2), func=AF.Copy),
                          r=["pskv"], w=[("Vc", j)])
                    s.add("pe", lambda e: e.transpose(out=pT[:, 0, :], in_=qkb[:, 0:128], identity=identb[:]), r=["qkb", "identb"], w=["pT"])
                    s.add("dve", lambda e: e.tensor_copy(out=kcT[:, j * 128:(j + 1) * 128], in_=pT[:, 0, :]), r=["pT"], w=[("kcT", j)])
                    return
                s.add("act", lambda e: e.activation(out=qkf[:, 0:512], in_=psq[:], func=AF.Copy), r=["psq"], w=[("qkf_q", par)])
                s.add("act", lambda e: e.activation(out=qkf[:, 512:640], in_=pskv[:, 0:128], func=AF.Copy), r=["pskv"], w=[("qkf_k", par)])
                s.add("act", lambda e: e.activation(out=Vaug[:, n, :, 0:64], in_=pskv[:, 128:256].rearrange("p (h d) -> p h d", h=2), func=AF.Copy),
                      r=["pskv"], w=[("V", n)])
                s.add("act", lambda e: e.activation(out=gu[:], in_=psu[:], func=AF.Gelu), r=["psu"], w=[("gu", par)])
                s.add("act", lambda e: e.activation(out=gz[:], in_=psz[:], func=AF.Gelu), r=["psz"], w=[("gz", par)])

            def blockA2(n):
                par = n % 2
                qkf, gu, gz = qkf2[par], gu2[par], gz2[par]
                rt = rCS[par]
                s.add("pool", lambda e: e.dma_start(out=rt[:, 0, :], in_=Dm["ropeC"][n * 128:(n + 1) * 128, :]), w=[("rC", par)], dma=("rC", par))
                s.add("pool", lambda e: e.dma_start(out=rt[:, 1, :], in_=Dm["ropeS"][n * 128:(n + 1) * 128, :]), w=[("rS", par)], dma=("rS", par))
                v3 = lambda t: t[:].rearrange("p (h d) -> p h d", d=64)
                s.add("dve", lambda e: e.tensor_tensor(out=v3(qk1), in0=v3(qkf), in1=rt[:, 0:1, :].broadcast_to([128, 10, 64]), op=ALU.mult),
                      r=[("qkf_q", par), ("qkf_k", par), ("rC", par)], w=["qk1"])
                v5 = lambda t, pr: t[:].rearrange("p (h a b c) -> p h a b c", a=2, b=2, c=16)[:, :, :, pr, :]
                sv = lambda pr: rt[:, 1:2, :].rearrange("p o (a b c) -> p o a b c", a=2, b=2, c=16)[:, :, :, pr, :].broadcast_to([128, 10, 2, 16])
                for pr in range(2):
                    s.add("pool", lambda e, pr=pr: e.tensor_tensor(out=v5(qk2, pr), in0=v5(qkf, 1 - pr), in1=sv(pr), op=ALU.mult),
                          r=[("qkf_q", par), ("qkf_k", par), ("rS", par)], w=[("qk2", pr)])
                s.add("dve", lambda e: e.tensor_tensor(out=qkb[:], in0=qk1[:], in1=qk2[:], op=ALU.add), r=["qk1", ("qk2", 0), ("qk2", 1)], w=["qkb"])
                for j in range(5):
                    s.add("pe", lambda e, j=j: e.transpose(out=pT[:, j, :], in_=qkb[:, j * 128:(j + 1) * 128], identity=identb[:]),
                          r=["qkb", "identb"], w=["pT"])
                s.add("act", lambda e: e.activation(out=qT_all[:, n, :], in_=pT[:, 0:4, :].rearrange("p a b -> p (a b)"), func=AF.Copy), r=["pT"], w=[("qT", n)])
                s.add("dve", lambda e: e.tensor_copy(out=kT_all[:, n * 128:(n + 1) * 128], in_=pT[:, 4, :]), r=["pT"], w=[("kT", n)])
                s.add("dve", lambda e: e.bn_stats(out=st6[:], in_=gz[:]), r=[("gz", par)], w=["st6"])
                s.add("dve", lambda e: e.bn_aggr(out=mv[:, 0:2], in_=st6[:]), r=["st6"], w=["mv"])
                s.add("act", lambda e: e.activation(out=mv[:, 2:3], in_=mv[:, 1:2], func=AF.Sqrt, scale=1.0, bias=EPS), r=["mv"], w=["mv2"])
                s.add("dve", lambda e: e.reciprocal(out=mv[:, 3:4], in_=mv[:, 2:3]), r=["mv2"], w=["mv3"])
                s.add("dve", lambda e: e.tensor_scalar(out=gz[:], in0=gz[:], scalar1=mv[:, 0:1], scalar2=mv[:, 3:4], op0=ALU.subtract, op1=ALU.mult),
                      r=[("gz", par), "mv", "mv3"], w=[("gz", par)])
                s.add("pool", lambda e: e.tensor_tensor(out=zb[:], in0=gz[:], in1=gsgu[:], op=ALU.mult), r=[("gz", par), "gsgu"], w=["zb"])
                for h in range(8):
                    s.add("pe", lambda e, h=h: e.matmul(out=pssp[:, h * 64:(h + 1) * 64], lhsT=wspb[:, h, :], rhs=zb[:, h * 64:(h + 1) * 64],
                                                        start=True, stop=True), r=["zb", "wspb"], w=["pssp"])
                s.add("dve", lambda e: e.tensor_tensor(out=t1[:].rearrange("p (h c) -> p h c", c=64), in0=pssp[:].rearrange("p (h c) -> p h c", c=64),
                                                       in1=bsp[:].unsqueeze(2).broadcast_to([128, 8, 64]), op=ALU.add),
                      r=["pssp", "bsp"], w=["t1"])
                s.add("pool", lambda e: e.tensor_tensor(out=sg_all[:, n, :], in0=t1[:], in1=gu[:], op=ALU.mult), r=["t1", ("gu", par)], w=[("sg", n)])

            load_rows(1)
            blockA(NB, True)
            blockA(NB + 1, True)
            load_rows(0)
            blockA(0, False, 1)
            for n in range(1, NB):
                blockA(n, False, 1)
                blockA(n - 1, False, 2)
            blockA(NB - 1, False, 2)
            s.emit()
        with ExitStack() as es:
            T = lambda n, sh, dt: es.enter_context(nc.sbuf_tensor(U(n), sh, dt))
            P = lambda n, sh, dt: es.enter_context(nc.psum_tensor(U(n), sh, dt))
            s = Sched(nc, "l0b")
            mrow = _load_modrows(s, C, T, 0, {"g1": 2, "sh2": 3, "sc2": 4}, 0)
            _make_G(s, T, C, 0, "gn2", mrow["sc2"], ("mrow", 0, 0, "sc2"), "b0")
            tl = _tail_alloc(C, T, P, s, 0, NF, mrow)
            tl.update(OHf=OHf, OHb=OHb, gates=gates)
            s.add("dve", lambda e: e.tensor_copy(out=tl["sc"][:, 15:16], in_=tl["sc"][:, 15:16]), r=[("mrow", 0, 0, "sc2")], w=["G2"])
            s.add("pool", lambda e: e.tensor_copy(out=tl["sc"][:, 14:15], in_=tl["sc"][:, 14:15]), r=[("mrow", 0, 0, "sh2")], w=["SH2"])
            stg = [T("b_stg0", [128, 8, 256], F32)] * 2
            w_out = T("b_wout", [128, 8, D], BF16)
            wres = _load_w_bf16(s, T, "w_out_e", w_out, Dm["w_out_e"], D, stg, "wout")
            mkf = T("b_mkf", [128, 2, 512], F32)
            mkb = T("b_mkb", [128, 2, 512], BF16)
            s.add("sync", lambda e: e.dma_start(out=mkf[:, 0, :], in_=Dm["maskP"]), w=["mkf0"], dma="mkf0")
            s.add("sync", lambda e: e.dma_start(out=mkf[:, 1, :], in_=Dm["maskN"]), w=["mkf1"], dma="mkf1")
            s.add("dve", lambda e: e.tensor_copy(out=mkb[:], in_=mkf[:]), r=["mkf0", "mkf1"], w=["mkb"])
            snk = T("b_snk", [128, 8], F32)
            exps = T("b_exps", [128, 8], F32)
            s.add("sync", lambda e: e.dma_start(out=snk[:], in_=Dm["sink"].broadcast_to([128, 8])), w=["snk"], dma="snk")
            s.add("act", lambda e: e.activation(out=exps[:], in_=snk[:], func=AF.Exp), r=["snk"], w=["exps"])
            PT = [T(f"b_PT{i}", [128, 512], BF16) for i in range(5)]
            mix2 = [T(f"b_mix{i}", [128, 512], BF16) for i in range(2)]
            mixT = T("b_mixT", [128, 8, 128], BF16)
            dd2 = [T(f"b_dd{i}", [128, 8], F32) for i in range(2)]
            xr = [T(f"b_x{i}", [128, D], F32) for i in range(2)]
            tt = T("b_tt", [128, D], F32)
            lat = [T(f"b_lat{i}", [128, D], F32) for i in range(2)]
            pS = [P(f"b_pS{i}", [128, 512], F32) for i in range(2)]
            pO = [P(f"b_pO{i}", [128, 512], F32) for i in range(2)]
            pT = P("b_pT", [128, 8, 128], BF16)
            pW = [tl["pT32"][0], tl["pT32"][1]]
            pWv = [p[:].rearrange("p a b -> p (a b)") for p in pW]

            def ATT(n):
                par = n % 2
                mix = mix2[par]
                dd = dd2[par]
                for h in range(2):
                    tiles = [("c", 0), ("c", 1)] + ([("p", n - 1)] if n > 0 else []) + [("l", n), ("n", n + 1)]
                    for i, (kind, j) in enumerate(tiles):
                        if kind == "c":
                            kt = kcT[h * 64:(h + 1) * 64, j * 128:(j + 1) * 128]
                            kres = ("kcT", j)
                        else:
                            kt = kT_all[h * 64:(h + 1) * 64, j * 128:(j + 1) * 128]
                            kres = ("kT", j)
                        masked = kind in ("p", "n")
                        ps_ = pS[i % 2]
                        s.add("pe", lambda e, kt=kt, ps_=ps_, masked=masked, n=n, h=h: e.matmul(
                            out=ps_[:], lhsT=kt, rhs=qT_all[h * 64:(h + 1) * 64, n, :], start=True, stop=not masked),
                            r=[kres, ("qT", n)], w=[("pS", i % 2)])
                        if masked:
                            mi = 0 if kind == "p" else 1
                            s.add("pe", lambda e, ps_=ps_, mi=mi: e.matmul(out=ps_[:], lhsT=identb[:], rhs=mkb[:, mi, :], start=False, stop=True),
                                  r=["mkb", "identb"], w=[("pS", i % 2)])
                        s.add("act", lambda e, ps_=ps_, i=i: e.activation(out=PT[i][:], in_=ps_[:], func=AF.Exp, scale=0.125),
                              r=[("pS", i % 2)], w=[("PT", i)])
                    nt = len(tiles)
                    for g in range(4):
                        for i, (kind, j) in enumerate(tiles):
                            if kind == "c":
                                vt = Vcaug[:, j, h, :]
                                vres = ("Vc", j)
                            else:
                                vt = Vaug[:, j, h, :]
                                vres = ("V", j)
                            s.add("pe", lambda e, g=g, i=i, vt=vt, h=h, nt=nt: e.matmul(
                                out=pO[h][:, g * 65:(g + 1) * 65], lhsT=PT[i][:, g * 128:(g + 1) * 128], rhs=vt,
                                start=(i == 0), stop=(i == nt - 1)), r=[("PT", i), vres], w=[("pO", h)])
                    pov = pO[h][:, 0:260].rearrange("p (g d) -> p g d", d=65)
                    s.add("dve", lambda e, pov=pov, h=h: e.tensor_tensor(out=dd[:, h * 4:(h + 1) * 4], in0=pov[:, :, 64], in1=exps[:, h * 4:(h + 1) * 4], op=ALU.add),
                          r=[("pO", h), "exps"], w=[("dd", par, h)])
                    s.add("dve", lambda e, h=h: e.reciprocal(out=dd[:, h * 4:(h + 1) * 4], in_=dd[:, h * 4:(h + 1) * 4]), r=[("dd", par, h)], w=[("dd", par, h)])
                    s.add("dve", lambda e, pov=pov, h=h: e.tensor_tensor(
                        out=mix[:, h * 256:(h + 1) * 256].rearrange("p (g d) -> p g d", d=64), in0=pov[:, :, 0:64],
                        in1=dd[:, h * 4:(h + 1) * 4].unsqueeze(2).broadcast_to([128, 4, 64]), op=ALU.mult),
                        r=[("pO", h), ("dd", par, h)], w=[("mix", par, h)])

            def OUT(n):
                par = n % 2
                mix = mix2[par]
                dd = dd2[par]
                s.add("sync", lambda e, n=n, par=par: e.dma_start(out=xr[par][:], in_=Dm["xs"][n * 128:(n + 1) * 128, :]), w=[("x", par)], dma=("x", par))
                for k in range(8):
                    if k < 4:
                        src = mix[:, k * 128:(k + 1) * 128]
                        rr = [("mix", par, 0), ("mix", par, 1)]
                    else:
                        src = sg_all[:, n, (k - 4) * 128:(k - 3) * 128]
                        rr = [("sg", n)]
                    s.add("pe", lambda e, k=k, src=src: e.transpose(out=pT[:, k, :], in_=src, identity=identb[:]), r=rr + ["identb"], w=["pT"])
                s.add("act", lambda e: e.activation(out=mixT[:], in_=pT[:], func=AF.Copy), r=["pT"], w=["mixT"])
                for jn in range(2):
                    for k in range(8):
                        s.add("pe", lambda e, jn=jn, k=k: e.matmul(out=pWv[jn], lhsT=mixT[:, k, :], rhs=w_out[:, k, jn * 512:(jn + 1) * 512],
                                                                   start=(k == 0), stop=(k == 7)),
                              r=["mixT"] + wres, w=[("pT32", jn)])
                for jn in range(2):
                    s.add("dve", lambda e, jn=jn: e.tensor_tensor(out=tt[:, jn * 512:(jn + 1) * 512], in0=pWv[jn], in1=mrow["g1"][:, jn * 512:(jn + 1) * 512], op=ALU.mult),
                          r=[("pT32", jn), ("mrow", 0, 0, "g1")], w=[("tt", jn)])
                lt = lat[par]
                s.add("dve", lambda e, lt=lt, par=par: e.tensor_tensor(out=lt[:], in0=tt[:], in1=xr[par][:], op=ALU.add),
                      r=[("tt", 0), ("tt", 1), ("x", par)], w=[("lat", par)])
                s.add("sync", lambda e, lt=lt, n=n: e.dma_start(out=Dm["L1"][n * 128:(n + 1) * 128, :], in_=lt[:]), r=[("lat", par)], w=[("L1", n)],
                      dma=("latst", par))
                _tail(s, n, lt, ("lat", par), tl, 0)

            ATT(0)
            for n in range(1, NF):
                ATT(n)
                OUT(n - 1)
            OUT(NF - 1)
            _route_all(s, tl, NF, T)
            s.add("sync", lambda e: e.nop(), r=[("L1", n) for n in range(NF)] + [("H2", n) for n in range(NF)])
            s.emit()


def phase_dispatch(C, pers, NT, NS):
    nc = C.nc
    Dm = C.d
    OHf, OHb, gates = pers["OHf"], pers["OHb"], pers["gates"]
    be_i = pers["be_i"]
    NSS = NS // 2
    with ExitStack() as es:
        T = lambda n, sh, dt: es.enter_context(nc.sbuf_tensor(U(n), sh, dt))
        P = lambda n, sh, dt: es.enter_context(nc.psum_tensor(U(n), sh, dt))
        s = Sched(nc, "disp")
        utf = T("d_utf", [128, 128], F32)
        utb = T("d_utb", [128, 128], BF16)
        oneb = T("d_oneb", [128, 128], BF16)
        OHbb = T("d_OHbb", [128, NF, 32], BF16)
        rank = T("d_rank", [128, NF, 32], F32)
        pr_ = [P(f"d_pr{i}", [128, 32], F32) for i in range(2)]
        pc = P("d_pc", [128, 32], F32)
        cnt = T("d_cnt", [128, 32], F32)
        rr = T("d_rr", [128, 32], F32)
        gt = T("d_gt", [128, 32], F32)
        pad = T("d_pad", [128, 32], F32)
        cs = [T(f"d_cs{i}", [128, 32], F32) for i in range(2)]
        pst = T("d_pst", [128, 32], F32)
        thr = T("d_thr", [128, NSS0], F32)
        cmp_ = T("d_cmp", [128, NSS0, 32], F32)
        bef = T("d_bef", [128, NSS0], F32)
        tokf = T("d_tokf", [128, NF], F32)
        tmp = T("d_tmp", [128, NF, 32], F32)
        prod = T("d_prod", [128, NF, 2, 32], F32)
        destf = T("d_destf", [128, NF, 2], F32)
        desti = T("d_desti", [128, NF, 2], I32)
        ris = T("d_ris", [128, NF, 2, 4], F32)
        fill = T("d_fill", [128, NS0 * 4], F32)
        s.add("sync", lambda e: e.dma_start(out=utf[:], in_=Dm["utri"]), w=["utf"], dma="utf")
        s.add("sync", lambda e: e.dma_start(out=thr[:], in_=Dm["thr"].broadcast_to([128, NSS0])), w=["thr"], dma="thr")
        s.add("sync", lambda e: e.dma_start(out=tokf[:], in_=Dm["tokf"]), w=["tokf"], dma="tokf")
        s.add("sync", lambda e: e.dma_start(out=fill[:], in_=Dm["rifill"]), w=["fill"], dma="fill")
        s.add("sync", lambda e: e.dma_start(out=Dm["RI"].rearrange("(p b) c -> p (b c)", p=128), in_=fill[:]), r=["fill"], w=["RIfill"], dma="RIfill")
        s.add("dve", lambda e: e.tensor_copy(out=utb[:], in_=utf[:]), r=["utf"], w=["utb"])
        s.add("dve", lambda e: e.memset(oneb[:], 1.0), w=["oneb"])
        s.add("dve", lambda e: e.tensor_copy(out=OHbb[:, 0:NT, :], in_=OHb[:, 0:NT, :]), w=["OHbb"])
        for n in range(NT):
            p = pr_[n % 2]
            s.add("pe", lambda e, n=n, p=p: e.matmul(out=p[:], lhsT=utb[:], rhs=OHbb[:, n, :], start=True, stop=(n == 0)), r=["utb", "OHbb"], w=[("pr", n % 2)])
            for m in range(n):
                s.add("pe", lambda e, m=m, n=n, p=p: e.matmul(out=p[:], lhsT=oneb[:], rhs=OHbb[:, m, :], start=False, stop=(m == n - 1)),
                      r=["oneb", "OHbb"], w=[("pr", n % 2)])
            s.add("dve", lambda e, n=n, p=p: e.tensor_copy(out=rank[:, n, :], in_=p[:]), r=[("pr", n % 2)], w=[("rank", n)])
        for n in range(NT):
            s.add("pe", lambda e, n=n: e.matmul(out=pc[:], lhsT=oneb[:], rhs=OHbb[:, n, :], start=(n == 0), stop=(n == NT - 1)), r=["oneb", "OHbb"], w=["pc"])
        s.add("dve", lambda e: e.tensor_copy(out=cnt[:], in_=pc[:]), r=["pc"], w=["cnt"])
        cmp2 = T("d_cmp2", [128, 32, 17], F32)
        s.add("dve", lambda e: e.tensor_tensor(out=cmp2[:], in0=cnt[:].unsqueeze(2).broadcast_to([128, 32, 17]),
                                               in1=thr[:, 0:17].unsqueeze(1).broadcast_to([128, 32, 17]), op=ALU.is_gt), r=["cnt", "thr"], w=["cmp2"])
        s.add("dve", lambda e: e.reduce_sum(out=pad[:], in_=cmp2[:], axis=AX.X), r=["cmp2"], w=["pad"])
        s.add("dve", lambda e: e.tensor_scalar(out=pad[:], in0=pad[:], scalar1=256.0, scalar2=None, op0=ALU.mult), r=["pad"], w=["pad"])
        s.add("dve", lambda e: e.tensor_copy(out=cs[0][:], in_=pad[:]), r=["pad"], w=[("cs", 0)])
        cur = 0
        for sh in (1, 2, 4, 8, 16):
            nx = 1 - cur
            s.add("dve", lambda e, cur=cur, nx=nx, sh=sh: e.tensor_copy(out=cs[nx][:, 0:sh], in_=cs[cur][:, 0:sh]), r=[("cs", cur)], w=[("csa", nx)])
            s.add("dve", lambda e, cur=cur, nx=nx, sh=sh: e.tensor_tensor(out=cs[nx][:, sh:32], in0=cs[cur][:, sh:32], in1=cs[cur][:, 0:32 - sh], op=ALU.add),
                  r=[("cs", cur), ("csa", nx)], w=[("cs", nx)])
            cur = nx
        pend = cs[cur]
        pres = ("cs", cur)
        s.add("dve", lambda e: e.tensor_tensor(out=pst[:], in0=pend[:], in1=pad[:], op=ALU.subtract), r=[pres, "pad"], w=["pst"])
        s.add("dve", lambda e: e.tensor_tensor(out=cmp_[:, 0:NSS, :], in0=pend[:].unsqueeze(1).broadcast_to([128, NSS, 32]),
                                               in1=thr[:, 0:NSS].unsqueeze(2).broadcast_to([128, NSS, 32]), op=ALU.is_le), r=[pres, "thr"], w=["cmp"])
        s.add("dve", lambda e: e.reduce_sum(out=bef[:, 0:NSS], in_=cmp_[:, 0:NSS, :], axis=AX.X), r=["cmp"], w=["bef"])
        s.add("dve", lambda e: e.tensor_scalar(out=bef[:, 0:NSS], in0=bef[:, 0:NSS], scalar1=31.0, scalar2=None, op0=ALU.min), r=["bef"], w=["bef"])
        s.add("dve", lambda e: e.tensor_scalar(out=be_i[:, 1, 0:NSS], in0=thr[:, 0:NSS], scalar1=pend[:, 31:32], scalar2=OOB, op0=ALU.is_ge, op1=ALU.mult), r=[pres, "thr"], w=["be_i2"])
        s.add("dve", lambda e: e.scalar_tensor_tensor(out=be_i[:, 0, 0:NSS], in0=bef[:, 0:NSS], scalar=128.0, in1=be_i[:, 1, 0:NSS], op0=ALU.mult, op1=ALU.add), r=["bef", "be_i2"], w=["be_i"])
        rb = es.enter_context(nc.gpsimd.register(U("rb_d")))
        s.add("pool", lambda e: e.reg_mov(rb, NS0 * 128 - 1))
        s.add("dve", lambda e: e.tensor_tensor(out=tmp[:, 0:NT, :], in0=rank[:, 0:NT, :], in1=pst[:].unsqueeze(1).broadcast_to([128, NT, 32]), op=ALU.add),
              r=[("rank", n) for n in range(NT)] + ["pst"], w=["tmp"])
        s.add("dve", lambda e: e.tensor_tensor(out=prod[:, 0:NT], in0=OHf[:, 0:NT], in1=tmp[:, 0:NT, :].unsqueeze(2).broadcast_to([128, NT, 2, 32]), op=ALU.mult),
              r=["tmp"], w=["prod"])
        s.add("dve", lambda e: e.reduce_sum(out=destf[:, 0:NT, :], in_=prod[:, 0:NT], axis=AX.X), r=["prod"], w=["destf"])
        s.add("dve", lambda e: e.tensor_copy(out=desti[:, 0:NT, :], in_=destf[:, 0:NT, :]), r=["destf"], w=["desti"])
        s.add("dve", lambda e: e.memset(ris[:], 0.0), w=["ris"])
        for k in range(2):
            s.add("dve", lambda e, k=k: e.tensor_copy(out=ris[:, 0:NT, k, 0], in_=tokf[:, 0:NT]), r=["tokf", "ris"], w=[("ris0", k)])
            s.add("dve", lambda e, k=k: e.tensor_copy(out=ris[:, 0:NT, k, 1], in_=gates[:, 0:NT, k]), r=["ris"], w=[("ris1", k)])
            s.add("dve", lambda e, k=k: e.tensor_scalar(out=ris[:, 0:NT, k, 2], in0=tokf[:, 0:NT], scalar1=2.0, scalar2=float(k), op0=ALU.mult, op1=ALU.add),
                  r=["tokf", "ris"], w=[("ris2", k)])
        rres = [("ris0", 0), ("ris0", 1), ("ris1", 0), ("ris1", 1), ("ris2", 0), ("ris2", 1)]
        allsc = []
        import os as _os
        _dcut = int(_os.environ.get("DCUT", "0"))
        _thr = int(_os.environ.get("DTHR", "8"))
        for n in range(NT if _dcut == 0 else (_dcut - 1)):
            for k in range(2):
                s.add("pool", lambda e, n=n, k=k: e.indirect_dma_start(
                    out=Dm["RI"][:, :], out_offset=bass.IndirectOffsetOnAxis(ap=desti[:, n, k:k + 1], axis=0),
                    in_=ris[:, n, k, :], in_offset=None, bounds_check=rb, oob_is_err=False),
                    r=rres + ["desti", "RIfill"] + (allsc[-_thr:-_thr + 1] if len(allsc) >= _thr else []), w=[("RIsc", n, k)], dma="RIsc")
                allsc.append(("RIsc", n, k))
        s.add("sync", lambda e: e.nop(), r=allsc + ["be_i", "be_i2"])
        s.emit()


def phase_moe(C, pers, l, NT, NS):
    nc = C.nc
    Dm = C.d
    be_i = pers["be_i"]
    import os as _os
    NSx = min(NS, int(_os.environ.get("MCUT", "1000")))
    with ExitStack() as es:
        T = lambda n, sh, dt: es.enter_context(nc.sbuf_tensor(U(n), sh, dt))
        P = lambda n, sh, dt: es.enter_context(nc.psum_tensor(U(n), sh, dt))
        s = Sched(nc, f"moe{l}")
        identf = T("e_identf", [128, 128], F32)
        identb = T("e_identb", [128, 128], BF16)
        s.add("sync", lambda e: e.dma_start(out=identf[:], in_=Dm["ident"]), w=["identf"], dma="identf")
        s.add("dve", lambda e: e.tensor_copy(out=identb[:], in_=identf[:]), r=["identf"], w=["identb"])
        sg_ = [T(f"e_sgf{i}", [128, 8, 512], F32) for i in range(2)]
        su_ = [T(f"e_suf{i}", [128, 8, 512], F32) for i in range(2)]
        sd_ = [T(f"e_sdf{i}", [128, 4, D], F32) for i in range(2)]
        wg = [T(f"e_wg{i}", [128, 8, 512], BF16) for i in range(2)]
        wu = [T(f"e_wu{i}", [128, 8, 512], BF16) for i in range(2)]
        wd = [T(f"e_wd{i}", [128, 4, D], BF16) for i in range(2)]
        ri = [T(f"e_ri{i}", [128, 4], F32) for i in range(4)]
        ii = [T(f"e_ii{i}", [128, 4], I32) for i in range(4)]
        wi = [T(f"e_wi{i}", [128, 1], I32) for i in range(3)]
        xg = [T(f"e_xg{i}", [128, D], BF16) for i in range(3)]
        xT = [T(f"e_xT{i}", [128, 8, 128], BF16) for i in range(2)]
        sgt = T("e_sg", [128, 512], F32)
        hid = T("e_hid", [128, 512], BF16)
        hidT = T("e_hidT", [128, 4, 128], BF16)
        y = [T(f"e_y{i}", [128, D], F32) for i in range(2)]
        pTx = P("e_pTx", [128, 8, 128], BF16)
        pTc = P("e_pTc", [128, 4, 128], BF16)
        pG = P("e_pG", [128, 512], F32)
        pU = P("e_pU", [128, 512], F32)
        pY = [P(f"e_pY{i}", [128, 512], F32) for i in range(2)]
        for i in range(3):
            s.add("dve", lambda e, i=i: e.memset(xg[i][:], 0.0), w=[("xg", i)])
        cst = T("e_cst", [128, 1], F32)
        s.add("sync", lambda e: e.dma_start(out=cst[:], in_=Dm["widx"]), w=["cst"], dma="cst")
        rbx = es.enter_context(nc.gpsimd.register(U("rbx")))
        rby = es.enter_context(nc.gpsimd.register(U("rby")))
        rbw1 = es.enter_context(nc.gpsimd.register(U("rbw1")))
        s.add("pool", lambda e: e.reg_mov(rbx, NT * 128 - 1))
        s.add("pool", lambda e: e.reg_mov(rby, 2 * NT * 128 - 1))
        s.add("pool", lambda e: e.reg_mov(rbw1, 2 * NE * 128 - 1))

        def wgather(st, src, nm, u):
            par = u % 2
            s.add("pool", lambda e: e.indirect_dma_start(
                out=st[par][:].rearrange("p a b -> p (a b)"), out_offset=None, in_=Dm[src],
                in_offset=bass.IndirectOffsetOnAxis(ap=wi[u % 3][:, 0:1], axis=0), bounds_check=rbw1, oob_is_err=False),
                r=[("wi", u % 3)], w=[("st" + nm, par)], dma=("st" + nm, par))

        def load_w(u):
            q = u % 3
            s.add("dve", lambda e: e.tensor_scalar(out=wi[q][:], in0=cst[:], scalar1=be_i[:, 0, u:u + 1], scalar2=float(l * NE * 128), op0=ALU.add, op1=ALU.add),
                  r=["cst"], w=[("wi", q)])
            wgather(sg_, "w_gate", "wg", u)
            wgather(su_, "w_up", "wu", u)
            wgather(sd_, "w_down", "wd", u)

        def conv_gu(u):
            par = u % 2
            s.add("act", lambda e: e.activation(out=wg[par][:], in_=sg_[par][:], func=AF.Copy), r=[("stwg", par)], w=[("wg", par)])
            s.add("dve", lambda e: e.tensor_copy(out=wu[par][:], in_=su_[par][:]), r=[("stwu", par)], w=[("wu", par)])

        def conv_d(u):
            par = u % 2
            s.add("act", lambda e: e.activation(out=wd[par][:, 0:2, :], in_=sd_[par][:, 0:2, :], func=AF.Copy), r=[("stwd", par)], w=[("wd", par)])
            s.add("dve", lambda e: e.tensor_copy(out=wd[par][:, 2:4, :], in_=sd_[par][:, 2:4, :]), r=[("stwd", par)], w=[("wd2", par)])

        def load_gu(b):
            par = b % 3
            q = b % 4
            s.add("sync", lambda e: e.dma_start(out=ri[q][:], in_=Dm["RI"][b * 128:(b + 1) * 128, :]), w=[("ri", q)], dma=("ri", q))
            s.add("pool", lambda e: e.tensor_copy(out=ii[q][:], in_=ri[q][:]), r=[("ri", q)], w=[("ii", q)])
            s.add("pool", lambda e: e.indirect_dma_start(
                out=xg[par][:, :], out_offset=None, in_=Dm["H2b"][:, :],
                in_offset=bass.IndirectOffsetOnAxis(ap=ii[q][:, 0:1], axis=0), bounds_check=rbx, oob_is_err=False),
                r=[("ii", q)], w=[("xg", par)], dma=("xg", par))

        def stA(b):
            par = b % 2
            xp = b % 3
            for k in range(8):
                s.add("pe", lambda e, k=k: e.transpose(out=pTx[:, k, :], in_=xg[xp][:, k * 128:(k + 1) * 128], identity=identb[:]),
                      r=[("xg", xp), "identb"], w=["pTx"])
            s.add("act", lambda e: e.activation(out=xT[par][:], in_=pTx[:], func=AF.Copy), r=["pTx"], w=[("xT", par)])

        def stB(b):
            par = b % 2
            wp = (b // 2) % 2
            for k in range(8):
                s.add("pe", lambda e, k=k: e.matmul(out=pG[:], lhsT=xT[par][:, k, :], rhs=wg[wp][:, k, :], start=(k == 0), stop=(k == 7)),
                      r=[("xT", par), ("wg", wp)], w=["pG"])
            for k in range(8):
                s.add("pe", lambda e, k=k: e.matmul(out=pU[:], lhsT=xT[par][:, k, :], rhs=wu[wp][:, k, :], start=(k == 0), stop=(k == 7)),
                      r=[("xT", par), ("wu", wp)], w=["pU"])
            s.add("act", lambda e: e.activation(out=sgt[:], in_=pG[:], func=AF.Silu), r=["pG"], w=["sgt"])
            s.add("dve", lambda e: e.tensor_tensor(out=hid[:], in0=pU[:], in1=sgt[:], op=ALU.mult), r=["pU", "sgt"], w=["hid"])

        def stT(b):
            for k in range(4):
                s.add("pe", lambda e, k=k: e.transpose(out=pTc[:, k, :], in_=hid[:, k * 128:(k + 1) * 128], identity=identb[:]),
                      r=["hid", "identb"], w=["pTc"])
            s.add("act", lambda e: e.activation(out=hidT[:], in_=pTc[:], func=AF.Copy), r=["pTc"], w=["hidT"])

        def stD(b):
            par = b % 2
            q = b % 4
            wp = (b // 2) % 2
            for jn in range(2):
                for k in range(4):
                    s.add("pe", lambda e, jn=jn, k=k: e.matmul(out=pY[jn][:], lhsT=hidT[:, k, :], rhs=wd[wp][:, k, jn * 512:(jn + 1) * 512],
                                                               start=(k == 0), stop=(k == 3)),
                          r=["hidT", ("wd", wp), ("wd2", wp)], w=[("pY", jn)])
            s.add("act", lambda e: e.activation(out=y[par][:, 0:512], in_=pY[0][:], func=AF.Copy, scale=ri[q][:, 1:2]),
                  r=[("pY", 0), ("ri", q)], w=[("y", par, 0)])
            s.add("dve", lambda e: e.tensor_scalar(out=y[par][:, 512:1024], in0=pY[1][:], scalar1=ri[q][:, 1:2], scalar2=None, op0=ALU.mult),
                  r=[("pY", 1), ("ri", q)], w=[("y", par, 1)])
            s.add("pool", lambda e: e.indirect_dma_start(
                out=Dm["Y"][:, :], out_offset=bass.IndirectOffsetOnAxis(ap=ii[q][:, 2:3], axis=0),
                in_=y[par][:, :], in_offset=None, bounds_check=rby, oob_is_err=False),
                r=[("y", par, 0), ("y", par, 1), ("ii", q)], w=[("Ysc", b)], dma=("ysc", par))

        NSSx = (NSx + 1) // 2
        load_w(0)
        load_gu(0)
        if NSx > 1:
            load_gu(1)
        if NSx > 2:
            load_gu(2)
        if NSSx > 1:
            load_w(1)
        conv_gu(0)
        conv_d(0)
        stA(0)
        stB(0)
        for b in range(NSx):
            if b + 3 < NSx:
                load_gu(b + 3)
            u = b // 2
            if b % 2 == 0:
                if u + 2 < NSSx:
                    load_w(u + 2)
            if b + 1 < NSx:
                stA(b + 1)
            stT(b)
            if b % 2 == 0 and u + 1 < NSSx:
                conv_gu(u + 1)
            if b % 2 == 1 and u + 1 < NSSx:
                conv_d(u + 1)
            if b + 1 < NSx:
                stB(b + 1)
            stD(b)
        s.add("sync", lambda e: e.nop(), r=[("Ysc", b) for b in range(NSx)])
        s.emit()


def phase_combine(C, l, NT, src, dst, final):
    nc = C.nc
    Dm = C.d
    with ExitStack() as es:
        T = lambda n, sh, dt: es.enter_context(nc.sbuf_tensor(U(n), sh, dt))
        s = Sched(nc, f"cmb{l}")
        mrow = _load_modrows(s, C, T, l, {"g2": 5}, 0)
        yt = [T(f"c_y{i}", [128, 2, D], F32) for i in range(2)]
        lt = [T(f"c_l{i}", [128, D], F32) for i in range(3)]
        ot = [T(f"c_o{i}", [128, D], F32) for i in range(2)]
        junk = T("c_junk", [128, D], BF16)
        ss = T("c_ss", [128, 2 * NF], F32)
        if final:
            gf = T("c_gf", [128, D], F32)
            s.add("sync", lambda e: e.dma_start(out=gf[:], in_=Dm["gfin"].broadcast_to([128, D])), w=["gf"], dma="gf")
            s.add("dve", lambda e: e.memset(ss[:], 0.0), w=[("ssc", c) for c in range(2 * NF)])
        outs = []

        def stage1(n):
            par = n % 2
            lp = n % 3
            s.add("sync", lambda e: e.dma_start(out=yt[par][:], in_=Dm["Y"][n * 256:(n + 1) * 256, :].rearrange("(p k) d -> p k d", k=2)),
                  w=[("yt", par)], dma=("yt", par))
            s.add("sync", lambda e: e.dma_start(out=lt[lp][:], in_=Dm[src][n * 128:(n + 1) * 128, :]), w=[("lt", lp)], dma=("lt", lp))
            s.add("pool", lambda e: e.tensor_tensor(out=yt[par][:, 0, :], in0=yt[par][:, 0, :], in1=yt[par][:, 1, :], op=ALU.add),
                  r=[("yt", par)], w=[("yt", par)])
            s.add("dve", lambda e: e.tensor_tensor(out=yt[par][:, 0, :], in0=yt[par][:, 0, :], in1=mrow["g2"][:], op=ALU.mult),
                  r=[("yt", par), ("mrow", l, 0, "g2")], w=[("yt", par)])
            s.add("dve", lambda e: e.tensor_tensor(out=lt[lp][:], in0=lt[lp][:], in1=yt[par][:, 0, :], op=ALU.add),
                  r=[("yt", par), ("lt", lp)], w=[("lt", lp)])

        def stage2(n):
            par = n % 2
            lp = n % 3
            if not final:
                s.add("sync", lambda e: e.dma_start(out=Dm[dst][n * 128:(n + 1) * 128, :], in_=lt[lp][:]), r=[("lt", lp)], w=[(dst, n)],
                      dma=("cst", lp))
            else:
                _rms_rstd(s, ss, 2 * n, lt[lp][:], ("lt", lp), junk, "c")
                s.add("dve", lambda e: e.scalar_tensor_tensor(out=ot[par][:], in0=lt[lp][:], scalar=ss[:, 2 * n + 1:2 * n + 2], in1=gf[:],
                                                              op0=ALU.mult, op1=ALU.mult),
                      r=[("lt", lp), ("rsc", 2 * n), "gf"], w=[("ot", par)])
                s.add("sync", lambda e: e.dma_start(out=Dm[dst][n * 128:(n + 1) * 128, :], in_=ot[par][:]), r=[("ot", par)], w=[(dst, n)],
                      dma=("cst", par))
            outs.append((dst, n))

        stage1(0)
        for n in range(NT):
            if n + 1 < NT:
                stage1(n + 1)
            stage2(n)
        s.add("sync", lambda e: e.nop(), r=outs)
        s.emit()


def phase_l1(C, pers):
    nc = C.nc
    Dm = C.d
    OHf, OHb, gates = pers["OHf"], pers["OHb"], pers["gates"]
    NW = 9
    with ExitStack() as es:
        T = lambda n, sh, dt: es.enter_context(nc.sbuf_tensor(U(n), sh, dt))
        P = lambda n, sh, dt: es.enter_context(nc.psum_tensor(U(n), sh, dt))
        s = Sched(nc, "l1")
        tt = T("f_tt", [128, D], F32)
        s.gn_tile = tt
        mrow = _load_modrows(s, C, T, 1, {"sh1": 0, "sc1": 1, "g1": 2, "sh2": 3, "sc2": 4}, 0)
        _make_G(s, T, C, 1, "gn1", mrow["sc1"], ("mrow", 1, 0, "sc1"), "c0")
        _make_G(s, T, C, 1, "gn2", mrow["sc2"], ("mrow", 1, 0, "sc2"), "c1")
        tl = _tail_alloc(C, T, P, s, 1, NO, mrow)
        tl.update(OHf=OHf, OHb=OHb, gates=gates)
        s.add("dve", lambda e: e.tensor_copy(out=tl["sc"][:, 15:16], in_=tl["sc"][:, 15:16]), r=[("mrow", 1, 0, "sc2")], w=["G2"])
        s.add("pool", lambda e: e.tensor_copy(out=tl["sc"][:, 14:15], in_=tl["sc"][:, 14:15]), r=[("mrow", 1, 0, "sh2")], w=["SH2"])
        stg = [T(f"f_stg{i}", [128, 8, 256], F32) for i in range(1)] * 2
        w_in = T("f_win", [128, 8, 3 * D], BF16)
        wres = _load_w_bf16(s, T, "w_in_o", w_in, Dm["w_in_o"], 3 * D, stg, "win")
        w_out = T("f_wout", [128, 8, D], BF16)
        wores = _load_w_bf16(s, T, "w_out_o", w_out, Dm["w_out_o"], D, stg, "wout")
        identb = T("f_identb", [128, 128], BF16)
        s.add("dve", lambda e: e.tensor_copy(out=identb[:], in_=tl["identf"][:]), r=["identf"], w=["identb"])
        cw = T("f_cw", [128, 3, 8], F32)
        s.add("sync", lambda e: e.dma_start(out=cw[:], in_=Dm["conv_c"]), w=["cw"], dma="cw")
        xr = [T(f"f_x{i}", [128, D], F32) for i in range(1)] * 2
        junk = tl["junk"]
        ss = T("f_ss", [128, 2 * NF], F32)
        s.add("dve", lambda e: e.memset(ss[:], 0.0), w=[("ssa", c) for c in range(2 * NF)])
        hf = T("f_hf", [128, D], F32)
        hb = T("f_hb", [128, D], BF16)
        hT = [T(f"f_hT{i}", [128, 8, 512], BF16) for i in range(1)]
        Yh = [T(f"f_Yh{i}", [128, 8, 514], F32) for i in range(2)]
        bgb = [T(f"f_bg{i}", [128, 8, 512], BF16) for i in range(2)]
        cgs = T("f_cgs", [128, 512], F32)
        ca = T("f_ca", [128, 512], F32)
        cb = T("f_cb", [128, 512], F32)
        gT = hT[0]
        lat = xr
        pT = P("f_pT", [128, 8, 128], BF16)
        pRot = [P(f"f_pRot{i}", [128, 512], F32) for i in range(4)]
        rot = [0]
        pW = [tl["pT32"][0], tl["pT32"][1]]
        pWv = [p[:].rearrange("p a b -> p (a b)") for p in pW]
        for i in range(2):
            s.add("dve", lambda e, i=i: e.memset(Yh[i][:], 0.0), w=[("Yh", i, c) for c in range(8)] + [("Yhl", i), ("Yhr", i)])

        def window_front(w):
            nt = 512 if w < 8 else 128
            hp = 0
            bp = w % 2
            for q in range(nt // 128):
                n = w * 4 + q
                par = 0
                s.add("sync", lambda e, n=n, par=par: e.dma_start(out=xr[par][:], in_=Dm["L2"][n * 128:(n + 1) * 128, :]), r=[("L2", n)], w=[("x", par)], dma=("x", par))
                _rms_rstd(s, ss, 2 * n, xr[par][:], ("x", par), junk, "a")
                s.add("dve", lambda e, n=n, par=par: e.scalar_tensor_tensor(out=hf[:], in0=xr[par][:], scalar=ss[:, 2 * n + 1:2 * n + 2], in1=mrow["sc1"][:],
                                                                            op0=ALU.mult, op1=ALU.mult),
                      r=[("x", par), ("rsa", 2 * n), ("mrow", 1, 0, "sc1")], w=["hf"])
                s.add("pool", lambda e: e.tensor_tensor(out=hb[:], in0=hf[:], in1=mrow["sh1"][:], op=ALU.add), r=["hf", ("mrow", 1, 0, "sh1")], w=["hb"])
                for k in range(8):
                    s.add("pe", lambda e, k=k: e.transpose(out=pT[:, k, :], in_=hb[:, k * 128:(k + 1) * 128], identity=identb[:]), r=["hb", "identb"], w=["pT"])
                s.add("act", lambda e, q=q, hp=hp: e.activation(out=hT[hp][:, :, q * 128:(q + 1) * 128], in_=pT[:], func=AF.Copy), r=["pT"], w=[("hT", hp, q), "HG"])
            hres = [("hT", hp, q) for q in range(nt // 128)] + ["HG"]
            yi = w % 2
            for c in range(8):
                def nxt():
                    i = rot[0] % 4
                    rot[0] += 1
                    return pRot[i], ("pRot", i)
                pC, pCn = nxt()
                pX, pXn = nxt()
                grp = [(pC, pCn, D + c * 128), (pX, pXn, 2 * D + c * 128)]
                if w < 8:
                    pB, pBn = nxt()
                    grp.append((pB, pBn, c * 128))
                for (pt, pn, c0) in grp:
                    for k in range(8):
                        s.add("pe", lambda e, pt=pt, c0=c0, k=k, nt=nt, hp=hp: e.matmul(out=pt[:, 0:nt], lhsT=w_in[:, k, c0:c0 + 128], rhs=hT[hp][:, k, 0:nt],
                                                                                         start=(k == 0), stop=(k == 7)),
                              r=hres + wres, w=[pn])
                s.add("act", lambda e, nt=nt, pC=pC: e.activation(out=cgs[:, 0:nt], in_=pC[:, 0:nt], func=AF.Copy), r=[pCn], w=["cgs"])
                s.add("dve", lambda e, c=c, nt=nt, yi=yi, pX=pX: e.tensor_tensor(out=Yh[yi][:, c, 1:1 + nt], in0=pX[:, 0:nt], in1=cgs[:, 0:nt], op=ALU.mult),
                      r=[pXn, "cgs"], w=[("Yh", yi, c)])
                if w < 8:
                    s.add("act", lambda e, c=c, bp=bp, pB=pB: e.activation(out=bgb[bp][:, c, :], in_=pB[:], func=AF.Copy), r=[pBn], w=[("bg", bp, c)])
            yall = [("Yh", yi, c) for c in range(8)]
            if w > 0:
                yp = (w - 1) % 2
                s.add("pool", lambda e, yi=yi, yp=yp: e.tensor_copy(out=Yh[yp][:, :, 513:514], in_=Yh[yi][:, :, 1:2]), r=yall, w=[("Yhr", yp)])
                s.add("pool", lambda e, yi=yi, yp=yp: e.tensor_copy(out=Yh[yi][:, :, 0:1], in_=Yh[yp][:, :, 512:513]), r=[("Yh", yp, c) for c in range(8)], w=[("Yhl", yi)])
            else:
                s.add("pool", lambda e, yi=yi: e.memset(Yh[yi][:, :, 0:1], 0.0), w=[("Yhl", yi)])

        def window_back(w):
            yi = w % 2
            hp = w % 2
            yres = [("Yhl", yi), ("Yhr", yi)]
            for c in range(8):
                s.add("dve", lambda e, c=c: e.tensor_scalar(out=ca[:], in0=Yh[yi][:, c, 1:513], scalar1=cw[:, 1, c:c + 1], scalar2=None, op0=ALU.mult),
                      r=yres + [("Yh", yi, c), "cw"], w=["ca"])
                s.add("dve", lambda e, c=c: e.scalar_tensor_tensor(out=cb[:], in0=Yh[yi][:, c, 0:512], scalar=cw[:, 0, c:c + 1], in1=ca[:], op0=ALU.mult, op1=ALU.add),
                      r=yres + [("Yh", yi, c), "cw", "ca"], w=["cb"])
                s.add("dve", lambda e, c=c: e.scalar_tensor_tensor(out=ca[:], in0=Yh[yi][:, c, 2:514], scalar=cw[:, 2, c:c + 1], in1=cb[:], op0=ALU.mult, op1=ALU.add),
                      r=yres + [("Yh", yi, c), "cw", "cb"], w=["ca"])
                s.add("pool", lambda e, c=c: e.tensor_tensor(out=gT[:, c, :], in0=ca[:], in1=bgb[hp][:, c, :], op=ALU.mult), r=["ca", ("bg", hp, c)], w=[("gT", c), "HG"])
            for q in range(4):
                n = w * 4 + q
                par = 0
                s.add("sync", lambda e, n=n, par=par: e.dma_start(out=xr[par][:], in_=Dm["L2"][n * 128:(n + 1) * 128, :]), r=[("L2", n)], w=[("x", par)], dma=("x", par))
                for jn in range(2):
                    for k in range(8):
                        s.add("pe", lambda e, jn=jn, k=k, q=q: e.matmul(out=pWv[jn], lhsT=gT[:, k, q * 128:(q + 1) * 128], rhs=w_out[:, k, jn * 512:(jn + 1) * 512],
                                                                        start=(k == 0), stop=(k == 7)),
                              r=[("gT", c) for c in range(8)] + ["HG"] + wores, w=[("pT32", jn)])
                for jn in range(2):
                    s.add("dve", lambda e, jn=jn: e.tensor_tensor(out=tt[:, jn * 512:(jn + 1) * 512], in0=pWv[jn], in1=mrow["g1"][:, jn * 512:(jn + 1) * 512], op=ALU.mult),
                          r=[("pT32", jn), ("mrow", 1, 0, "g1")], w=[("tt", jn)])
                lt = lat[par]
                s.add("dve", lambda e, lt=lt, par=par: e.tensor_tensor(out=lt[:], in0=tt[:], in1=xr[par][:], op=ALU.add),
                      r=[("tt", 0), ("tt", 1), ("x", par)], w=[("x", par)])
                s.add("sync", lambda e, lt=lt, n=n: e.dma_start(out=Dm["L3"][n * 128:(n + 1) * 128, :], in_=lt[:]), r=[("x", par)], w=[("L3", n)],
                      dma=("latst", par))
                _tail(s, n, lt, ("x", par), tl, 1)

        import os as _os
        _l1w = int(_os.environ.get("L1W", "8"))
        _l1b = int(_os.environ.get("L1B", "1"))
        window_front(0)
        for w in range(_l1w):
            window_front(w + 1)
            if _l1b:
                window_back(w)
        if _l1w == 8 and _l1b:
            _route_all(s, tl, NO, T)
        s.add("sync", lambda e: e.nop(), r=[("L3", n) for n in range(NO if (_l1w == 8 and _l1b) else 0)] + [("H2", n) for n in range(NO if (_l1w == 8 and _l1b) else 0)])
        s.emit()


def build(stages=("mod", "l0", "d0", "m0", "c0", "l1", "d1", "m1", "c1"), ext_in_extra=(), ext_out_extra=()):
    nc = bass.Bass("TRN2", target_bir_lowering=False)
    ext_in = set(INPUT_SHAPES) | set(ext_in_extra)
    ext_out = {"out"} | set(ext_out_extra)
    C = Ctx(nc, ext_in, ext_out)
    for k, sh in INPUT_SHAPES.items():
        C.dram(k, sh, F32)
    C.dram("out", (NO * 128, D), F32)
    C.dram("modrow", (2, 2, 6 * D), F32)
    C.dram("L1", (NF * 128, D), F32)
    C.dram("L2", (NF * 128, D), F32)
    C.dram("L3", (NO * 128, D), F32)
    C.dram("H2", (NF * 128, D), F32)
    C.dram("H2b", (NF * 128, D), BF16)
    C.dram("RI", (NS0 * 128, 4), F32)
    C.dram("Y", (2 * NF * 128, D), F32)
    C.dram("DBG", (128, 2 * NF + NS0 + 64), F32)
    with ExitStack() as es:
        _SEMPOOL.clear()
        _SEMPOOL["es"] = es
        pers = {}
        pers["OHf"] = es.enter_context(nc.sbuf_tensor("p_OHf", [128, NF, 2, 32], F32))
        pers["OHb"] = es.enter_context(nc.sbuf_tensor("p_OHb", [128, NF, 32], F32))
        pers["gates"] = es.enter_context(nc.sbuf_tensor("p_gates", [128, NF, 2], F32))
        pers["be_i"] = es.enter_context(nc.sbuf_tensor("p_be", [128, 2, NSS0], F32))
        for st in stages:
            if st == "mod":
                phase_mod(C)
            elif st == "l0":
                phase_l0(C, pers)
            elif st == "d0":
                phase_dispatch(C, pers, NF, NS0)
            elif st == "m0":
                phase_moe(C, pers, 0, NF, NS0)
            elif st == "c0":
                phase_combine(C, 0, NF, "L1", "L2", False)
            elif st == "l1":
                phase_l1(C, pers)
            elif st == "d1":
                phase_dispatch(C, pers, NO, NS1)
            elif st == "m1":
                phase_moe(C, pers, 1, NO, NS1)
            elif st == "c1":
                phase_combine(C, 1, NO, "L3", "out", True)
    return nc


def _consts():
    c = {}
    c["ident"] = np.eye(128, dtype=np.float32)
    c["widx"] = np.arange(128, dtype=np.float32)[:, None]
    c["utri"] = np.triu(np.ones((128, 128), np.float32), 1)
    c["tokf"] = (np.arange(NF)[None, :] * 128 + np.arange(128)[:, None]).astype(np.float32)
    c["thr"] = (np.arange(NSS0, dtype=np.float32) * 256.0)[None, :]
    fill = np.zeros((128, NS0, 4), np.float32)
    fill[:, :, 0] = OOB
    fill[:, :, 2] = OOB
    c["rifill"] = fill.reshape(128, NS0 * 4)
    j = np.arange(128)[:, None]
    i = np.arange(128)[None, :]
    mp = np.where(j >= i, 0.0, -30000.0).astype(np.float32)
    mn = np.where(j <= i, 0.0, -30000.0).astype(np.float32)
    c["maskP"] = np.tile(mp, (1, 4))
    c["maskN"] = np.tile(mn, (1, 4))
    return c


def _rope_tables(pos):
    quarter = 16
    inv = (10000.0 ** (-np.arange(quarter, dtype=np.float32) / quarter)).astype(np.float32)
    row = (pos // 64).astype(np.float32)
    col = (pos % 64).astype(np.float32)
    ar = row[:, None] * inv[None, :]
    ac = col[:, None] * inv[None, :]
    cr, sr, cc_, sc_ = np.cos(ar), np.sin(ar), np.cos(ac), np.sin(ac)
    Ct = np.concatenate([cr, cr, cc_, cc_], axis=1).astype(np.float32)
    St = np.concatenate([-sr, sr, -sc_, sc_], axis=1).astype(np.float32)
    return Ct, St


def _relayout_experts(inp):
    out = {}
    out["w_gate"] = np.ascontiguousarray(np.asarray(inp["w_gate"], np.float32).reshape(2, NE, 8, 128, 512).transpose(0, 1, 3, 2, 4)).reshape(2 * NE * 128, 4096)
    out["w_up"] = np.ascontiguousarray(np.asarray(inp["w_up"], np.float32).reshape(2, NE, 8, 128, 512).transpose(0, 1, 3, 2, 4)).reshape(2 * NE * 128, 4096)
    out["w_down"] = np.ascontiguousarray(np.asarray(inp["w_down"], np.float32).reshape(2, NE, 4, 128, 1024).transpose(0, 1, 3, 2, 4)).reshape(2 * NE * 128, 4096)
    return out


def _core_inputs(inp, core, consts, consts_w):
    b, hf = core // 2, core % 2
    m = dict(consts)
    x = inp["x"][b]
    if hf == 0:
        pos = np.arange(0, NB * 128)
        xs = x[0:NB * 128]
    else:
        pos = np.arange(8191, 8191 - NB * 128, -1)
        xs = x[::-1][0:NB * 128]
    m["xs"] = np.ascontiguousarray(xs)
    m["ctxs"] = np.ascontiguousarray(inp["ctx"][b])
    cc = np.stack([inp["c"][b], inp["c_ctx"]], axis=-1)
    m["cc"] = np.ascontiguousarray(cc.reshape(8, 128, 2).transpose(1, 0, 2))
    m["w_ada"] = inp["w_ada"]
    m["b_ada"] = inp["b_ada"]
    m["gn1"] = inp["g_norm1"]
    m["gn2"] = inp["g_norm2"]
    m["gfin"] = inp["g_final"][None, :]
    w = inp["w_in_even"][0]
    qperm = np.concatenate([np.arange((h * 4 + g) * 64, (h * 4 + g) * 64 + 64) for g in range(4) for h in range(2)])
    m["w_in_e"] = np.ascontiguousarray(np.concatenate([w[:, qperm], w[:, 512:]], axis=1))
    m["sink"] = inp["attn_sink"][0][None, :]
    m["g_sgu"] = inp["g_sgu"][0][None, :]
    wsp = inp["w_spatial"][0]
    bsp = inp["b_spatial"][0]
    cw = inp["conv_w"][0]
    if hf == 1:
        wsp = wsp[:, ::-1, ::-1]
        bsp = bsp[:, ::-1]
        cw = cw[::-1]
    m["w_spT"] = np.ascontiguousarray(wsp.transpose(2, 0, 1))
    m["b_spc"] = np.ascontiguousarray(bsp.T)
    m["w_out_e"] = inp["w_out_even"][0]
    m["w_in_o"] = inp["w_in_odd"][0]
    m["conv_c"] = np.ascontiguousarray(cw.reshape(3, 8, 128).transpose(2, 0, 1))
    m["w_out_o"] = inp["w_out_odd"][0]
    m["wr"] = np.ascontiguousarray(np.concatenate([inp["w_router_group"], inp["w_router_expert"]], axis=-1))
    m["br"] = np.ascontiguousarray(np.concatenate([inp["b_router_group"], inp["b_router_expert"]], axis=-1))
    m["w_gate"] = consts_w["w_gate"]
    m["w_up"] = consts_w["w_up"]
    m["w_down"] = consts_w["w_down"]
    Ct, St = _rope_tables(pos)
    m["ropeC"] = Ct
    m["ropeS"] = St
    return {k: np.ascontiguousarray(np.asarray(v, dtype=np.float32)) for k, v in m.items()}


def kernel(**inputs):
    inp = {k: np.asarray(v) for k, v in inputs.items()}
    consts = _consts()
    nc = build()
    cw = _relayout_experts(inp)
    in_maps = [_core_inputs(inp, c, consts, cw) for c in range(8)]
    res = run_bass_kernel_spmd(nc, in_maps, core_ids=list(range(8)))
    out = np.empty((4, 8192, D), np.float32)
    for c in range(8):
        b, hf = c // 2, c % 2
        o = res.results[c]["out"]
        if hf == 0:
            out[b, 0:4096] = o
        else:
            out[b, 4096:8192] = o[::-1]
    return out
```

```python
import numpy as np
from contextlib import ExitStack
import concourse.bass as bass
import concourse.mybir as mybir
from concourse.bass_utils import run_bass_kernel_spmd

F32 = mybir.dt.float32
F32R = mybir.dt.float32r
BF16 = mybir.dt.bfloat16
I32 = mybir.dt.int32
AF = mybir.ActivationFunctionType
ALU = mybir.AluOpType
AX = mybir.AxisListType

NB = 34
NF = 33
NO = 32
D = 1024
NE = 32
EPS = 1e-6
NSS0 = NF + NE
NSS1 = NO + NE
NS0 = 2 * NSS0
NS1 = 2 * NSS1
OOB = 1.0e6


_UC = [0]
_JR = {}
_SEMPOOL = {"free": [], "es": None}


def U(n):
    _UC[0] += 1
    return f"{n}_u{_UC[0]}"


class Sched:
    ENG = ("sync", "act", "pool", "pe", "dve")

    def __init__(self, nc, name):
        self.nc = nc
        self.name = name
        self.ops = []
        self.last_w = {}
        self.readers = {}

    def add(self, eng, fn, r=(), w=(), dma=None):
        idx = len(self.ops)
        deps = set()
        for x in r:
            j = self.last_w.get(x)
            if j is not None:
                deps.add(j)
        for x in w:
            j = self.last_w.get(x)
            if j is not None:
                deps.add(j)
            deps.update(self.readers.get(x, ()))
        deps.discard(idx)
        self.ops.append(dict(eng=eng, fn=fn, deps=deps, dma=dma))
        for x in r:
            self.readers.setdefault(x, []).append(idx)
        for x in w:
            self.last_w[x] = idx
            self.readers[x] = []
        return idx

    def emit(self):
        nc = self.nc
        ops = self.ops
        has_dep = [False] * len(ops)
        for o in ops:
            for j in o["deps"]:
                has_dep[j] = True
        semkeys = []
        seen = set()
        for o in ops:
            k = (("dmasw" if o["eng"] == "pool" else "dma"), o["dma"]) if o["dma"] is not None else ("eng", o["eng"])
            o["semkey"] = k
            if k not in seen:
                seen.add(k)
                semkeys.append(k)
        pool = _SEMPOOL
        semobj = {}
        counts = {}
        for k in semkeys:
            fl = pool.setdefault("free_" + k[0], [])
            if fl:
                so, c0 = fl.pop()
            else:
                so, c0 = pool["es"].enter_context(nc.semaphore(U("sem"))), 0
            semobj[k] = so
            counts[k] = c0
        start = dict(counts)
        for i, o in enumerate(ops):
            k = o["semkey"]
            if o["dma"] is not None:
                counts[k] += 16
                o["semval"] = counts[k]
                o["inc"] = 16
            elif has_dep[i]:
                counts[k] += 1
                o["semval"] = counts[k]
                o["inc"] = 1
            else:
                o["semval"] = None
                o["inc"] = 0
        waited = {e: dict(start) for e in self.ENG}
        for o in ops:
            need = {}
            for j in o["deps"]:
                d = ops[j]
                if d["eng"] == "pe" and o["eng"] == "pe" and d["dma"] is None:
                    continue
                k = d["semkey"]
                v = d["semval"]
                if v is None:
                    continue
                if need.get(k, 0) < v:
                    need[k] = v
            wl = []
            for k, v in need.items():
                if waited[o["eng"]].get(k, 0) >= v:
                    continue
                waited[o["eng"]][k] = v
                wl.append((k, v))
            o["waits"] = wl
        final_waits = [(k, counts[k]) for k in semkeys if k[0] in ("dma", "dmasw") and counts[k] > start[k]]
        with ExitStack() as es:
            sems = semobj
            blk = es.enter_context(nc.Block())

            def run(engname):
                def body(eng):
                    for o in ops:
                        if o["eng"] != engname:
                            continue
                        for k, v in o["waits"]:
                            eng.wait_ge(sems[k], v)
                        ins = o["fn"](eng)
                        if o["inc"]:
                            ins.then_inc(sems[o["semkey"]], o["inc"])
                return body

            present = {o["eng"] for o in ops}

            def run_sync(eng):
                run("sync")(eng)
                for k, v in final_waits:
                    eng.wait_ge(sems[k], v)

            blk.sync(run_sync)
            if False:
                blk.sync(run("sync"))
            if "act" in present:
                blk.scalar(run("act"))
            if "pool" in present:
                blk.gpsimd(run("pool"))
            if "pe" in present:
                blk.tensor(run("pe"))
            if "dve" in present:
                blk.vector(run("dve"))
        nc.all_engine_barrier()
        for k in semkeys:
            pool["free_" + k[0]].append((semobj[k], counts[k]))
        return len(ops)


class Ctx:
    def __init__(self, nc, ext_in, ext_out):
        self.nc = nc
        self.ext_in = ext_in
        self.ext_out = ext_out
        self.d = {}

    def dram(self, name, shape, dtype=F32):
        kind = "ExternalInput" if name in self.ext_in else ("ExternalOutput" if name in self.ext_out else "Internal")
        ap = self.nc.dram_tensor(name, list(shape), dtype, kind=kind).ap()
        self.d[name] = ap
        return ap


INPUT_SHAPES = {
    "xs": (NB * 128, D), "ctxs": (256, D), "cc": (128, 8, 2),
    "w_ada": (2, D, 6 * D), "b_ada": (2, 6 * D), "gn1": (2, D), "gn2": (2, D), "gfin": (1, D),
    "w_in_e": (D, 1792), "sink": (1, 8), "g_sgu": (1, 512), "w_spT": (128, 8, 128), "b_spc": (128, 8),
    "w_out_e": (D, D), "w_in_o": (D, 3 * D), "conv_c": (128, 3, 8), "w_out_o": (D, D),
    "wr": (2, D, 36), "br": (2, 36),
    "w_gate": (2 * NE * 128, 4096), "w_up": (2 * NE * 128, 4096), "w_down": (2 * NE * 128, 4096),
    "ropeC": (NB * 128, 64), "ropeS": (NB * 128, 64), "maskP": (128, 512), "maskN": (128, 512),
    "widx": (128, 1), "ident": (128, 128), "utri": (128, 128), "tokf": (128, NF), "thr": (1, NSS0), "rifill": (128, NS0 * 4),
}


def _rms_rstd(s, ss, col, xtile, xres, junk, tag):
    s.add("act", lambda e: e.activation(out=junk[:], in_=xtile, func=AF.Square, accum_out=ss[:, col:col + 1]),
          r=[xres], w=[_JR.get(id(junk), "junk" + tag), ("ss" + tag, col)])
    s.add("act", lambda e: e.activation(out=ss[:, col + 1:col + 2], in_=ss[:, col:col + 1], func=AF.Sqrt,
                                        scale=1.0 / D, bias=EPS),
          r=[("ss" + tag, col)], w=[("rs" + tag, col)])
    s.add("dve", lambda e: e.reciprocal(out=ss[:, col + 1:col + 2], in_=ss[:, col + 1:col + 2]),
          r=[("rs" + tag, col)], w=[("rs" + tag, col)])


def phase_mod(C):
    nc = C.nc
    Dm = C.d
    with ExitStack() as es:
        T = lambda n, sh, dt: es.enter_context(nc.sbuf_tensor(U(n), sh, dt))
        P = lambda n, sh, dt: es.enter_context(nc.psum_tensor(U(n), sh, dt))
        cc = T("m_cc", [128, 8, 2], F32)
        sl = T("m_sl", [128, 8, 2], F32)
        wa = [T(f"m_wa{i}", [128, 8, 512], F32) for i in range(2)]
        bb = [T(f"m_bb{l}", [2, 6 * D], F32) for l in range(2)]
        mr = [T(f"m_mr{l}", [2, 6 * D], F32) for l in range(2)]
        ps = [P(f"m_ps{i}", [2, 512], F32) for i in range(2)]
        s = Sched(nc, "mod")
        s.add("sync", lambda e: e.dma_start(out=cc[:], in_=Dm["cc"]), w=["cc"], dma="cc")
        s.add("act", lambda e: e.activation(out=sl[:], in_=cc[:], func=AF.Silu), r=["cc"], w=["sl"])
        for l in range(2):
            s.add("sync", lambda e, l=l: e.dma_start(out=bb[l][:], in_=Dm["b_ada"][l:l + 1, :].broadcast_to([2, 6 * D])),
                  w=[("bb", l)], dma=("bb", l))
        it = 0
        import os as _os
        for l in range(2):
            for j in range(int(_os.environ.get("MODJ", "12"))):
                par = it % 2
                it += 1
                s.add("sync", lambda e, l=l, j=j, par=par: e.dma_start(
                    out=wa[par][:], in_=Dm["w_ada"][l, :, j * 512:(j + 1) * 512].rearrange("(k p) n -> p k n", p=128)),
                    w=[("wa", par)], dma=("wa", par))
                for k in range(8):
                    s.add("pe", lambda e, k=k, par=par: e.matmul(out=ps[par][:], lhsT=sl[:, k, :], rhs=wa[par][:, k, :],
                                                                  start=(k == 0), stop=(k == 7)),
                          r=["sl", ("wa", par)], w=[("ps", par)])
                s.add("dve", lambda e, l=l, j=j, par=par: e.tensor_tensor(
                    out=mr[l][:, j * 512:(j + 1) * 512], in0=ps[par][:], in1=bb[l][:, j * 512:(j + 1) * 512], op=ALU.add),
                    r=[("ps", par), ("bb", l)], w=[("mr", l)])
            s.add("sync", lambda e, l=l: e.dma_start(out=Dm["modrow"][l], in_=mr[l][:]), r=[("mr", l)], w=[("modrow", l)],
                  dma=("modst", l))
        s.add("sync", lambda e: e.nop(), r=[("modrow", 0), ("modrow", 1)])
        s.emit()


def _load_modrows(s, C, T, l, names, sidx=0):
    out = {}
    for nm, pi in names.items():
        t = T(f"mr_{l}_{sidx}_{nm}", [128, D], F32)
        s.add("sync", lambda e, t=t, pi=pi: e.dma_start(
            out=t[:], in_=C.d["modrow"][l, sidx:sidx + 1, pi * D:(pi + 1) * D].broadcast_to([128, D])),
            w=[("mrow", l, sidx, nm)], dma=("mrow", l, sidx, nm))
        out[nm] = t
    return out


def _make_G(s, T, C, l, which, scrow, res_sc, tag):
    if getattr(s, "gn_tile", None) is None:
        s.gn_tile = T("gn_shared", [128, D], F32)
    g = s.gn_tile
    s.add("sync", lambda e: e.dma_start(out=g[:], in_=C.d[which][l:l + 1, :].broadcast_to([128, D])),
          w=["gnS"], dma="gnS")
    s.add("dve", lambda e: e.scalar_tensor_tensor(out=scrow[:], in0=scrow[:], scalar=1.0, in1=g[:], op0=ALU.add, op1=ALU.mult),
          r=[res_sc, "gnS"], w=[res_sc])


def _load_w_bf16(s, T, name, dst, src_ap, ncols, stg, tag):
    j = 0
    c0 = 0
    while c0 < ncols:
        cw = min(256, ncols - c0)
        par = (j % 2) if stg[0] is not stg[1] else 0
        s.add("sync", lambda e, c0=c0, cw=cw, par=par: e.dma_start(
            out=stg[par][:, :, 0:cw], in_=src_ap[:, c0:c0 + cw].rearrange("(k p) n -> p k n", p=128)),
            w=[("stg", par)], dma=("stg", par))
        s.add("pool", lambda e, c0=c0, cw=cw, par=par: e.tensor_copy(out=dst[:, :, c0:c0 + cw], in_=stg[par][:, :, 0:cw]),
              r=[("stg", par)], w=[(tag, j)])
        c0 += cw
        j += 1
    return [(tag, jj) for jj in range(j)]


def _tail(s, n, lat, latres, tl, l):
    nc = tl["nc"]
    ss, junk = tl["ss2"], tl["junk"]
    _rms_rstd(s, ss, 2 * n, lat[:], latres, junk, "t")
    h2 = tl["h2"][0]
    hres = ("h2", 0)
    s.add("dve", lambda e: e.scalar_tensor_tensor(out=h2[:], in0=lat[:], scalar=ss[:, 2 * n + 1:2 * n + 2], in1=tl["G2"][:],
                                                  op0=ALU.mult, op1=ALU.mult),
          r=[latres, ("rst", 2 * n), "G2"], w=[hres])
    s.add("dve", lambda e: e.tensor_tensor(out=h2[:], in0=h2[:], in1=tl["SH2"][:], op=ALU.add), r=[hres, "SH2"], w=[hres])
    h2b = tl["h2b"]
    s.add("act", lambda e: e.activation(out=h2b[:], in_=h2[:], func=AF.Copy), r=[hres], w=["h2b"])
    s.add("sync", lambda e: e.dma_start(out=tl["H2b"][n * 128:(n + 1) * 128, :], in_=h2b[:]), r=["h2b"], w=[("H2", n)],
          dma=("h2st", 0))
    pT = tl["pT32"]
    for k in range(8):
        s.add("pe", lambda e, k=k: e.transpose(out=pT[k // 4][:, k % 4, :], in_=h2[:, k * 128:(k + 1) * 128], identity=tl["identf"][:]),
              r=[hres, "identf"], w=[("pT32", k // 4)])
    h2T = tl["h2T"]
    s.add("act", lambda e: e.activation(out=h2T[:, 0:4, :], in_=pT[0][:], func=AF.Copy), r=[("pT32", 0)], w=["h2Ta"])
    s.add("act", lambda e: e.activation(out=h2T[:, 4:8, :], in_=pT[1][:], func=AF.Copy), r=[("pT32", 1)], w=["h2Tb"])
    pR = tl["pR"]
    for k in range(8):
        s.add("pe", lambda e, k=k: e.matmul(out=pR[:, 0:36], lhsT=h2T[:, k, :], rhs=tl["wr"][:, k, :], start=(k == 0), stop=(k == 7)),
              r=["h2Ta", "h2Tb", "wr"], w=["pR"])
    lg = tl["lg_all"]
    s.add("dve", lambda e: e.tensor_tensor(out=lg[:, n, :], in0=pR[:, 0:36], in1=tl["brow"][:], op=ALU.add), r=["pR", "brow"], w=[("lg", n)])


def _route_all(s, tl, NT, T):
    L = tl["lg_all"]
    G = L[:, 0:NT, 0:4]
    E4 = L[:, 0:NT, 4:36].rearrange("p n (g j) -> p n g j", g=4)
    OHf, OHb, gates = tl["OHf"], tl["OHb"], tl["gates"]
    gmax = T("r_gmax", [128, NF], F32)
    ohg = T("r_ohg", [128, NF, 4], F32)
    gsh = T("r_gsh", [128, NF, 4], F32)
    sumg = T("r_sumg", [128, NF], F32)
    tmp4 = T("r_tmp4", [128, NF, 4, 8], F32)
    esel = T("r_esel", [128, NF, 8], F32)
    class _V:
        def __init__(self, i):
            self.i = i

        def __getitem__(self, idx):
            return tmp4[:, :, self.i, :][idx]
    esel2, oh1, oh2 = _V(0), _V(1), _V(2)
    m1 = T("r_m1", [128, NF], F32)
    m2 = T("r_m2", [128, NF], F32)
    e21 = T("r_e21", [128, NF], F32)
    w1 = T("r_w1", [128, NF], F32)
    w2 = T("r_w2", [128, NF], F32)
    allg = [("lg", n) for n in range(NT)]
    b3 = lambda t, k: t[:, 0:NT].unsqueeze(2).broadcast_to([128, NT, k])
    s.add("dve", lambda e: e.reduce_max(out=gmax[:, 0:NT], in_=G, axis=AX.X), r=allg, w=["gmax"])
    s.add("dve", lambda e: e.tensor_tensor(out=ohg[:, 0:NT, :], in0=G, in1=b3(gmax, 4), op=ALU.is_ge), r=allg + ["gmax"], w=["ohg"])
    s.add("dve", lambda e: e.tensor_tensor(out=gsh[:, 0:NT, :], in0=G, in1=b3(gmax, 4), op=ALU.subtract), r=allg + ["gmax"], w=["gsh"])
    s.add("act", lambda e: e.activation(out=gsh[:, 0:NT, :], in_=gsh[:, 0:NT, :], func=AF.Exp), r=["gsh"], w=["gsh"])
    s.add("dve", lambda e: e.reduce_sum(out=sumg[:, 0:NT], in_=gsh[:, 0:NT, :], axis=AX.X), r=["gsh"], w=["sumg"])
    s.add("dve", lambda e: e.reciprocal(out=sumg[:, 0:NT], in_=sumg[:, 0:NT]), r=["sumg"], w=["sumg"])
    s.add("dve", lambda e: e.tensor_tensor(out=tmp4[:, 0:NT], in0=E4, in1=ohg[:, 0:NT, :].unsqueeze(3).broadcast_to([128, NT, 4, 8]), op=ALU.mult),
          r=allg + ["ohg"], w=["tmp4"])
    s.add("dve", lambda e: e.reduce_sum(out=esel[:, 0:NT, :], in_=tmp4[:, 0:NT].rearrange("p n g j -> p n j g"), axis=AX.X), r=["tmp4"], w=["esel"])
    s.add("dve", lambda e: e.reduce_max(out=m1[:, 0:NT], in_=esel[:, 0:NT, :], axis=AX.X), r=["esel"], w=["m1"])
    s.add("dve", lambda e: e.tensor_tensor(out=oh1[:, 0:NT, :], in0=esel[:, 0:NT, :], in1=b3(m1, 8), op=ALU.is_ge), r=["esel", "m1"], w=["oh1"])
    s.add("dve", lambda e: e.scalar_tensor_tensor(out=esel2[:, 0:NT, :], in0=oh1[:, 0:NT, :], scalar=-1.0e30, in1=esel[:, 0:NT, :], op0=ALU.mult, op1=ALU.add),
          r=["oh1", "esel"], w=["esel2"])
    s.add("dve", lambda e: e.reduce_max(out=m2[:, 0:NT], in_=esel2[:, 0:NT, :], axis=AX.X), r=["esel2"], w=["m2"])
    s.add("dve", lambda e: e.tensor_tensor(out=oh2[:, 0:NT, :], in0=esel2[:, 0:NT, :], in1=b3(m2, 8), op=ALU.is_ge), r=["esel2", "m2"], w=["oh2"])
    s.add("dve", lambda e: e.tensor_tensor(out=e21[:, 0:NT], in0=m2[:, 0:NT], in1=m1[:, 0:NT], op=ALU.subtract), r=["m1", "m2"], w=["e21"])
    s.add("act", lambda e: e.activation(out=e21[:, 0:NT], in_=e21[:, 0:NT], func=AF.Exp), r=["e21"], w=["e21"])
    s.add("dve", lambda e: e.tensor_scalar(out=w1[:, 0:NT], in0=e21[:, 0:NT], scalar1=1.0, scalar2=None, op0=ALU.add), r=["e21"], w=["w1"])
    s.add("dve", lambda e: e.reciprocal(out=w1[:, 0:NT], in_=w1[:, 0:NT]), r=["w1"], w=["w1"])
    s.add("dve", lambda e: e.tensor_tensor(out=w2[:, 0:NT], in0=e21[:, 0:NT], in1=w1[:, 0:NT], op=ALU.mult), r=["e21", "w1"], w=["w2"])
    s.add("dve", lambda e: e.tensor_tensor(out=gates[:, 0:NT, 0], in0=w1[:, 0:NT], in1=sumg[:, 0:NT], op=ALU.mult), r=["w1", "sumg"], w=["gate0"])
    s.add("dve", lambda e: e.tensor_tensor(out=gates[:, 0:NT, 1], in0=w2[:, 0:NT], in1=sumg[:, 0:NT], op=ALU.mult), r=["w2", "sumg"], w=["gate1"])
    for k, oh, nm in ((0, oh1, "oh1"), (1, oh2, "oh2")):
        s.add("dve", lambda e, k=k, oh=oh: e.tensor_tensor(
            out=OHf[:, 0:NT, k, :].rearrange("p n (g j) -> p n g j", g=4),
            in0=ohg[:, 0:NT, :].unsqueeze(3).broadcast_to([128, NT, 4, 8]), in1=oh[:, 0:NT, :].unsqueeze(2).broadcast_to([128, NT, 4, 8]), op=ALU.mult),
            r=["ohg", nm], w=[("OHf", k)])
    s.add("dve", lambda e: e.tensor_tensor(out=OHb[:, 0:NT, :], in0=OHf[:, 0:NT, 0, :], in1=OHf[:, 0:NT, 1, :], op=ALU.add),
          r=[("OHf", 0), ("OHf", 1)], w=["OHb"])
    s.add("sync", lambda e: e.nop(), r=["OHb", "gate0", "gate1"])


def _tail_alloc(C, T, P, s, l, NT, mrow):
    nc = C.nc
    tl = dict(nc=nc)
    tl["ss2"] = T("t_ss2", [128, 2 * NF], F32)
    tl["h2"] = [T("t_h2_0", [128, D], F32)] * 2
    tl["h2T"] = T("t_h2T", [128, 8, 128], F32)
    tl["pT32"] = [P(f"t_pT32_{i}", [128, 4, 128], F32) for i in range(2)]
    tl["pR"] = P("t_pR", [128, 512], F32)
    tl["lg_all"] = T("t_lg_all", [128, NF, 36], F32)
    tl["sc"] = T("t_sc", [128, 16], F32)
    tl["identf"] = T("t_identf", [128, 128], F32)
    tl["wr"] = T("t_wr", [128, 8, 36], F32)
    tl["brow"] = T("t_brow", [128, 36], F32)
    tl["G2"] = mrow["sc2"]
    tl["SH2"] = mrow["sh2"]
    tl["H2b"] = C.d["H2b"]
    tl["h2b"] = T("t_h2b", [128, D], BF16)
    tl["junk"] = tl["h2b"]
    _JR[id(tl["h2b"])] = "h2b"
    s.add("dve", lambda e: e.memset(tl["ss2"][:], 0.0), w=[("sst", c) for c in range(2 * NF)])
    s.add("sync", lambda e: e.dma_start(out=tl["identf"][:], in_=C.d["ident"]), w=["identf"], dma="identf")
    s.add("sync", lambda e: e.dma_start(out=tl["wr"][:], in_=C.d["wr"][l].rearrange("(k p) n -> p k n", p=128)), w=["wr"], dma="wr")
    s.add("sync", lambda e: e.dma_start(out=tl["brow"][:], in_=C.d["br"][l:l + 1, :].broadcast_to([128, 36])), w=["brow"], dma="brow")
    return tl


def phase_l0(C, pers):
    nc = C.nc
    Dm = C.d
    OHf, OHb, gates = pers["OHf"], pers["OHb"], pers["gates"]
    with ExitStack() as es0:
        T0 = lambda n, sh, dt: es0.enter_context(nc.sbuf_tensor(U(n), sh, dt))
        qT_all = T0("qT_all", [128, NB, 512], BF16)
        kT_all = T0("kT_all", [128, NB * 128], BF16)
        Vaug = T0("Vaug", [128, NB, 2, 65], BF16)
        sg_all = T0("sg_all", [128, NB, 512], BF16)
        kcT = T0("kcT", [128, 256], BF16)
        Vcaug = T0("Vcaug", [128, 2, 2, 65], BF16)
        identb = T0("identb", [128, 128], BF16)
        with ExitStack() as es:
            T = lambda n, sh, dt: es.enter_context(nc.sbuf_tensor(U(n), sh, dt))
            P = lambda n, sh, dt: es.enter_context(nc.psum_tensor(U(n), sh, dt))
            s = Sched(nc, "l0a")
            Gt = T("a_Gt", [128, D], F32)
            St = T("a_St", [128, D], F32)
            gnt = T("a_gnt", [128, D], F32)
            s.add("sync", lambda e: e.dma_start(out=gnt[:], in_=Dm["gn1"][0:1, :].broadcast_to([128, D])), w=["gn"], dma="gn")

            def load_rows(sidx):
                s.add("sync", lambda e: e.dma_start(out=St[:], in_=Dm["modrow"][0, sidx:sidx + 1, 0:D].broadcast_to([128, D])), w=["SHrow"], dma="SHrow")
                s.add("sync", lambda e: e.dma_start(out=Gt[:], in_=Dm["modrow"][0, sidx:sidx + 1, D:2 * D].broadcast_to([128, D])), w=["Grow"], dma="Grow")
                s.add("dve", lambda e: e.scalar_tensor_tensor(out=Gt[:], in0=Gt[:], scalar=1.0, in1=gnt[:], op0=ALU.add, op1=ALU.mult),
                      r=["Grow", "gn"], w=["Grow"])
            stg = [T("a_stg0", [128, 8, 256], F32)] * 2
            w_in = T("a_win", [128, 8, 1792], BF16)
            wres = _load_w_bf16(s, T, "w_in_e", w_in, Dm["w_in_e"], 1792, stg, "win")
            identf = T("a_identf", [128, 128], F32)
            s.add("sync", lambda e: e.dma_start(out=identf[:], in_=Dm["ident"]), w=["identf"], dma="identf")
            s.add("dve", lambda e: e.tensor_copy(out=identb[:], in_=identf[:]), r=["identf"], w=["identb"])
            wspf = T("a_wspf", [128, 8, 128], F32)
            wspb = T("a_wspb", [128, 8, 128], BF16)
            s.add("sync", lambda e: e.dma_start(out=wspf[:], in_=Dm["w_spT"]), w=["wspf"], dma="wspf")
            s.add("dve", lambda e: e.tensor_copy(out=wspb[:], in_=wspf[:]), r=["wspf"], w=["wspb"])
            bsp = T("a_bsp", [128, 8], F32)
            s.add("sync", lambda e: e.dma_start(out=bsp[:], in_=Dm["b_spc"]), w=["bsp"], dma="bsp")
            gsgu = T("a_gsgu", [128, 512], F32)
            s.add("sync", lambda e: e.dma_start(out=gsgu[:], in_=Dm["g_sgu"].broadcast_to([128, 512])), w=["gsgu"], dma="gsgu")
            rCS = [T(f"a_rCS{i}", [128, 2, 64], F32) for i in range(2)]
            xr = [T(f"a_x{i}", [128, D], F32) for i in range(2)]
            junk = T("a_junk", [128, D], BF16)
            ss = T("a_ss", [128, 2 * (NB + 2)], F32)
            hf = T("a_hf", [128, D], F32)
            hb = T("a_hb", [128, D], BF16)
            hT = T("a_hT", [128, 8, 128], BF16)
            qkf2 = [T(f"a_qkf{i}", [128, 640], F32) for i in range(2)]
            qk1 = T("a_qk1", [128, 640], F32)
            qk2 = T("a_qk2", [128, 640], F32)
            qkb = T("a_qkb", [128, 640], BF16)
            gu2 = [T(f"a_gu{i}", [128, 512], F32) for i in range(2)]
            gz2 = [T(f"a_gz{i}", [128, 512], F32) for i in range(2)]
            zb = T("a_zb", [128, 512], BF16)
            st6 = T("a_st6", [128, 6], F32)
            mv = T("a_mv", [128, 4], F32)
            t1 = T("a_t1", [128, 512], F32)
            pT = P("a_pT", [128, 8, 128], BF16)
            psq = P("a_psq", [128, 512], F32)
            pskv = P("a_pskv", [128, 512], F32)
            psu = P("a_psu", [128, 512], F32)
            psz = P("a_psz", [128, 512], F32)
            pssp = P("a_pssp", [128, 512], F32)
            s.add("dve", lambda e: e.memset(ss[:], 0.0), w=[("ssa", c) for c in range(2 * (NB + 2))])
            s.add("dve", lambda e: e.memset(Vaug[:], 1.0), w=[("V", n) for n in range(NB)])
            s.add("dve", lambda e: e.memset(Vcaug[:], 1.0), w=[("Vc", j) for j in range(2)])

            def blockA(n, is_ctx, stage=0):
                par = n % 2
                qkf, gu, gz = qkf2[par], gu2[par], gz2[par]
                if stage == 2:
                    return blockA2(n)
                col = 2 * n
                src = Dm["ctxs"] if is_ctx else Dm["xs"]
                bi = n - NB if is_ctx else n
                s.add("sync", lambda e: e.dma_start(out=xr[par][:], in_=src[bi * 128:(bi + 1) * 128, :]), w=[("x", par)], dma=("x", par))
                _rms_rstd(s, ss, col, xr[par][:], ("x", par), junk, "a")
                gres = "Grow"
                sres = "SHrow"
                s.add("dve", lambda e: e.scalar_tensor_tensor(out=hf[:], in0=xr[par][:], scalar=ss[:, col + 1:col + 2], in1=Gt[:],
                                                              op0=ALU.mult, op1=ALU.mult),
                      r=[("x", par), ("rsa", col), gres], w=["hf"])
                s.add("pool", lambda e: e.tensor_tensor(out=hb[:], in0=hf[:], in1=St[:], op=ALU.add), r=["hf", sres], w=["hb"])
                for k in range(8):
                    s.add("pe", lambda e, k=k: e.transpose(out=pT[:, k, :], in_=hb[:, k * 128:(k + 1) * 128], identity=identb[:]),
                          r=["hb", "identb"], w=["pT"])
                s.add("act", lambda e: e.activation(out=hT[:], in_=pT[:], func=AF.Copy), r=["pT"], w=["hT"])
                groups = [(pskv, "pskv", 512, 256)] if is_ctx else [(psq, "psq", 0, 512), (pskv, "pskv", 512, 256), (psu, "psu", 768, 512), (psz, "psz", 1280, 512)]
                for (pt, pn, c0, cw) in groups:
                    for k in range(8):
                        s.add("pe", lambda e, pt=pt, c0=c0, cw=cw, k=k: e.matmul(out=pt[:, 0:cw], lhsT=hT[:, k, :], rhs=w_in[:, k, c0:c0 + cw],
                                                                                 start=(k == 0), stop=(k == 7)),
                              r=["hT"] + wres, w=[pn])
                if is_ctx:
                    j = bi
                    s.add("act", lambda e: e.activation(out=qkb[:, 0:128], in_=pskv[:, 0:128], func=AF.Copy), r=["pskv"], w=["qkb"])
                    s.add("act", lambda e: e.activation(out=Vcaug[:, j, :, 0:64], in_=pskv[:, 128:256].rearrange("p (h d) -> p h d", h=2), func=AF.Copy),
                          r=["pskv"], w=[("Vc", j)])
                    s.add("pe", lambda e: e.transpose(out=pT[:, 0, :], in_=qkb[:, 0:128], identity=identb[:]), r=["qkb", "identb"], w=["pT"])
                    s.add("dve", lambda e: e.tensor_copy(out=kcT[:, j * 128:(j + 1) * 128], in_=pT[:, 0, :]), r=["pT"], w=[("kcT", j)])
                    return
                s.add("act", lambda e: e.activation(out=qkf[:, 0:512], in_=psq[:], func=AF.Copy), r=["psq"], w=[("qkf_q", par)])
                s.add("act", lambda e: e.activation(out=qkf[:, 512:640], in_=pskv[:, 0:128], func=AF.Copy), r=["pskv"], w=[("qkf_k", par)])
                s.add("act", lambda e: e.activation(out=Vaug[:, n, :, 0:64], in_=pskv[:, 128:256].rearrange("p (h d) -> p h d", h=2), func=AF.Copy),
                      r=["pskv"], w=[("V", n)])
                s.add("act", lambda e: e.activation(out=gu[:], in_=psu[:], func=AF.Gelu), r=["psu"], w=[("gu", par)])
                s.add("act", lambda e: e.activation(out=gz[:], in_=psz[:], func=AF.Gelu), r=["psz"], w=[("gz", par)])

            def blockA2(n):
                par = n % 2
                qkf, gu, gz = qkf2[par], gu2[par], gz2[par]
                rt = rCS[par]
                s.add("pool", lambda e: e.dma_start(out=rt[:, 0, :], in_=Dm["ropeC"][n * 128:(n + 1) * 128, :]), w=[("rC", par)], dma=("rC", par))
                s.add("pool", lambda e: e.dma_start(out=rt[:, 1, :], in_=Dm["ropeS"][n * 128:(n + 1) * 128, :]), w=[("rS", par)], dma=("rS", par))
                v3 = lambda t: t[:].rearrange("p (h d) -> p h d", d=64)
                s.add("dve", lambda e: e.tensor_tensor(out=v3(qk1), in0=v3(qkf), in1=rt[:, 0:1, :].broadcast_to([128, 10, 64]), op=ALU.mult),
                      r=[("qkf_q", par), ("qkf_k", par), ("rC", par)], w=["qk1"])
                v5 = lambda t, pr: t[:].rearrange("p (h a b c) -> p h a b c", a=2, b=2, c=16)[:, :, :, pr, :]
                sv = lambda pr: rt[:, 1:2, :].rearrange("p o (a b c) -> p o a b c", a=2, b=2, c=16)[:, :, :, pr, :].broadcast_to([128, 10, 2, 16])
                for pr in range(2):
                    s.add("pool", lambda e, pr=pr: e.tensor_tensor(out=v5(qk2, pr), in0=v5(qkf, 1 - pr), in1=sv(pr), op=ALU.mult),
                          r=[("qkf_q", par), ("qkf_k", par), ("rS", par)], w=[("qk2", pr)])
                s.add("dve", lambda e: e.tensor_tensor(out=qkb[:], in0=qk1[:], in1=qk2[:], op=ALU.add), r=["qk1", ("qk2", 0), ("qk2", 1)], w=["qkb"])
                for j in range(5):
                    s.add("pe", lambda e, j=j: e.transpose(out=pT[:, j, :], in_=qkb[:, j * 128:(j + 1) * 128], identity=identb[:]),
                          r=["qkb", "identb"], w=["pT"])
                s.add("act", lambda e: e.activation(out=qT_all[:, n, :], in_=pT[:, 0:4, :].rearrange("p a b -> p (a b)"), func=AF.Copy), r=["pT"], w=[("qT", n)])
                s.add("dve", lambda e: e.tensor_copy(out=kT_all[:, n * 128:(n + 1) * 128], in_=pT[:, 4, :]), r=["pT"], w=[("kT", n)])
                s.add("dve", lambda e: e.bn_stats(out=st6[:], in_=gz[:]), r=[("gz", par)], w=["st6"])
                s.add("dve", lambda e: e.bn_aggr(out=mv[:, 0:2], in_=st6[:]), r=["st6"], w=["mv"])
                s.add("act", lambda e: e.activation(out=mv[:, 2:3], in_=mv[:, 1:2], func=AF.Sqrt, scale=1.0, bias=EPS), r=["mv"], w=["mv2"])
                s.add("dve", lambda e: e.reciprocal(out=mv[:, 3:4], in_=mv[:, 2:3]), r=["mv2"], w=["mv3"])
                s.add("dve", lambda e: e.tensor_scalar(out=gz[:], in0=gz[:], scalar1=mv[:, 0:1], scalar2=mv[:, 3:4], op0=ALU.subtract, op1=ALU.mult),
                      r=[("gz", par), "mv", "mv3"], w=[("gz", par)])
                s.add("pool", lambda e: e.tensor_tensor(out=zb[:], in0=gz[:], in1=gsgu[:], op=ALU.mult), r=[("gz", par), "gsgu"], w=["zb"])
                for h in range(8):
                    s.add("pe", lambda e, h=h: e.matmul(out=pssp[:, h * 64:(h + 1) * 64], lhsT=wspb[:, h, :], rhs=zb[:, h * 64:(h + 1) * 64],
                                                        start=True, stop=True), r=["zb", "wspb"], w=["pssp"])
                s.add("dve", lambda e: e.tensor_tensor(out=t1[:].rearrange("p (h c) -> p h c", c=64), in0=pssp[:].rearrange("p (h c) -> p h c", c=64),
                                                       in1=bsp[:].unsqueeze(2).broadcast_to([128, 8, 64]), op=ALU.add),
                      r=["pssp", "bsp"], w=["t1"])
                s.add("pool", lambda e: e.tensor_tensor(out=sg_all[:, n, :], in0=t1[:], in1=gu[:], op=ALU.mult), r=["t1", ("gu", par)], w=[("sg", n)])

            load_rows(1)
            blockA(NB, True)
            blockA(NB + 1, True)
            load_rows(0)
            blockA(0, False, 1)
            for n in range(1, NB):
                blockA(n, False, 1)
                blockA(n - 1, False, 2)
            blockA(NB - 1, False, 2)
            s.emit()
        with ExitStack() as es:
            T = lambda n, sh, dt: es.enter_context(nc.sbuf_tensor(U(n), sh, dt))
            P = lambda n, sh, dt: es.enter_context(nc.psum_tensor(U(n), sh, dt))
            s = Sched(nc, "l0b")
            mrow = _load_modrows(s, C, T, 0, {"g1": 2, "sh2": 3, "sc2": 4}, 0)
            _make_G(s, T, C, 0, "gn2", mrow["sc2"], ("mrow", 0, 0, "sc2"), "b0")
            tl = _tail_alloc(C, T, P, s, 0, NF, mrow)
            tl.update(OHf=OHf, OHb=OHb, gates=gates)
            s.add("dve", lambda e: e.tensor_copy(out=tl["sc"][:, 15:16], in_=tl["sc"][:, 15:16]), r=[("mrow", 0, 0, "sc2")], w=["G2"])
            s.add("pool", lambda e: e.tensor_copy(out=tl["sc"][:, 14:15], in_=tl["sc"][:, 14:15]), r=[("mrow", 0, 0, "sh2")], w=["SH2"])
            stg = [T("b_stg0", [128, 8, 256], F32)] * 2
            w_out = T("b_wout", [128, 8, D], BF16)
            wres = _load_w_bf16(s, T, "w_out_e", w_out, Dm["w_out_e"], D, stg, "wout")
            mkf = T("b_mkf", [128, 2, 512], F32)
            mkb = T("b_mkb", [128, 2, 512], BF16)
            s.add("sync", lambda e: e.dma_start(out=mkf[:, 0, :], in_=Dm["maskP"]), w=["mkf0"], dma="mkf0")
            s.add("sync", lambda e: e.dma_start(out=mkf[:, 1, :], in_=Dm["maskN"]), w=["mkf1"], dma="mkf1")
            s.add("dve", lambda e: e.tensor_copy(out=mkb[:], in_=mkf[:]), r=["mkf0", "mkf1"], w=["mkb"])
            snk = T("b_snk", [128, 8], F32)
            exps = T("b_exps", [128, 8], F32)
            s.add("sync", lambda e: e.dma_start(out=snk[:], in_=Dm["sink"].broadcast_to([128, 8])), w=["snk"], dma="snk")
            s.add("act", lambda e: e.activation(out=exps[:], in_=snk[:], func=AF.Exp), r=["snk"], w=["exps"])
            PT = [T(f"b_PT{i}", [128, 512], BF16) for i in range(5)]
            mix2 = [T(f"b_mix{i}", [128, 512], BF16) for i in range(2)]
            mixT = T("b_mixT", [128, 8, 128], BF16)
            dd2 = [T(f"b_dd{i}", [128, 8], F32) for i in range(2)]
            xr = [T(f"b_x{i}", [128, D], F32) for i in range(2)]
            tt = T("b_tt", [128, D], F32)
            lat = [T(f"b_lat{i}", [128, D], F32) for i in range(2)]
            pS = [P(f"b_pS{i}", [128, 512], F32) for i in range(2)]
            pO = [P(f"b_pO{i}", [128, 512], F32) for i in range(2)]
            pT = P("b_pT", [128, 8, 128], BF16)
            pW = [tl["pT32"][0], tl["pT32"][1]]
            pWv = [p[:].rearrange("p a b -> p (a b)") for p in pW]

            def ATT(n):
                par = n % 2
                mix = mix2[par]
                dd = dd2[par]
                for h in range(2):
                    tiles = [("c", 0), ("c", 1)] + ([("p", n - 1)] if n > 0 else []) + [("l", n), ("n", n + 1)]
                    for i, (kind, j) in enumerate(tiles):
                        if kind == "c":
                            kt = kcT[h * 64:(h + 1) * 64, j * 128:(j + 1) * 128]
                            kres = ("kcT", j)
                        else:
                            kt = kT_all[h * 64:(h + 1) * 64, j * 128:(j + 1) * 128]
                            kres = ("kT", j)
                        masked = kind in ("p", "n")
                        ps_ = pS[i % 2]
                        s.add("pe", lambda e, kt=kt, ps_=ps_, masked=masked, n=n, h=h: e.matmul(
                            out=ps_[:], lhsT=kt, rhs=qT_all[h * 64:(h + 1) * 64, n, :], start=True, stop=not masked),
                            r=[kres, ("qT", n)], w=[("pS", i % 2)])
                        if masked:
                            mi = 0 if kind == "p" else 1
                            s.add("pe", lambda e, ps_=ps_, mi=mi: e.matmul(out=ps_[:], lhsT=identb[:], rhs=mkb[:, mi, :], start=False, stop=True),
                                  r=["mkb", "identb"], w=[("pS", i % 2)])
                        s.add("act", lambda e, ps_=ps_, i=i: e.activation(out=PT[i][:], in_=ps_[:], func=AF.Exp, scale=0.125),
                              r=[("pS", i % 2)], w=[("PT", i)])
                    nt = len(tiles)
                    for g in range(4):
                        for i, (kind, j) in enumerate(tiles):
                            if kind == "c":
                                vt = Vcaug[:, j, h, :]
                                vres = ("Vc", j)
                            else:
                                vt = Vaug[:, j, h, :]
                                vres = ("V", j)
                            s.add("pe", lambda e, g=g, i=i, vt=vt, h=h, nt=nt: e.matmul(
                                out=pO[h][:, g * 65:(g + 1) * 65], lhsT=PT[i][:, g * 128:(g + 1) * 128], rhs=vt,
                                start=(i == 0), stop=(i == nt - 1)), r=[("PT", i), vres], w=[("pO", h)])
                    pov = pO[h][:, 0:260].rearrange("p (g d) -> p g d", d=65)
                    s.add("dve", lambda e, pov=pov, h=h: e.tensor_tensor(out=dd[:, h * 4:(h + 1) * 4], in0=pov[:, :, 64], in1=exps[:, h * 4:(h + 1) * 4], op=ALU.add),
                          r=[("pO", h), "exps"], w=[("dd", par, h)])
                    s.add("dve", lambda e, h=h: e.reciprocal(out=dd[:, h * 4:(h + 1) * 4], in_=dd[:, h * 4:(h + 1) * 4]), r=[("dd", par, h)], w=[("dd", par, h)])
                    s.add("dve", lambda e, pov=pov, h=h: e.tensor_tensor(
                        out=mix[:, h * 256:(h + 1) * 256].rearrange("p (g d) -> p g d", d=64), in0=pov[:, :, 0:64],
                        in1=dd[:, h * 4:(h + 1) * 4].unsqueeze(2).broadcast_to([128, 4, 64]), op=ALU.mult),
                        r=[("pO", h), ("dd", par, h)], w=[("mix", par, h)])

            def OUT(n):
                par = n % 2
                mix = mix2[par]
                dd = dd2[par]
                s.add("sync", lambda e, n=n, par=par: e.dma_start(out=xr[par][:], in_=Dm["xs"][n * 128:(n + 1) * 128, :]), w=[("x", par)], dma=("x", par))
                for k in range(8):
                    if k < 4:
                        src = mix[:, k * 128:(k + 1) * 128]
                        rr = [("mix", par, 0), ("mix", par, 1)]
                    else:
                        src = sg_all[:, n, (k - 4) * 128:(k - 3) * 128]
                        rr = [("sg", n)]
                    s.add("pe", lambda e, k=k, src=src: e.transpose(out=pT[:, k, :], in_=src, identity=identb[:]), r=rr + ["identb"], w=["pT"])
                s.add("act", lambda e: e.activation(out=mixT[:], in_=pT[:], func=AF.Copy), r=["pT"], w=["mixT"])
                for jn in range(2):
                    for k in range(8):
                        s.add("pe", lambda e, jn=jn, k=k: e.matmul(out=pWv[jn], lhsT=mixT[:, k, :], rhs=w_out[:, k, jn * 512:(jn + 1) * 512],
                                                                   start=(k == 0), stop=(k == 7)),
                              r=["mixT"] + wres, w=[("pT32", jn)])
                for jn in range(2):
                    s.add("dve", lambda e, jn=jn: e.tensor_tensor(out=tt[:, jn * 512:(jn + 1) * 512], in0=pWv[jn], in1=mrow["g1"][:, jn * 512:(jn + 1) * 512], op=ALU.mult),
                          r=[("pT32", jn), ("mrow", 0, 0, "g1")], w=[("tt", jn)])
                lt = lat[par]
                s.add("dve", lambda e, lt=lt, par=par: e.tensor_tensor(out=lt[:], in0=tt[:], in1=xr[par][:], op=ALU.add),
                      r=[("tt", 0), ("tt", 1), ("x", par)], w=[("lat", par)])
                s.add("sync", lambda e, lt=lt, n=n: e.dma_start(out=Dm["L1"][n * 128:(n + 1) * 128, :], in_=lt[:]), r=[("lat", par)], w=[("L1", n)],
                      dma=("latst", par))

            def TAIL(n):
                _tail(s, n, lat[n % 2], ("lat", n % 2), tl, 0)

            ATT(0)
            for n in range(1, NF):
                ATT(n)
                OUT(n - 1)
                if n >= 2:
                    TAIL(n - 2)
            OUT(NF - 1)
            TAIL(NF - 2)
            TAIL(NF - 1)
            _route_all(s, tl, NF, T)
            s.add("sync", lambda e: e.nop(), r=[("L1", n) for n in range(NF)] + [("H2", n) for n in range(NF)])
            s.emit()


def phase_dispatch(C, pers, NT, NS):
    nc = C.nc
    Dm = C.d
    OHf, OHb, gates = pers["OHf"], pers["OHb"], pers["gates"]
    be_i = pers["be_i"]
    NSS = NS // 2
    with ExitStack() as es:
        T = lambda n, sh, dt: es.enter_context(nc.sbuf_tensor(U(n), sh, dt))
        P = lambda n, sh, dt: es.enter_context(nc.psum_tensor(U(n), sh, dt))
        s = Sched(nc, "disp")
        utf = T("d_utf", [128, 128], F32)
        utb = T("d_utb", [128, 128], BF16)
        oneb = T("d_oneb", [128, 128], BF16)
        OHbb = T("d_OHbb", [128, NF, 32], BF16)
        rank = T("d_rank", [128, NF, 32], F32)
        pr_ = [P(f"d_pr{i}", [128, 32], F32) for i in range(2)]
        pc = P("d_pc", [128, 32], F32)
        cnt = T("d_cnt", [128, 32], F32)
        rr = T("d_rr", [128, 32], F32)
        gt = T("d_gt", [128, 32], F32)
        pad = T("d_pad", [128, 32], F32)
        cs = [T(f"d_cs{i}", [128, 32], F32) for i in range(2)]
        pst = T("d_pst", [128, 32], F32)
        thr = T("d_thr", [128, NSS0], F32)
        cmp_ = T("d_cmp", [128, NSS0, 32], F32)
        bef = T("d_bef", [128, NSS0], F32)
        tokf = T("d_tokf", [128, NF], F32)
        tmp = T("d_tmp", [128, NF, 32], F32)
        prod = T("d_prod", [128, NF, 2, 32], F32)
        destf = T("d_destf", [128, NF, 2], F32)
        desti = T("d_desti", [128, NF, 2], I32)
        ris = T("d_ris", [128, NF, 2, 4], F32)
        fill = T("d_fill", [128, NS0 * 4], F32)
        s.add("sync", lambda e: e.dma_start(out=utf[:], in_=Dm["utri"]), w=["utf"], dma="utf")
        s.add("sync", lambda e: e.dma_start(out=thr[:], in_=Dm["thr"].broadcast_to([128, NSS0])), w=["thr"], dma="thr")
        s.add("sync", lambda e: e.dma_start(out=tokf[:], in_=Dm["tokf"]), w=["tokf"], dma="tokf")
        s.add("sync", lambda e: e.dma_start(out=fill[:], in_=Dm["rifill"]), w=["fill"], dma="fill")
        s.add("sync", lambda e: e.dma_start(out=Dm["RI"].rearrange("(p b) c -> p (b c)", p=128), in_=fill[:]), r=["fill"], w=["RIfill"], dma="RIfill")
        s.add("dve", lambda e: e.tensor_copy(out=utb[:], in_=utf[:]), r=["utf"], w=["utb"])
        s.add("dve", lambda e: e.memset(oneb[:], 1.0), w=["oneb"])
        s.add("dve", lambda e: e.tensor_copy(out=OHbb[:, 0:NT, :], in_=OHb[:, 0:NT, :]), w=["OHbb"])
        for n in range(NT):
            p = pr_[n % 2]
            s.add("pe", lambda e, n=n, p=p: e.matmul(out=p[:], lhsT=utb[:], rhs=OHbb[:, n, :], start=True, stop=(n == 0)), r=["utb", "OHbb"], w=[("pr", n % 2)])
            for m in range(n):
                s.add("pe", lambda e, m=m, n=n, p=p: e.matmul(out=p[:], lhsT=oneb[:], rhs=OHbb[:, m, :], start=False, stop=(m == n - 1)),
                      r=["oneb", "OHbb"], w=[("pr", n % 2)])
            s.add("dve", lambda e, n=n, p=p: e.tensor_copy(out=rank[:, n, :], in_=p[:]), r=[("pr", n % 2)], w=[("rank", n)])
        for n in range(NT):
            s.add("pe", lambda e, n=n: e.matmul(out=pc[:], lhsT=oneb[:], rhs=OHbb[:, n, :], start=(n == 0), stop=(n == NT - 1)), r=["oneb", "OHbb"], w=["pc"])
        s.add("dve", lambda e: e.tensor_copy(out=cnt[:], in_=pc[:]), r=["pc"], w=["cnt"])
        cmp2 = T("d_cmp2", [128, 32, 17], F32)
        s.add("dve", lambda e: e.tensor_tensor(out=cmp2[:], in0=cnt[:].unsqueeze(2).broadcast_to([128, 32, 17]),
                                               in1=thr[:, 0:17].unsqueeze(1).broadcast_to([128, 32, 17]), op=ALU.is_gt), r=["cnt", "thr"], w=["cmp2"])
        s.add("dve", lambda e: e.reduce_sum(out=pad[:], in_=cmp2[:], axis=AX.X), r=["cmp2"], w=["pad"])
        s.add("dve", lambda e: e.tensor_scalar(out=pad[:], in0=pad[:], scalar1=256.0, scalar2=None, op0=ALU.mult), r=["pad"], w=["pad"])
        s.add("dve", lambda e: e.tensor_copy(out=cs[0][:], in_=pad[:]), r=["pad"], w=[("cs", 0)])
        cur = 0
        for sh in (1, 2, 4, 8, 16):
            nx = 1 - cur
            s.add("dve", lambda e, cur=cur, nx=nx, sh=sh: e.tensor_copy(out=cs[nx][:, 0:sh], in_=cs[cur][:, 0:sh]), r=[("cs", cur)], w=[("csa", nx)])
            s.add("dve", lambda e, cur=cur, nx=nx, sh=sh: e.tensor_tensor(out=cs[nx][:, sh:32], in0=cs[cur][:, sh:32], in1=cs[cur][:, 0:32 - sh], op=ALU.add),
                  r=[("cs", cur), ("csa", nx)], w=[("cs", nx)])
            cur = nx
        pend = cs[cur]
        pres = ("cs", cur)
        s.add("dve", lambda e: e.tensor_tensor(out=pst[:], in0=pend[:], in1=pad[:], op=ALU.subtract), r=[pres, "pad"], w=["pst"])
        s.add("dve", lambda e: e.tensor_tensor(out=cmp_[:, 0:NSS, :], in0=pend[:].unsqueeze(1).broadcast_to([128, NSS, 32]),
                                               in1=thr[:, 0:NSS].unsqueeze(2).broadcast_to([128, NSS, 32]), op=ALU.is_le), r=[pres, "thr"], w=["cmp"])
        s.add("dve", lambda e: e.reduce_sum(out=bef[:, 0:NSS], in_=cmp_[:, 0:NSS, :], axis=AX.X), r=["cmp"], w=["bef"])
        s.add("dve", lambda e: e.tensor_scalar(out=bef[:, 0:NSS], in0=bef[:, 0:NSS], scalar1=31.0, scalar2=None, op0=ALU.min), r=["bef"], w=["bef"])
        s.add("dve", lambda e: e.tensor_scalar(out=be_i[:, 1, 0:NSS], in0=thr[:, 0:NSS], scalar1=pend[:, 31:32], scalar2=OOB, op0=ALU.is_ge, op1=ALU.mult), r=[pres, "thr"], w=["be_i2"])
        s.add("dve", lambda e: e.scalar_tensor_tensor(out=be_i[:, 0, 0:NSS], in0=bef[:, 0:NSS], scalar=128.0, in1=be_i[:, 1, 0:NSS], op0=ALU.mult, op1=ALU.add), r=["bef", "be_i2"], w=["be_i"])
        rb = es.enter_context(nc.gpsimd.register(U("rb_d")))
        s.add("pool", lambda e: e.reg_mov(rb, NS0 * 128 - 1))
        s.add("dve", lambda e: e.tensor_tensor(out=tmp[:, 0:NT, :], in0=rank[:, 0:NT, :], in1=pst[:].unsqueeze(1).broadcast_to([128, NT, 32]), op=ALU.add),
              r=[("rank", n) for n in range(NT)] + ["pst"], w=["tmp"])
        s.add("dve", lambda e: e.tensor_tensor(out=prod[:, 0:NT], in0=OHf[:, 0:NT], in1=tmp[:, 0:NT, :].unsqueeze(2).broadcast_to([128, NT, 2, 32]), op=ALU.mult),
              r=["tmp"], w=["prod"])
        s.add("dve", lambda e: e.reduce_sum(out=destf[:, 0:NT, :], in_=prod[:, 0:NT], axis=AX.X), r=["prod"], w=["destf"])
        s.add("dve", lambda e: e.tensor_copy(out=desti[:, 0:NT, :], in_=destf[:, 0:NT, :]), r=["destf"], w=["desti"])
        s.add("dve", lambda e: e.memset(ris[:], 0.0), w=["ris"])
        for k in range(2):
            s.add("dve", lambda e, k=k: e.tensor_copy(out=ris[:, 0:NT, k, 0], in_=tokf[:, 0:NT]), r=["tokf", "ris"], w=[("ris0", k)])
            s.add("dve", lambda e, k=k: e.tensor_copy(out=ris[:, 0:NT, k, 1], in_=gates[:, 0:NT, k]), r=["ris"], w=[("ris1", k)])
            s.add("dve", lambda e, k=k: e.tensor_scalar(out=ris[:, 0:NT, k, 2], in0=tokf[:, 0:NT], scalar1=2.0, scalar2=float(k), op0=ALU.mult, op1=ALU.add),
                  r=["tokf", "ris"], w=[("ris2", k)])
        rres = [("ris0", 0), ("ris0", 1), ("ris1", 0), ("ris1", 1), ("ris2", 0), ("ris2", 1)]
        allsc = []
        import os as _os
        _dcut = int(_os.environ.get("DCUT", "0"))
        _thr = int(_os.environ.get("DTHR", "8"))
        for n in range(NT if _dcut == 0 else (_dcut - 1)):
            for k in range(2):
                s.add("pool", lambda e, n=n, k=k: e.indirect_dma_start(
                    out=Dm["RI"][:, :], out_offset=bass.IndirectOffsetOnAxis(ap=desti[:, n, k:k + 1], axis=0),
                    in_=ris[:, n, k, :], in_offset=None, bounds_check=rb, oob_is_err=False),
                    r=rres + ["desti", "RIfill"] + (allsc[-_thr:-_thr + 1] if len(allsc) >= _thr else []), w=[("RIsc", n, k)], dma="RIsc")
                allsc.append(("RIsc", n, k))
        s.add("sync", lambda e: e.nop(), r=allsc + ["be_i", "be_i2"])
        s.emit()


def phase_moe(C, pers, l, NT, NS):
    nc = C.nc
    Dm = C.d
    be_i = pers["be_i"]
    import os as _os
    NSx = min(NS, int(_os.environ.get("MCUT", "1000")))
    with ExitStack() as es:
        T = lambda n, sh, dt: es.enter_context(nc.sbuf_tensor(U(n), sh, dt))
        P = lambda n, sh, dt: es.enter_context(nc.psum_tensor(U(n), sh, dt))
        s = Sched(nc, f"moe{l}")
        identf = T("e_identf", [128, 128], F32)
        identb = T("e_identb", [128, 128], BF16)
        s.add("sync", lambda e: e.dma_start(out=identf[:], in_=Dm["ident"]), w=["identf"], dma="identf")
        s.add("dve", lambda e: e.tensor_copy(out=identb[:], in_=identf[:]), r=["identf"], w=["identb"])
        sg_ = [T(f"e_sgf{i}", [128, 8, 512], F32) for i in range(2)]
        su_ = [T(f"e_suf{i}", [128, 8, 512], F32) for i in range(2)]
        sd_ = [T(f"e_sdf{i}", [128, 4, D], F32) for i in range(2)]
        wg = [T(f"e_wg{i}", [128, 8, 512], BF16) for i in range(2)]
        wu = [T(f"e_wu{i}", [128, 8, 512], BF16) for i in range(2)]
        wd = [T(f"e_wd{i}", [128, 4, D], BF16) for i in range(2)]
        ri = [T(f"e_ri{i}", [128, 4], F32) for i in range(4)]
        ii = [T(f"e_ii{i}", [128, 4], I32) for i in range(4)]
        wi = [T(f"e_wi{i}", [128, 1], I32) for i in range(3)]
        xg = [T(f"e_xg{i}", [128, D], BF16) for i in range(3)]
        xT = [T(f"e_xT{i}", [128, 8, 128], BF16) for i in range(2)]
        sgt = T("e_sg", [128, 512], F32)
        hid = T("e_hid", [128, 512], BF16)
        hidT = T("e_hidT", [128, 4, 128], BF16)
        y = [T(f"e_y{i}", [128, D], F32) for i in range(2)]
        pTx = P("e_pTx", [128, 8, 128], BF16)
        pTc = P("e_pTc", [128, 4, 128], BF16)
        pG = P("e_pG", [128, 512], F32)
        pU = P("e_pU", [128, 512], F32)
        pY = [P(f"e_pY{i}", [128, 512], F32) for i in range(2)]
        for i in range(3):
            s.add("dve", lambda e, i=i: e.memset(xg[i][:], 0.0), w=[("xg", i)])
        cst = T("e_cst", [128, 1], F32)
        s.add("sync", lambda e: e.dma_start(out=cst[:], in_=Dm["widx"]), w=["cst"], dma="cst")
        rbx = es.enter_context(nc.gpsimd.register(U("rbx")))
        rby = es.enter_context(nc.gpsimd.register(U("rby")))
        rbw1 = es.enter_context(nc.gpsimd.register(U("rbw1")))
        s.add("pool", lambda e: e.reg_mov(rbx, NT * 128 - 1))
        s.add("pool", lambda e: e.reg_mov(rby, 2 * NT * 128 - 1))
        s.add("pool", lambda e: e.reg_mov(rbw1, 2 * NE * 128 - 1))

        def wgather(st, src, nm, u):
            par = u % 2
            s.add("pool", lambda e: e.indirect_dma_start(
                out=st[par][:].rearrange("p a b -> p (a b)"), out_offset=None, in_=Dm[src],
                in_offset=bass.IndirectOffsetOnAxis(ap=wi[u % 3][:, 0:1], axis=0), bounds_check=rbw1, oob_is_err=False),
                r=[("wi", u % 3)], w=[("st" + nm, par)], dma=("st" + nm, par))

        def load_w(u):
            q = u % 3
            s.add("dve", lambda e: e.tensor_scalar(out=wi[q][:], in0=cst[:], scalar1=be_i[:, 0, u:u + 1], scalar2=float(l * NE * 128), op0=ALU.add, op1=ALU.add),
                  r=["cst"], w=[("wi", q)])
            wgather(sg_, "w_gate", "wg", u)
            wgather(su_, "w_up", "wu", u)
            wgather(sd_, "w_down", "wd", u)

        def conv_gu(u):
            par = u % 2
            s.add("act", lambda e: e.activation(out=wg[par][:], in_=sg_[par][:], func=AF.Copy), r=[("stwg", par)], w=[("wg", par)])
            s.add("dve", lambda e: e.tensor_copy(out=wu[par][:], in_=su_[par][:]), r=[("stwu", par)], w=[("wu", par)])

        def conv_d(u):
            par = u % 2
            s.add("act", lambda e: e.activation(out=wd[par][:, 0:2, :], in_=sd_[par][:, 0:2, :], func=AF.Copy), r=[("stwd", par)], w=[("wd", par)])
            s.add("dve", lambda e: e.tensor_copy(out=wd[par][:, 2:4, :], in_=sd_[par][:, 2:4, :]), r=[("stwd", par)], w=[("wd2", par)])

        def load_gu(b):
            par = b % 3
            q = b % 4
            s.add("sync", lambda e: e.dma_start(out=ri[q][:], in_=Dm["RI"][b * 128:(b + 1) * 128, :]), w=[("ri", q)], dma=("ri", q))
            s.add("pool", lambda e: e.tensor_copy(out=ii[q][:], in_=ri[q][:]), r=[("ri", q)], w=[("ii", q)])
            s.add("pool", lambda e: e.indirect_dma_start(
                out=xg[par][:, :], out_offset=None, in_=Dm["H2b"][:, :],
                in_offset=bass.IndirectOffsetOnAxis(ap=ii[q][:, 0:1], axis=0), bounds_check=rbx, oob_is_err=False),
                r=[("ii", q)], w=[("xg", par)], dma=("xg", par))

        def stA(b):
            par = b % 2
            xp = b % 3
            for k in range(8):
                s.add("pe", lambda e, k=k: e.transpose(out=pTx[:, k, :], in_=xg[xp][:, k * 128:(k + 1) * 128], identity=identb[:]),
                      r=[("xg", xp), "identb"], w=["pTx"])
            s.add("act", lambda e: e.activation(out=xT[par][:], in_=pTx[:], func=AF.Copy), r=["pTx"], w=[("xT", par)])

        def stB(b):
            par = b % 2
            wp = (b // 2) % 2
            for k in range(8):
                s.add("pe", lambda e, k=k: e.matmul(out=pG[:], lhsT=xT[par][:, k, :], rhs=wg[wp][:, k, :], start=(k == 0), stop=(k == 7)),
                      r=[("xT", par), ("wg", wp)], w=["pG"])
            for k in range(8):
                s.add("pe", lambda e, k=k: e.matmul(out=pU[:], lhsT=xT[par][:, k, :], rhs=wu[wp][:, k, :], start=(k == 0), stop=(k == 7)),
                      r=[("xT", par), ("wu", wp)], w=["pU"])
            s.add("act", lambda e: e.activation(out=sgt[:], in_=pG[:], func=AF.Silu), r=["pG"], w=["sgt"])
            s.add("dve", lambda e: e.tensor_tensor(out=hid[:], in0=pU[:], in1=sgt[:], op=ALU.mult), r=["pU", "sgt"], w=["hid"])

        def stT(b):
            for k in range(4):
                s.add("pe", lambda e, k=k: e.transpose(out=pTc[:, k, :], in_=hid[:, k * 128:(k + 1) * 128], identity=identb[:]),
                      r=["hid", "identb"], w=["pTc"])
            s.add("act", lambda e: e.activation(out=hidT[:], in_=pTc[:], func=AF.Copy), r=["pTc"], w=["hidT"])

        def stD(b):
            par = b % 2
            q = b % 4
            wp = (b // 2) % 2
            for jn in range(2):
                for k in range(4):
                    s.add("pe", lambda e, jn=jn, k=k: e.matmul(out=pY[jn][:], lhsT=hidT[:, k, :], rhs=wd[wp][:, k, jn * 512:(jn + 1) * 512],
                                                               start=(k == 0), stop=(k == 3)),
                          r=["hidT", ("wd", wp), ("wd2", wp)], w=[("pY", jn)])
            s.add("act", lambda e: e.activation(out=y[par][:, 0:512], in_=pY[0][:], func=AF.Copy, scale=ri[q][:, 1:2]),
                  r=[("pY", 0), ("ri", q)], w=[("y", par, 0)])
            s.add("dve", lambda e: e.tensor_scalar(out=y[par][:, 512:1024], in0=pY[1][:], scalar1=ri[q][:, 1:2], scalar2=None, op0=ALU.mult),
                  r=[("pY", 1), ("ri", q)], w=[("y", par, 1)])
            s.add("pool", lambda e: e.indirect_dma_start(
                out=Dm["Y"][:, :], out_offset=bass.IndirectOffsetOnAxis(ap=ii[q][:, 2:3], axis=0),
                in_=y[par][:, :], in_offset=None, bounds_check=rby, oob_is_err=False),
                r=[("y", par, 0), ("y", par, 1), ("ii", q)], w=[("Ysc", b)], dma=("ysc", par))

        NSSx = (NSx + 1) // 2
        load_w(0)
        load_gu(0)
        if NSx > 1:
            load_gu(1)
        if NSx > 2:
            load_gu(2)
        if NSSx > 1:
            load_w(1)
        conv_gu(0)
        conv_d(0)
        stA(0)
        stB(0)
        for b in range(NSx):
            if b + 3 < NSx:
                load_gu(b + 3)
            u = b // 2
            if b % 2 == 0:
                if u + 2 < NSSx:
                    load_w(u + 2)
            if b + 1 < NSx:
                stA(b + 1)
            stT(b)
            if b % 2 == 0 and u + 1 < NSSx:
                conv_gu(u + 1)
            if b % 2 == 1 and u + 1 < NSSx:
                conv_d(u + 1)
            if b + 1 < NSx:
                stB(b + 1)
            stD(b)
        s.add("sync", lambda e: e.nop(), r=[("Ysc", b) for b in range(NSx)])
        s.emit()


def phase_combine(C, l, NT, src, dst, final):
    nc = C.nc
    Dm = C.d
    with ExitStack() as es:
        T = lambda n, sh, dt: es.enter_context(nc.sbuf_tensor(U(n), sh, dt))
        s = Sched(nc, f"cmb{l}")
        mrow = _load_modrows(s, C, T, l, {"g2": 5}, 0)
        yt = [T(f"c_y{i}", [128, 2, D], F32) for i in range(2)]
        lt = [T(f"c_l{i}", [128, D], F32) for i in range(3)]
        ot = [T(f"c_o{i}", [128, D], F32) for i in range(2)]
        junk = T("c_junk", [128, D], BF16)
        ss = T("c_ss", [128, 2 * NF], F32)
        if final:
            gf = T("c_gf", [128, D], F32)
            s.add("sync", lambda e: e.dma_start(out=gf[:], in_=Dm["gfin"].broadcast_to([128, D])), w=["gf"], dma="gf")
            s.add("dve", lambda e: e.memset(ss[:], 0.0), w=[("ssc", c) for c in range(2 * NF)])
        outs = []

        def stage1(n):
            par = n % 2
            lp = n % 3
            s.add("sync", lambda e: e.dma_start(out=yt[par][:], in_=Dm["Y"][n * 256:(n + 1) * 256, :].rearrange("(p k) d -> p k d", k=2)),
                  w=[("yt", par)], dma=("yt", par))
            s.add("sync", lambda e: e.dma_start(out=lt[lp][:], in_=Dm[src][n * 128:(n + 1) * 128, :]), w=[("lt", lp)], dma=("lt", lp))
            s.add("pool", lambda e: e.tensor_tensor(out=yt[par][:, 0, :], in0=yt[par][:, 0, :], in1=yt[par][:, 1, :], op=ALU.add),
                  r=[("yt", par)], w=[("yt", par)])
            s.add("dve", lambda e: e.tensor_tensor(out=yt[par][:, 0, :], in0=yt[par][:, 0, :], in1=mrow["g2"][:], op=ALU.mult),
                  r=[("yt", par), ("mrow", l, 0, "g2")], w=[("yt", par)])
            s.add("dve", lambda e: e.tensor_tensor(out=lt[lp][:], in0=lt[lp][:], in1=yt[par][:, 0, :], op=ALU.add),
                  r=[("yt", par), ("lt", lp)], w=[("lt", lp)])

        def stage2(n):
            par = n % 2
            lp = n % 3
            if not final:
                s.add("sync", lambda e: e.dma_start(out=Dm[dst][n * 128:(n + 1) * 128, :], in_=lt[lp][:]), r=[("lt", lp)], w=[(dst, n)],
                      dma=("cst", lp))
            else:
                _rms_rstd(s, ss, 2 * n, lt[lp][:], ("lt", lp), junk, "c")
                s.add("dve", lambda e: e.scalar_tensor_tensor(out=ot[par][:], in0=lt[lp][:], scalar=ss[:, 2 * n + 1:2 * n + 2], in1=gf[:],
                                                              op0=ALU.mult, op1=ALU.mult),
                      r=[("lt", lp), ("rsc", 2 * n), "gf"], w=[("ot", par)])
                s.add("sync", lambda e: e.dma_start(out=Dm[dst][n * 128:(n + 1) * 128, :], in_=ot[par][:]), r=[("ot", par)], w=[(dst, n)],
                      dma=("cst", par))
            outs.append((dst, n))

        stage1(0)
        for n in range(NT):
            if n + 1 < NT:
                stage1(n + 1)
            stage2(n)
        s.add("sync", lambda e: e.nop(), r=outs)
        s.emit()


def phase_l1(C, pers):
    nc = C.nc
    Dm = C.d
    OHf, OHb, gates = pers["OHf"], pers["OHb"], pers["gates"]
    NW = 9
    with ExitStack() as es:
        T = lambda n, sh, dt: es.enter_context(nc.sbuf_tensor(U(n), sh, dt))
        P = lambda n, sh, dt: es.enter_context(nc.psum_tensor(U(n), sh, dt))
        s = Sched(nc, "l1")
        tt = T("f_tt", [128, D], F32)
        s.gn_tile = tt
        mrow = _load_modrows(s, C, T, 1, {"sh1": 0, "sc1": 1, "g1": 2, "sh2": 3, "sc2": 4}, 0)
        _make_G(s, T, C, 1, "gn1", mrow["sc1"], ("mrow", 1, 0, "sc1"), "c0")
        _make_G(s, T, C, 1, "gn2", mrow["sc2"], ("mrow", 1, 0, "sc2"), "c1")
        tl = _tail_alloc(C, T, P, s, 1, NO, mrow)
        tl.update(OHf=OHf, OHb=OHb, gates=gates)
        s.add("dve", lambda e: e.tensor_copy(out=tl["sc"][:, 15:16], in_=tl["sc"][:, 15:16]), r=[("mrow", 1, 0, "sc2")], w=["G2"])
        s.add("pool", lambda e: e.tensor_copy(out=tl["sc"][:, 14:15], in_=tl["sc"][:, 14:15]), r=[("mrow", 1, 0, "sh2")], w=["SH2"])
        stg = [T(f"f_stg{i}", [128, 8, 256], F32) for i in range(1)] * 2
        w_in = T("f_win", [128, 8, 3 * D], BF16)
        wres = _load_w_bf16(s, T, "w_in_o", w_in, Dm["w_in_o"], 3 * D, stg, "win")
        w_out = T("f_wout", [128, 8, D], BF16)
        wores = _load_w_bf16(s, T, "w_out_o", w_out, Dm["w_out_o"], D, stg, "wout")
        identb = T("f_identb", [128, 128], BF16)
        s.add("dve", lambda e: e.tensor_copy(out=identb[:], in_=tl["identf"][:]), r=["identf"], w=["identb"])
        cw = T("f_cw", [128, 3, 8], F32)
        s.add("sync", lambda e: e.dma_start(out=cw[:], in_=Dm["conv_c"]), w=["cw"], dma="cw")
        xr = [T(f"f_x{i}", [128, D], F32) for i in range(1)] * 2
        junk = tl["junk"]
        ss = T("f_ss", [128, 2 * NF], F32)
        s.add("dve", lambda e: e.memset(ss[:], 0.0), w=[("ssa", c) for c in range(2 * NF)])
        hf = T("f_hf", [128, D], F32)
        hb = T("f_hb", [128, D], BF16)
        hT = [T(f"f_hT{i}", [128, 8, 512], BF16) for i in range(1)]
        Yh = [T(f"f_Yh{i}", [128, 8, 514], F32) for i in range(2)]
        bgb = [T(f"f_bg{i}", [128, 8, 512], BF16) for i in range(2)]
        cgs = T("f_cgs", [128, 512], F32)
        ca = T("f_ca", [128, 512], F32)
        cb = T("f_cb", [128, 512], F32)
        gT = hT[0]
        lat = xr
        pT = P("f_pT", [128, 8, 128], BF16)
        pRot = [P(f"f_pRot{i}", [128, 512], F32) for i in range(4)]
        rot = [0]
        pW = [tl["pT32"][0], tl["pT32"][1]]
        pWv = [p[:].rearrange("p a b -> p (a b)") for p in pW]
        for i in range(2):
            s.add("dve", lambda e, i=i: e.memset(Yh[i][:], 0.0), w=[("Yh", i, c) for c in range(8)] + [("Yhl", i), ("Yhr", i)])

        def window_front(w):
            nt = 512 if w < 8 else 128
            hp = 0
            bp = w % 2
            for q in range(nt // 128):
                n = w * 4 + q
                par = 0
                s.add("sync", lambda e, n=n, par=par: e.dma_start(out=xr[par][:], in_=Dm["L2"][n * 128:(n + 1) * 128, :]), r=[("L2", n)], w=[("x", par)], dma=("x", par))
                _rms_rstd(s, ss, 2 * n, xr[par][:], ("x", par), junk, "a")
                s.add("dve", lambda e, n=n, par=par: e.scalar_tensor_tensor(out=hf[:], in0=xr[par][:], scalar=ss[:, 2 * n + 1:2 * n + 2], in1=mrow["sc1"][:],
                                                                            op0=ALU.mult, op1=ALU.mult),
                      r=[("x", par), ("rsa", 2 * n), ("mrow", 1, 0, "sc1")], w=["hf"])
                s.add("pool", lambda e: e.tensor_tensor(out=hb[:], in0=hf[:], in1=mrow["sh1"][:], op=ALU.add), r=["hf", ("mrow", 1, 0, "sh1")], w=["hb"])
                for k in range(8):
                    s.add("pe", lambda e, k=k: e.transpose(out=pT[:, k, :], in_=hb[:, k * 128:(k + 1) * 128], identity=identb[:]), r=["hb", "identb"], w=["pT"])
                s.add("act", lambda e, q=q, hp=hp: e.activation(out=hT[hp][:, :, q * 128:(q + 1) * 128], in_=pT[:], func=AF.Copy), r=["pT"], w=[("hT", hp, q), "HG"])
            hres = [("hT", hp, q) for q in range(nt // 128)] + ["HG"]
            yi = w % 2
            for c in range(8):
                def nxt():
                    i = rot[0] % 4
                    rot[0] += 1
                    return pRot[i], ("pRot", i)
                pC, pCn = nxt()
                pX, pXn = nxt()
                grp = [(pC, pCn, D + c * 128), (pX, pXn, 2 * D + c * 128)]
                if w < 8:
                    pB, pBn = nxt()
                    grp.append((pB, pBn, c * 128))
                for (pt, pn, c0) in grp:
                    for k in range(8):
                        s.add("pe", lambda e, pt=pt, c0=c0, k=k, nt=nt, hp=hp: e.matmul(out=pt[:, 0:nt], lhsT=w_in[:, k, c0:c0 + 128], rhs=hT[hp][:, k, 0:nt],
                                                                                         start=(k == 0), stop=(k == 7)),
                              r=hres + wres, w=[pn])
                s.add("act", lambda e, nt=nt, pC=pC: e.activation(out=cgs[:, 0:nt], in_=pC[:, 0:nt], func=AF.Copy), r=[pCn], w=["cgs"])
                s.add("dve", lambda e, c=c, nt=nt, yi=yi, pX=pX: e.tensor_tensor(out=Yh[yi][:, c, 1:1 + nt], in0=pX[:, 0:nt], in1=cgs[:, 0:nt], op=ALU.mult),
                      r=[pXn, "cgs"], w=[("Yh", yi, c)])
                if w < 8:
                    s.add("act", lambda e, c=c, bp=bp, pB=pB: e.activation(out=bgb[bp][:, c, :], in_=pB[:], func=AF.Copy), r=[pBn], w=[("bg", bp, c)])
            yall = [("Yh", yi, c) for c in range(8)]
            if w > 0:
                yp = (w - 1) % 2
                s.add("pool", lambda e, yi=yi, yp=yp: e.tensor_copy(out=Yh[yp][:, :, 513:514], in_=Yh[yi][:, :, 1:2]), r=yall, w=[("Yhr", yp)])
                s.add("pool", lambda e, yi=yi, yp=yp: e.tensor_copy(out=Yh[yi][:, :, 0:1], in_=Yh[yp][:, :, 512:513]), r=[("Yh", yp, c) for c in range(8)], w=[("Yhl", yi)])
            else:
                s.add("pool", lambda e, yi=yi: e.memset(Yh[yi][:, :, 0:1], 0.0), w=[("Yhl", yi)])

        def window_back(w):
            yi = w % 2
            hp = w % 2
            yres = [("Yhl", yi), ("Yhr", yi)]
            for c in range(8):
                s.add("dve", lambda e, c=c: e.tensor_scalar(out=ca[:], in0=Yh[yi][:, c, 1:513], scalar1=cw[:, 1, c:c + 1], scalar2=None, op0=ALU.mult),
                      r=yres + [("Yh", yi, c), "cw"], w=["ca"])
                s.add("dve", lambda e, c=c: e.scalar_tensor_tensor(out=cb[:], in0=Yh[yi][:, c, 0:512], scalar=cw[:, 0, c:c + 1], in1=ca[:], op0=ALU.mult, op1=ALU.add),
                      r=yres + [("Yh", yi, c), "cw", "ca"], w=["cb"])
                s.add("dve", lambda e, c=c: e.scalar_tensor_tensor(out=ca[:], in0=Yh[yi][:, c, 2:514], scalar=cw[:, 2, c:c + 1], in1=cb[:], op0=ALU.mult, op1=ALU.add),
                      r=yres + [("Yh", yi, c), "cw", "cb"], w=["ca"])
                s.add("pool", lambda e, c=c: e.tensor_tensor(out=gT[:, c, :], in0=ca[:], in1=bgb[hp][:, c, :], op=ALU.mult), r=["ca", ("bg", hp, c)], w=[("gT", c), "HG"])
            for q in range(4):
                n = w * 4 + q
                par = 0
                s.add("sync", lambda e, n=n, par=par: e.dma_start(out=xr[par][:], in_=Dm["L2"][n * 128:(n + 1) * 128, :]), r=[("L2", n)], w=[("x", par)], dma=("x", par))
                for jn in range(2):
                    for k in range(8):
                        s.add("pe", lambda e, jn=jn, k=k, q=q: e.matmul(out=pWv[jn], lhsT=gT[:, k, q * 128:(q + 1) * 128], rhs=w_out[:, k, jn * 512:(jn + 1) * 512],
                                                                        start=(k == 0), stop=(k == 7)),
                              r=[("gT", c) for c in range(8)] + ["HG"] + wores, w=[("pT32", jn)])
                for jn in range(2):
                    s.add("dve", lambda e, jn=jn: e.tensor_tensor(out=tt[:, jn * 512:(jn + 1) * 512], in0=pWv[jn], in1=mrow["g1"][:, jn * 512:(jn + 1) * 512], op=ALU.mult),
                          r=[("pT32", jn), ("mrow", 1, 0, "g1")], w=[("tt", jn)])
                lt = lat[par]
                s.add("dve", lambda e, lt=lt, par=par: e.tensor_tensor(out=lt[:], in0=tt[:], in1=xr[par][:], op=ALU.add),
                      r=[("tt", 0), ("tt", 1), ("x", par)], w=[("x", par)])
                s.add("sync", lambda e, lt=lt, n=n: e.dma_start(out=Dm["L3"][n * 128:(n + 1) * 128, :], in_=lt[:]), r=[("x", par)], w=[("L3", n)],
                      dma=("latst", par))
                _tail(s, n, lt, ("x", par), tl, 1)

        import os as _os
        _l1w = int(_os.environ.get("L1W", "8"))
        _l1b = int(_os.environ.get("L1B", "1"))
        window_front(0)
        for w in range(_l1w):
            window_front(w + 1)
            if _l1b:
                window_back(w)
        if _l1w == 8 and _l1b:
            _route_all(s, tl, NO, T)
        s.add("sync", lambda e: e.nop(), r=[("L3", n) for n in range(NO if (_l1w == 8 and _l1b) else 0)] + [("H2", n) for n in range(NO if (_l1w == 8 and _l1b) else 0)])
        s.emit()


def build(stages=("mod", "l0", "d0", "m0", "c0", "l1", "d1", "m1", "c1"), ext_in_extra=(), ext_out_extra=()):
    nc = bass.Bass("TRN2", target_bir_lowering=False)
    ext_in = set(INPUT_SHAPES) | set(ext_in_extra)
    ext_out = {"out"} | set(ext_out_extra)
    C = Ctx(nc, ext_in, ext_out)
    for k, sh in INPUT_SHAPES.items():
        C.dram(k, sh, F32)
    C.dram("out", (NO * 128, D), F32)
    C.dram("modrow", (2, 2, 6 * D), F32)
    C.dram("L1", (NF * 128, D), F32)
    C.dram("L2", (NF * 128, D), F32)
    C.dram("L3", (NO * 128, D), F32)
    C.dram("H2", (NF * 128, D), F32)
    C.dram("H2b", (NF * 128, D), BF16)
    C.dram("RI", (NS0 * 128, 4), F32)
    C.dram("Y", (2 * NF * 128, D), F32)
    C.dram("DBG", (128, 2 * NF + NS0 + 64), F32)
    with ExitStack() as es:
        _SEMPOOL.clear()
        _SEMPOOL["es"] = es
        pers = {}
        pers["OHf"] = es.enter_context(nc.sbuf_tensor("p_OHf", [128, NF, 2, 32], F32))
        pers["OHb"] = es.enter_context(nc.sbuf_tensor("p_OHb", [128, NF, 32], F32))
        pers["gates"] = es.enter_context(nc.sbuf_tensor("p_gates", [128, NF, 2], F32))
        pers["be_i"] = es.enter_context(nc.sbuf_tensor("p_be", [128, 2, NSS0], F32))
        for st in stages:
            if st == "mod":
                phase_mod(C)
            elif st == "l0":
                phase_l0(C, pers)
            elif st == "d0":
                phase_dispatch(C, pers, NF, NS0)
            elif st == "m0":
                phase_moe(C, pers, 0, NF, NS0)
            elif st == "c0":
                phase_combine(C, 0, NF, "L1", "L2", False)
            elif st == "l1":
                phase_l1(C, pers)
            elif st == "d1":
                phase_dispatch(C, pers, NO, NS1)
            elif st == "m1":
                phase_moe(C, pers, 1, NO, NS1)
            elif st == "c1":
                phase_combine(C, 1, NO, "L3", "out", True)
    return nc


def _consts():
    c = {}
    c["ident"] = np.eye(128, dtype=np.float32)
    c["widx"] = np.arange(128, dtype=np.float32)[:, None]
    c["utri"] = np.triu(np.ones((128, 128), np.float32), 1)
    c["tokf"] = (np.arange(NF)[None, :] * 128 + np.arange(128)[:, None]).astype(np.float32)
    c["thr"] = (np.arange(NSS0, dtype=np.float32) * 256.0)[None, :]
    fill = np.zeros((128, NS0, 4), np.float32)
    fill[:, :, 0] = OOB
    fill[:, :, 2] = OOB
    c["rifill"] = fill.reshape(128, NS0 * 4)
    j = np.arange(128)[:, None]
    i = np.arange(128)[None, :]
    mp = np.where(j >= i, 0.0, -30000.0).astype(np.float32)
    mn = np.where(j <= i, 0.0, -30000.0).astype(np.float32)
    c["maskP"] = np.tile(mp, (1, 4))
    c["maskN"] = np.tile(mn, (1, 4))
    return c


def _rope_tables(pos):
    quarter = 16
    inv = (10000.0 ** (-np.arange(quarter, dtype=np.float32) / quarter)).astype(np.float32)
    row = (pos // 64).astype(np.float32)
    col = (pos % 64).astype(np.float32)
    ar = row[:, None] * inv[None, :]
    ac = col[:, None] * inv[None, :]
    cr, sr, cc_, sc_ = np.cos(ar), np.sin(ar), np.cos(ac), np.sin(ac)
    Ct = np.concatenate([cr, cr, cc_, cc_], axis=1).astype(np.float32)
    St = np.concatenate([-sr, sr, -sc_, sc_], axis=1).astype(np.float32)
    return Ct, St


def _relayout_experts(inp):
    out = {}
    out["w_gate"] = np.ascontiguousarray(np.asarray(inp["w_gate"], np.float32).reshape(2, NE, 8, 128, 512).transpose(0, 1, 3, 2, 4)).reshape(2 * NE * 128, 4096)
    out["w_up"] = np.ascontiguousarray(np.asarray(inp["w_up"], np.float32).reshape(2, NE, 8, 128, 512).transpose(0, 1, 3, 2, 4)).reshape(2 * NE * 128, 4096)
    out["w_down"] = np.ascontiguousarray(np.asarray(inp["w_down"], np.float32).reshape(2, NE, 4, 128, 1024).transpose(0, 1, 3, 2, 4)).reshape(2 * NE * 128, 4096)
    return out


def _core_inputs(inp, core, consts, consts_w):
    b, hf = core // 2, core % 2
    m = dict(consts)
    x = inp["x"][b]
    if hf == 0:
        pos = np.arange(0, NB * 128)
        xs = x[0:NB * 128]
    else:
        pos = np.arange(8191, 8191 - NB * 128, -1)
        xs = x[::-1][0:NB * 128]
    m["xs"] = np.ascontiguousarray(xs)
    m["ctxs"] = np.ascontiguousarray(inp["ctx"][b])
    cc = np.stack([inp["c"][b], inp["c_ctx"]], axis=-1)
    m["cc"] = np.ascontiguousarray(cc.reshape(8, 128, 2).transpose(1, 0, 2))
    m["w_ada"] = inp["w_ada"]
    m["b_ada"] = inp["b_ada"]
    m["gn1"] = inp["g_norm1"]
    m["gn2"] = inp["g_norm2"]
    m["gfin"] = inp["g_final"][None, :]
    w = inp["w_in_even"][0]
    qperm = np.concatenate([np.arange((h * 4 + g) * 64, (h * 4 + g) * 64 + 64) for g in range(4) for h in range(2)])
    m["w_in_e"] = np.ascontiguousarray(np.concatenate([w[:, qperm], w[:, 512:]], axis=1))
    m["sink"] = inp["attn_sink"][0][None, :]
    m["g_sgu"] = inp["g_sgu"][0][None, :]
    wsp = inp["w_spatial"][0]
    bsp = inp["b_spatial"][0]
    cw = inp["conv_w"][0]
    if hf == 1:
        wsp = wsp[:, ::-1, ::-1]
        bsp = bsp[:, ::-1]
        cw = cw[::-1]
    m["w_spT"] = np.ascontiguousarray(wsp.transpose(2, 0, 1))
    m["b_spc"] = np.ascontiguousarray(bsp.T)
    m["w_out_e"] = inp["w_out_even"][0]
    m["w_in_o"] = inp["w_in_odd"][0]
    m["conv_c"] = np.ascontiguousarray(cw.reshape(3, 8, 128).transpose(2, 0, 1))
    m["w_out_o"] = inp["w_out_odd"][0]
    m["wr"] = np.ascontiguousarray(np.concatenate([inp["w_router_group"], inp["w_router_expert"]], axis=-1))
    m["br"] = np.ascontiguousarray(np.concatenate([inp["b_router_group"], inp["b_router_expert"]], axis=-1))
    m["w_gate"] = consts_w["w_gate"]
    m["w_up"] = consts_w["w_up"]
    m["w_down"] = consts_w["w_down"]
    Ct, St = _rope_tables(pos)
    m["ropeC"] = Ct
    m["ropeS"] = St
    return {k: np.ascontiguousarray(np.asarray(v, dtype=np.float32)) for k, v in m.items()}


def kernel(**inputs):
    inp = {k: np.asarray(v) for k, v in inputs.items()}
    consts = _consts()
    nc = build()
    cw = _relayout_experts(inp)
    in_maps = [_core_inputs(inp, c, consts, cw) for c in range(8)]
    res = run_bass_kernel_spmd(nc, in_maps, core_ids=list(range(8)))
    out = np.empty((4, 8192, D), np.float32)
    for c in range(8):
        b, hf = c // 2, c % 2
        o = res.results[c]["out"]
        if hf == 0:
            out[b, 0:4096] = o
        else:
            out[b, 4096:8192] = o[::-1]
    return out
```

```python
import numpy as np
from contextlib import ExitStack
import concourse.bass as bass
import concourse.mybir as mybir
from concourse.bass_utils import run_bass_kernel_spmd

F32 = mybir.dt.float32
F32R = mybir.dt.float32r
BF16 = mybir.dt.bfloat16
I32 = mybir.dt.int32
AF = mybir.ActivationFunctionType
ALU = mybir.AluOpType
AX = mybir.AxisListType

NB = 34
NF = 33
NO = 32
D = 1024
NE = 32
EPS = 1e-6
NSS0 = NF + NE
NSS1 = NO + NE
NS0 = 2 * NSS0
NS1 = 2 * NSS1
OOB = 1.0e6


_UC = [0]
_JR = {}
_SEMPOOL = {"free": [], "es": None}


def U(n):
    _UC[0] += 1
    return f"{n}_u{_UC[0]}"


class Sched:
    ENG = ("sync", "act", "pool", "pe", "dve")

    def __init__(self, nc, name):
        self.nc = nc
        self.name = name
        self.ops = []
        self.last_w = {}
        self.readers = {}

    def add(self, eng, fn, r=(), w=(), dma=None):
        idx = len(self.ops)
        deps = set()
        for x in r:
            j = self.last_w.get(x)
            if j is not None:
                deps.add(j)
        for x in w:
            j = self.last_w.get(x)
            if j is not None:
                deps.add(j)
            deps.update(self.readers.get(x, ()))
        deps.discard(idx)
        self.ops.append(dict(eng=eng, fn=fn, deps=deps, dma=dma))
        for x in r:
            self.readers.setdefault(x, []).append(idx)
        for x in w:
            self.last_w[x] = idx
            self.readers[x] = []
        return idx

    def emit(self):
        nc = self.nc
        ops = self.ops
        has_dep = [False] * len(ops)
        for o in ops:
            for j in o["deps"]:
                has_dep[j] = True
        semkeys = []
        seen = set()
        for o in ops:
            k = (("dmasw" if o["eng"] == "pool" else "dma"), o["dma"]) if o["dma"] is not None else ("eng", o["eng"])
            o["semkey"] = k
            if k not in seen:
                seen.add(k)
                semkeys.append(k)
        pool = _SEMPOOL
        semobj = {}
        counts = {}
        for k in semkeys:
            fl = pool.setdefault("free_" + k[0], [])
            if fl:
                so, c0 = fl.pop()
            else:
                so, c0 = pool["es"].enter_context(nc.semaphore(U("sem"))), 0
            semobj[k] = so
            counts[k] = c0
        start = dict(counts)
        for i, o in enumerate(ops):
            k = o["semkey"]
            if o["dma"] is not None:
                counts[k] += 16
                o["semval"] = counts[k]
                o["inc"] = 16
            elif has_dep[i]:
                counts[k] += 1
                o["semval"] = counts[k]
                o["inc"] = 1
            else:
                o["semval"] = None
                o["inc"] = 0
        waited = {e: dict(start) for e in self.ENG}
        for o in ops:
            need = {}
            for j in o["deps"]:
                d = ops[j]
                if d["eng"] == "pe" and o["eng"] == "pe" and d["dma"] is None:
                    continue
                k = d["semkey"]
                v = d["semval"]
                if v is None:
                    continue
                if need.get(k, 0) < v:
                    need[k] = v
            wl = []
            for k, v in need.items():
                if waited[o["eng"]].get(k, 0) >= v:
                    continue
                waited[o["eng"]][k] = v
                wl.append((k, v))
            o["waits"] = wl
        final_waits = [(k, counts[k]) for k in semkeys if k[0] in ("dma", "dmasw") and counts[k] > start[k]]
        with ExitStack() as es:
            sems = semobj
            blk = es.enter_context(nc.Block())

            def run(engname):
                def body(eng):
                    for o in ops:
                        if o["eng"] != engname:
                            continue
                        for k, v in o["waits"]:
                            eng.wait_ge(sems[k], v)
                        ins = o["fn"](eng)
                        if o["inc"]:
                            ins.then_inc(sems[o["semkey"]], o["inc"])
                return body

            present = {o["eng"] for o in ops}

            def run_sync(eng):
                run("sync")(eng)
                for k, v in final_waits:
                    eng.wait_ge(sems[k], v)

            blk.sync(run_sync)
            if False:
                blk.sync(run("sync"))
            if "act" in present:
                blk.scalar(run("act"))
            if "pool" in present:
                blk.gpsimd(run("pool"))
            if "pe" in present:
                blk.tensor(run("pe"))
            if "dve" in present:
                blk.vector(run("dve"))
        nc.all_engine_barrier()
        for k in semkeys:
            pool["free_" + k[0]].append((semobj[k], counts[k]))
        return len(ops)


class Ctx:
    def __init__(self, nc, ext_in, ext_out):
        self.nc = nc
        self.ext_in = ext_in
        self.ext_out = ext_out
        self.d = {}

    def dram(self, name, shape, dtype=F32):
        kind = "ExternalInput" if name in self.ext_in else ("ExternalOutput" if name in self.ext_out else "Internal")
        ap = self.nc.dram_tensor(name, list(shape), dtype, kind=kind).ap()
        self.d[name] = ap
        return ap


INPUT_SHAPES = {
    "xs": (NB * 128, D), "ctxs": (256, D), "cc": (128, 8, 2),
    "w_ada": (2, D, 6 * D), "b_ada": (2, 6 * D), "gn1": (2, D), "gn2": (2, D), "gfin": (1, D),
    "w_in_e": (D, 1792), "sink": (1, 8), "g_sgu": (1, 512), "w_spT": (128, 8, 128), "b_spc": (128, 8),
    "w_out_e": (D, D), "w_in_o": (D, 3 * D), "conv_c": (128, 3, 8), "w_out_o": (D, D),
    "wr": (2, D, 36), "br": (2, 36),
    "w_gate": (2 * NE * 128, 4096), "w_up": (2 * NE * 128, 4096), "w_down": (2 * NE * 128, 4096),
    "ropeC": (NB * 128, 64), "ropeS": (NB * 128, 64), "maskP": (128, 512), "maskN": (128, 512),
    "widx": (128, 1), "ident": (128, 128), "utri": (128, 128), "tokf": (128, NF), "thr": (1, NSS0), "rifill": (128, NS0 * 4),
}


def _rms_rstd(s, ss, col, xtile, xres, junk, tag):
    s.add("act", lambda e: e.activation(out=junk[:], in_=xtile, func=AF.Square, accum_out=ss[:, col:col + 1]),
          r=[xres], w=[_JR.get(id(junk), "junk" + tag), ("ss" + tag, col)])
    s.add("act", lambda e: e.activation(out=ss[:, col + 1:col + 2], in_=ss[:, col:col + 1], func=AF.Sqrt,
                                        scale=1.0 / D, bias=EPS),
          r=[("ss" + tag, col)], w=[("rs" + tag, col)])
    s.add("dve", lambda e: e.reciprocal(out=ss[:, col + 1:col + 2], in_=ss[:, col + 1:col + 2]),
          r=[("rs" + tag, col)], w=[("rs" + tag, col)])


def phase_mod(C):
    nc = C.nc
    Dm = C.d
    with ExitStack() as es:
        T = lambda n, sh, dt: es.enter_context(nc.sbuf_tensor(U(n), sh, dt))
        P = lambda n, sh, dt: es.enter_context(nc.psum_tensor(U(n), sh, dt))
        cc = T("m_cc", [128, 8, 2], F32)
        sl = T("m_sl", [128, 8, 2], F32)
        wa = [T(f"m_wa{i}", [128, 8, 512], F32) for i in range(2)]
        bb = [T(f"m_bb{l}", [2, 6 * D], F32) for l in range(2)]
        mr = [T(f"m_mr{l}", [2, 6 * D], F32) for l in range(2)]
        ps = [P(f"m_ps{i}", [2, 512], F32) for i in range(2)]
        s = Sched(nc, "mod")
        s.add("sync", lambda e: e.dma_start(out=cc[:], in_=Dm["cc"]), w=["cc"], dma="cc")
        s.add("act", lambda e: e.activation(out=sl[:], in_=cc[:], func=AF.Silu), r=["cc"], w=["sl"])
        for l in range(2):
            s.add("sync", lambda e, l=l: e.dma_start(out=bb[l][:], in_=Dm["b_ada"][l:l + 1, :].broadcast_to([2, 6 * D])),
                  w=[("bb", l)], dma=("bb", l))
        it = 0
        import os as _os
        for l in range(2):
            for j in range(int(_os.environ.get("MODJ", "12"))):
                par = it % 2
                it += 1
                s.add("sync", lambda e, l=l, j=j, par=par: e.dma_start(
                    out=wa[par][:], in_=Dm["w_ada"][l, :, j * 512:(j + 1) * 512].rearrange("(k p) n -> p k n", p=128)),
                    w=[("wa", par)], dma=("wa", par))
                for k in range(8):
                    s.add("pe", lambda e, k=k, par=par: e.matmul(out=ps[par][:], lhsT=sl[:, k, :], rhs=wa[par][:, k, :],
                                                                  start=(k == 0), stop=(k == 7)),
                          r=["sl", ("wa", par)], w=[("ps", par)])
                s.add("dve", lambda e, l=l, j=j, par=par: e.tensor_tensor(
                    out=mr[l][:, j * 512:(j + 1) * 512], in0=ps[par][:], in1=bb[l][:, j * 512:(j + 1) * 512], op=ALU.add),
                    r=[("ps", par), ("bb", l)], w=[("mr", l)])
            s.add("sync", lambda e, l=l: e.dma_start(out=Dm["modrow"][l], in_=mr[l][:]), r=[("mr", l)], w=[("modrow", l)],
                  dma=("modst", l))
        s.add("sync", lambda e: e.nop(), r=[("modrow", 0), ("modrow", 1)])
        s.emit()


def _load_modrows(s, C, T, l, names, sidx=0):
    out = {}
    for nm, pi in names.items():
        t = T(f"mr_{l}_{sidx}_{nm}", [128, D], F32)
        s.add("sync", lambda e, t=t, pi=pi: e.dma_start(
            out=t[:], in_=C.d["modrow"][l, sidx:sidx + 1, pi * D:(pi + 1) * D].broadcast_to([128, D])),
            w=[("mrow", l, sidx, nm)], dma=("mrow", l, sidx, nm))
        out[nm] = t
    return out


def _make_G(s, T, C, l, which, scrow, res_sc, tag):
    if getattr(s, "gn_tile", None) is None:
        s.gn_tile = T("gn_shared", [128, D], F32)
    g = s.gn_tile
    s.add("sync", lambda e: e.dma_start(out=g[:], in_=C.d[which][l:l + 1, :].broadcast_to([128, D])),
          w=["gnS"], dma="gnS")
    s.add("dve", lambda e: e.scalar_tensor_tensor(out=scrow[:], in0=scrow[:], scalar=1.0, in1=g[:], op0=ALU.add, op1=ALU.mult),
          r=[res_sc, "gnS"], w=[res_sc])


def _load_w_bf16(s, T, name, dst, src_ap, ncols, stg, tag):
    j = 0
    c0 = 0
    while c0 < ncols:
        cw = min(256, ncols - c0)
        par = (j % 2) if stg[0] is not stg[1] else 0
        s.add("sync", lambda e, c0=c0, cw=cw, par=par: e.dma_start(
            out=stg[par][:, :, 0:cw], in_=src_ap[:, c0:c0 + cw].rearrange("(k p) n -> p k n", p=128)),
            w=[("stg", par)], dma=("stg", par))
        s.add("pool", lambda e, c0=c0, cw=cw, par=par: e.tensor_copy(out=dst[:, :, c0:c0 + cw], in_=stg[par][:, :, 0:cw]),
              r=[("stg", par)], w=[(tag, j)])
        c0 += cw
        j += 1
    return [(tag, jj) for jj in range(j)]


def _tail(s, n, lat, latres, tl, l):
    nc = tl["nc"]
    ss, junk = tl["ss2"], tl["junk"]
    _rms_rstd(s, ss, 2 * n, lat[:], latres, junk, "t")
    h2 = tl["h2"][0]
    hres = ("h2", 0)
    s.add("dve", lambda e: e.scalar_tensor_tensor(out=h2[:], in0=lat[:], scalar=ss[:, 2 * n + 1:2 * n + 2], in1=tl["G2"][:],
                                                  op0=ALU.mult, op1=ALU.mult),
          r=[latres, ("rst", 2 * n), "G2"], w=[hres])
    s.add("dve", lambda e: e.tensor_tensor(out=h2[:], in0=h2[:], in1=tl["SH2"][:], op=ALU.add), r=[hres, "SH2"], w=[hres])
    h2b = tl["h2b"]
    s.add("act", lambda e: e.activation(out=h2b[:], in_=h2[:], func=AF.Copy), r=[hres], w=["h2b"])
    s.add("sync", lambda e: e.dma_start(out=tl["H2b"][n * 128:(n + 1) * 128, :], in_=h2b[:]), r=["h2b"], w=[("H2", n)],
          dma=("h2st", 0))
    pT = tl["pT32"]
    for k in range(8):
        s.add("pe", lambda e, k=k: e.transpose(out=pT[k // 4][:, k % 4, :], in_=h2[:, k * 128:(k + 1) * 128], identity=tl["identf"][:]),
              r=[hres, "identf"], w=[("pT32", k // 4)])
    h2T = tl["h2T"]
    s.add("act", lambda e: e.activation(out=h2T[:, 0:4, :], in_=pT[0][:], func=AF.Copy), r=[("pT32", 0)], w=["h2Ta"])
    s.add("act", lambda e: e.activation(out=h2T[:, 4:8, :], in_=pT[1][:], func=AF.Copy), r=[("pT32", 1)], w=["h2Tb"])
    pR = tl["pR"]
    for k in range(8):
        s.add("pe", lambda e, k=k: e.matmul(out=pR[:, 0:36], lhsT=h2T[:, k, :], rhs=tl["wr"][:, k, :], start=(k == 0), stop=(k == 7)),
              r=["h2Ta", "h2Tb", "wr"], w=["pR"])
    lg = tl["lg_all"]
    s.add("dve", lambda e: e.tensor_tensor(out=lg[:, n, :], in0=pR[:, 0:36], in1=tl["brow"][:], op=ALU.add), r=["pR", "brow"], w=[("lg", n)])


def _route_all(s, tl, NT, T):
    L = tl["lg_all"]
    G = L[:, 0:NT, 0:4]
    E4 = L[:, 0:NT, 4:36].rearrange("p n (g j) -> p n g j", g=4)
    OHf, OHb, gates = tl["OHf"], tl["OHb"], tl["gates"]
    gmax = T("r_gmax", [128, NF], F32)
    ohg = T("r_ohg", [128, NF, 4], F32)
    gsh = T("r_gsh", [128, NF, 4], F32)
    sumg = T("r_sumg", [128, NF], F32)
    tmp4 = T("r_tmp4", [128, NF, 4, 8], F32)
    esel = T("r_esel", [128, NF, 8], F32)
    class _V:
        def __init__(self, i):
            self.i = i

        def __getitem__(self, idx):
            return tmp4[:, :, self.i, :][idx]
    esel2, oh1, oh2 = _V(0), _V(1), _V(2)
    m1 = T("r_m1", [128, NF], F32)
    m2 = T("r_m2", [128, NF], F32)
    e21 = T("r_e21", [128, NF], F32)
    w1 = gmax
    w2 = m2
    allg = [("lg", n) for n in range(NT)]
    b3 = lambda t, k: t[:, 0:NT].unsqueeze(2).broadcast_to([128, NT, k])
    s.add("dve", lambda e: e.reduce_max(out=gmax[:, 0:NT], in_=G, axis=AX.X), r=allg, w=["gmax"])
    s.add("dve", lambda e: e.tensor_tensor(out=ohg[:, 0:NT, :], in0=G, in1=b3(gmax, 4), op=ALU.is_ge), r=allg + ["gmax"], w=["ohg"])
    s.add("dve", lambda e: e.tensor_tensor(out=gsh[:, 0:NT, :], in0=G, in1=b3(gmax, 4), op=ALU.subtract), r=allg + ["gmax"], w=["gsh"])
    s.add("act", lambda e: e.activation(out=gsh[:, 0:NT, :], in_=gsh[:, 0:NT, :], func=AF.Exp), r=["gsh"], w=["gsh"])
    s.add("dve", lambda e: e.reduce_sum(out=sumg[:, 0:NT], in_=gsh[:, 0:NT, :], axis=AX.X), r=["gsh"], w=["sumg"])
    s.add("dve", lambda e: e.reciprocal(out=sumg[:, 0:NT], in_=sumg[:, 0:NT]), r=["sumg"], w=["sumg"])
    s.add("dve", lambda e: e.tensor_tensor(out=tmp4[:, 0:NT], in0=E4, in1=ohg[:, 0:NT, :].unsqueeze(3).broadcast_to([128, NT, 4, 8]), op=ALU.mult),
          r=allg + ["ohg"], w=["tmp4"])
    s.add("dve", lambda e: e.reduce_sum(out=esel[:, 0:NT, :], in_=tmp4[:, 0:NT].rearrange("p n g j -> p n j g"), axis=AX.X), r=["tmp4"], w=["esel"])
    s.add("dve", lambda e: e.reduce_max(out=m1[:, 0:NT], in_=esel[:, 0:NT, :], axis=AX.X), r=["esel"], w=["m1"])
    s.add("dve", lambda e: e.tensor_tensor(out=oh1[:, 0:NT, :], in0=esel[:, 0:NT, :], in1=b3(m1, 8), op=ALU.is_ge), r=["esel", "m1"], w=["oh1"])
    s.add("dve", lambda e: e.scalar_tensor_tensor(out=esel2[:, 0:NT, :], in0=oh1[:, 0:NT, :], scalar=-1.0e30, in1=esel[:, 0:NT, :], op0=ALU.mult, op1=ALU.add),
          r=["oh1", "esel"], w=["esel2"])
    s.add("dve", lambda e: e.reduce_max(out=m2[:, 0:NT], in_=esel2[:, 0:NT, :], axis=AX.X), r=["esel2"], w=["m2"])
    s.add("dve", lambda e: e.tensor_tensor(out=oh2[:, 0:NT, :], in0=esel2[:, 0:NT, :], in1=b3(m2, 8), op=ALU.is_ge), r=["esel2", "m2"], w=["oh2"])
    s.add("dve", lambda e: e.tensor_tensor(out=e21[:, 0:NT], in0=m2[:, 0:NT], in1=m1[:, 0:NT], op=ALU.subtract), r=["m1", "m2"], w=["e21"])
    s.add("act", lambda e: e.activation(out=e21[:, 0:NT], in_=e21[:, 0:NT], func=AF.Exp), r=["e21"], w=["e21"])
    s.add("dve", lambda e: e.tensor_scalar(out=w1[:, 0:NT], in0=e21[:, 0:NT], scalar1=1.0, scalar2=None, op0=ALU.add), r=["e21"], w=["w1"])
    s.add("dve", lambda e: e.reciprocal(out=w1[:, 0:NT], in_=w1[:, 0:NT]), r=["w1"], w=["w1"])
    s.add("dve", lambda e: e.tensor_tensor(out=w2[:, 0:NT], in0=e21[:, 0:NT], in1=w1[:, 0:NT], op=ALU.mult), r=["e21", "w1"], w=["w2"])
    s.add("dve", lambda e: e.tensor_tensor(out=gates[:, 0:NT, 0], in0=w1[:, 0:NT], in1=sumg[:, 0:NT], op=ALU.mult), r=["w1", "sumg"], w=["gate0"])
    s.add("dve", lambda e: e.tensor_tensor(out=gates[:, 0:NT, 1], in0=w2[:, 0:NT], in1=sumg[:, 0:NT], op=ALU.mult), r=["w2", "sumg"], w=["gate1"])
    for k, oh, nm in ((0, oh1, "oh1"), (1, oh2, "oh2")):
        s.add("dve", lambda e, k=k, oh=oh: e.tensor_tensor(
            out=OHf[:, 0:NT, k, :].rearrange("p n (g j) -> p n g j", g=4),
            in0=ohg[:, 0:NT, :].unsqueeze(3).broadcast_to([128, NT, 4, 8]), in1=oh[:, 0:NT, :].unsqueeze(2).broadcast_to([128, NT, 4, 8]), op=ALU.mult),
            r=["ohg", nm], w=[("OHf", k)])
    s.add("dve", lambda e: e.tensor_tensor(out=OHb[:, 0:NT, :], in0=OHf[:, 0:NT, 0, :], in1=OHf[:, 0:NT, 1, :], op=ALU.add),
          r=[("OHf", 0), ("OHf", 1)], w=["OHb"])
    s.add("sync", lambda e: e.nop(), r=["OHb", "gate0", "gate1"])


def _tail_alloc(C, T, P, s, l, NT, mrow):
    nc = C.nc
    tl = dict(nc=nc)
    tl["ss2"] = T("t_ss2", [128, 2 * NF], F32)
    tl["h2"] = [T("t_h2_0", [128, D], F32)] * 2
    tl["h2T"] = T("t_h2T", [128, 8, 128], F32)
    tl["pT32"] = [P(f"t_pT32_{i}", [128, 4, 128], F32) for i in range(2)]
    tl["pR"] = P("t_pR", [128, 512], F32)
    tl["lg_all"] = T("t_lg_all", [128, NF, 36], F32)
    tl["sc"] = T("t_sc", [128, 16], F32)
    tl["identf"] = T("t_identf", [128, 128], F32)
    tl["wr"] = T("t_wr", [128, 8, 36], F32)
    tl["brow"] = T("t_brow", [128, 36], F32)
    tl["G2"] = mrow["sc2"]
    tl["SH2"] = mrow["sh2"]
    tl["H2b"] = C.d["H2b"]
    tl["h2b"] = T("t_h2b", [128, D], BF16)
    tl["junk"] = tl["h2b"]
    _JR[id(tl["h2b"])] = "h2b"
    s.add("dve", lambda e: e.memset(tl["ss2"][:], 0.0), w=[("sst", c) for c in range(2 * NF)])
    s.add("sync", lambda e: e.dma_start(out=tl["identf"][:], in_=C.d["ident"]), w=["identf"], dma="identf")
    s.add("sync", lambda e: e.dma_start(out=tl["wr"][:], in_=C.d["wr"][l].rearrange("(k p) n -> p k n", p=128)), w=["wr"], dma="wr")
    s.add("sync", lambda e: e.dma_start(out=tl["brow"][:], in_=C.d["br"][l:l + 1, :].broadcast_to([128, 36])), w=["brow"], dma="brow")
    return tl


def phase_l0(C, pers):
    nc = C.nc
    Dm = C.d
    OHf, OHb, gates = pers["OHf"], pers["OHb"], pers["gates"]
    with ExitStack() as es0:
        T0 = lambda n, sh, dt: es0.enter_context(nc.sbuf_tensor(U(n), sh, dt))
        qT_all = T0("qT_all", [128, NB, 512], BF16)
        kT_all = T0("kT_all", [128, NB * 128], BF16)
        Vaug = T0("Vaug", [128, NB, 2, 65], BF16)
        sg_all = T0("sg_all", [128, NB, 512], BF16)
        kcT = T0("kcT", [128, 256], BF16)
        Vcaug = T0("Vcaug", [128, 2, 2, 65], BF16)
        identb = T0("identb", [128, 128], BF16)
        with ExitStack() as es:
            T = lambda n, sh, dt: es.enter_context(nc.sbuf_tensor(U(n), sh, dt))
            P = lambda n, sh, dt: es.enter_context(nc.psum_tensor(U(n), sh, dt))
            s = Sched(nc, "l0a")
            Gt = T("a_Gt", [128, D], F32)
            St = T("a_St", [128, D], F32)
            gnt = T("a_gnt", [128, D], F32)
            s.add("sync", lambda e: e.dma_start(out=gnt[:], in_=Dm["gn1"][0:1, :].broadcast_to([128, D])), w=["gn"], dma="gn")

            def load_rows(sidx):
                s.add("sync", lambda e: e.dma_start(out=St[:], in_=Dm["modrow"][0, sidx:sidx + 1, 0:D].broadcast_to([128, D])), w=["SHrow"], dma="SHrow")
                s.add("sync", lambda e: e.dma_start(out=Gt[:], in_=Dm["modrow"][0, sidx:sidx + 1, D:2 * D].broadcast_to([128, D])), w=["Grow"], dma="Grow")
                s.add("dve", lambda e: e.scalar_tensor_tensor(out=Gt[:], in0=Gt[:], scalar=1.0, in1=gnt[:], op0=ALU.add, op1=ALU.mult),
                      r=["Grow", "gn"], w=["Grow"])
            stg = [T("a_stg0", [128, 8, 256], F32)] * 2
            w_in = T("a_win", [128, 8, 1792], BF16)
            wres = _load_w_bf16(s, T, "w_in_e", w_in, Dm["w_in_e"], 1792, stg, "win")
            identf = T("a_identf", [128, 128], F32)
            s.add("sync", lambda e: e.dma_start(out=identf[:], in_=Dm["ident"]), w=["identf"], dma="identf")
            s.add("dve", lambda e: e.tensor_copy(out=identb[:], in_=identf[:]), r=["identf"], w=["identb"])
            wspf = T("a_wspf", [128, 8, 128], F32)
            wspb = T("a_wspb", [128, 8, 128], BF16)
            s.add("sync", lambda e: e.dma_start(out=wspf[:], in_=Dm["w_spT"]), w=["wspf"], dma="wspf")
            s.add("dve", lambda e: e.tensor_copy(out=wspb[:], in_=wspf[:]), r=["wspf"], w=["wspb"])
            bsp = T("a_bsp", [128, 8], F32)
            s.add("sync", lambda e: e.dma_start(out=bsp[:], in_=Dm["b_spc"]), w=["bsp"], dma="bsp")
            gsgu = T("a_gsgu", [128, 512], F32)
            s.add("sync", lambda e: e.dma_start(out=gsgu[:], in_=Dm["g_sgu"].broadcast_to([128, 512])), w=["gsgu"], dma="gsgu")
            rCS = [T(f"a_rCS{i}", [128, 2, 64], F32) for i in range(2)]
            xr = [T(f"a_x{i}", [128, D], F32) for i in range(2)]
            junk = T("a_junk", [128, D], BF16)
            ss = T("a_ss", [128, 2 * (NB + 2)], F32)
            hf = T("a_hf", [128, D], F32)
            hb = T("a_hb", [128, D], BF16)
            hT = T("a_hT", [128, 8, 128], BF16)
            qkf2 = [T(f"a_qkf{i}", [128, 640], F32) for i in range(2)]
            qk1 = T("a_qk1", [128, 640], F32)
            qk2 = T("a_qk2", [128, 640], F32)
            qkb = T("a_qkb", [128, 640], BF16)
            gu2 = [T(f"a_gu{i}", [128, 512], F32) for i in range(2)]
            gz2 = [T(f"a_gz{i}", [128, 512], F32) for i in range(2)]
            zb = T("a_zb", [128, 512], BF16)
            st6 = T("a_st6", [128, 6], F32)
            mv = T("a_mv", [128, 4], F32)
            t1 = T("a_t1", [128, 512], F32)
            pT = P("a_pT", [128, 8, 128], BF16)
            psq = P("a_psq", [128, 512], F32)
            pskv = P("a_pskv", [128, 512], F32)
            psu = P("a_psu", [128, 512], F32)
            psz = P("a_psz", [128, 512], F32)
            pssp = P("a_pssp", [128, 512], F32)
            s.add("dve", lambda e: e.memset(ss[:], 0.0), w=[("ssa", c) for c in range(2 * (NB + 2))])
            s.add("dve", lambda e: e.memset(Vaug[:], 1.0), w=[("V", n) for n in range(NB)])
            s.add("dve", lambda e: e.memset(Vcaug[:], 1.0), w=[("Vc", j) for j in range(2)])

            def blockA(n, is_ctx, stage=0):
                par = n % 2
                qkf, gu, gz = qkf2[par], gu2[par], gz2[par]
                if stage == 2:
                    return blockA2(n)
                col = 2 * n
                src = Dm["ctxs"] if is_ctx else Dm["xs"]
                bi = n - NB if is_ctx else n
                s.add("sync", lambda e: e.dma_start(out=xr[par][:], in_=src[bi * 128:(bi + 1) * 128, :]), w=[("x", par)], dma=("x", par))
                _rms_rstd(s, ss, col, xr[par][:], ("x", par), junk, "a")
                gres = "Grow"
                sres = "SHrow"
                s.add("dve", lambda e: e.scalar_tensor_tensor(out=hf[:], in0=xr[par][:], scalar=ss[:, col + 1:col + 2], in1=Gt[:],
                                                              op0=ALU.mult, op1=ALU.mult),
                      r=[("x", par), ("rsa", col), gres], w=["hf"])
                s.add("pool", lambda e: e.tensor_tensor(out=hb[:], in0=hf[:], in1=St[:], op=ALU.add), r=["hf", sres], w=["hb"])
                for k in range(8):
                    s.add("pe", lambda e, k=k: e.transpose(out=pT[:, k, :], in_=hb[:, k * 128:(k + 1) * 128], identity=identb[:]),
                          r=["hb", "identb"], w=["pT"])
                s.add("act", lambda e: e.activation(out=hT[:], in_=pT[:], func=AF.Copy), r=["pT"], w=["hT"])
                groups = [(pskv, "pskv", 512, 256)] if is_ctx else [(psq, "psq", 0, 512), (pskv, "pskv", 512, 256), (psu, "psu", 768, 512), (psz, "psz", 1280, 512)]
                for (pt, pn, c0, cw) in groups:
                    for k in range(8):
                        s.add("pe", lambda e, pt=pt, c0=c0, cw=cw, k=k: e.matmul(out=pt[:, 0:cw], lhsT=hT[:, k, :], rhs=w_in[:, k, c0:c0 + cw],
                                                                                 start=(k == 0), stop=(k == 7)),
                              r=["hT"] + wres, w=[pn])
                if is_ctx:
                    j = bi
                    s.add("act", lambda e: e.activation(out=qkb[:, 0:128], in_=pskv[:, 0:128], func=AF.Copy), r=["pskv"], w=["qkb"])
                    s.add("act", lambda e: e.activation(out=Vcaug[:, j, :, 0:64], in_=pskv[:, 128:256].rearrange("p (h d) -> p h d", h=2), func=AF.Copy),
                          r=["pskv"], w=[("Vc", j)])
                    s.add("pe", lambda e: e.transpose(out=pT[:, 0, :], in_=qkb[:, 0:128], identity=identb[:]), r=["qkb", "identb"], w=["pT"])
                    s.add("dve", lambda e: e.tensor_copy(out=kcT[:, j * 128:(j + 1) * 128], in_=pT[:, 0, :]), r=["pT"], w=[("kcT", j)])
                    return
                s.add("act", lambda e: e.activation(out=qkf[:, 0:512], in_=psq[:], func=AF.Copy), r=["psq"], w=[("qkf_q", par)])
                s.add("act", lambda e: e.activation(out=qkf[:, 512:640], in_=pskv[:, 0:128], func=AF.Copy), r=["pskv"], w=[("qkf_k", par)])
                s.add("act", lambda e: e.activation(out=Vaug[:, n, :, 0:64], in_=pskv[:, 128:256].rearrange("p (h d) -> p h d", h=2), func=AF.Copy),
                      r=["pskv"], w=[("V", n)])
                s.add("act", lambda e: e.activation(out=gu[:], in_=psu[:], func=AF.Gelu), r=["psu"], w=[("gu", par)])
                s.add("act", lambda e: e.activation(out=gz[:], in_=psz[:], func=AF.Gelu), r=["psz"], w=[("gz", par)])

            def blockA2(n):
                par = n % 2
                qkf, gu, gz = qkf2[par], gu2[par], gz2[par]
                rt = rCS[par]
                s.add("pool", lambda e: e.dma_start(out=rt[:, 0, :], in_=Dm["ropeC"][n * 128:(n + 1) * 128, :]), w=[("rC", par)], dma=("rC", par))
                s.add("pool", lambda e: e.dma_start(out=rt[:, 1, :], in_=Dm["ropeS"][n * 128:(n + 1) * 128, :]), w=[("rS", par)], dma=("rS", par))
                v3 = lambda t: t[:].rearrange("p (h d) -> p h d", d=64)
                s.add("dve", lambda e: e.tensor_tensor(out=v3(qk1), in0=v3(qkf), in1=rt[:, 0:1, :].broadcast_to([128, 10, 64]), op=ALU.mult),
                      r=[("qkf_q", par), ("qkf_k", par), ("rC", par)], w=["qk1"])
                v5 = lambda t, pr: t[:].rearrange("p (h a b c) -> p h a b c", a=2, b=2, c=16)[:, :, :, pr, :]
                sv = lambda pr: rt[:, 1:2, :].rearrange("p o (a b c) -> p o a b c", a=2, b=2, c=16)[:, :, :, pr, :].broadcast_to([128, 10, 2, 16])
                for pr in range(2):
                    s.add("pool", lambda e, pr=pr: e.tensor_tensor(out=v5(qk2, pr), in0=v5(qkf, 1 - pr), in1=sv(pr), op=ALU.mult),
                          r=[("qkf_q", par), ("qkf_k", par), ("rS", par)], w=[("qk2", pr)])
                s.add("dve", lambda e: e.tensor_tensor(out=qkb[:], in0=qk1[:], in1=qk2[:], op=ALU.add), r=["qk1", ("qk2", 0), ("qk2", 1)], w=["qkb"])
                for j in range(5):
                    s.add("pe", lambda e, j=j: e.transpose(out=pT[:, j, :], in_=qkb[:, j * 128:(j + 1) * 128], identity=identb[:]),
                          r=["qkb", "identb"], w=["pT"])
                s.add("act", lambda e: e.activation(out=qT_all[:, n, :], in_=pT[:, 0:4, :].rearrange("p a b -> p (a b)"), func=AF.Copy), r=["pT"], w=[("qT", n)])
                s.add("dve", lambda e: e.tensor_copy(out=kT_all[:, n * 128:(n + 1) * 128], in_=pT[:, 4, :]), r=["pT"], w=[("kT", n)])
                s.add("dve", lambda e: e.bn_stats(out=st6[:], in_=gz[:]), r=[("gz", par)], w=["st6"])
                s.add("dve", lambda e: e.bn_aggr(out=mv[:, 0:2], in_=st6[:]), r=["st6"], w=["mv"])
                s.add("act", lambda e: e.activation(out=mv[:, 2:3], in_=mv[:, 1:2], func=AF.Sqrt, scale=1.0, bias=EPS), r=["mv"], w=["mv2"])
                s.add("dve", lambda e: e.reciprocal(out=mv[:, 3:4], in_=mv[:, 2:3]), r=["mv2"], w=["mv3"])
                s.add("dve", lambda e: e.tensor_scalar(out=gz[:], in0=gz[:], scalar1=mv[:, 0:1], scalar2=mv[:, 3:4], op0=ALU.subtract, op1=ALU.mult),
                      r=[("gz", par), "mv", "mv3"], w=[("gz", par)])
                s.add("pool", lambda e: e.tensor_tensor(out=zb[:], in0=gz[:], in1=gsgu[:], op=ALU.mult), r=[("gz", par), "gsgu"], w=["zb"])
                for h in range(8):
                    s.add("pe", lambda e, h=h: e.matmul(out=pssp[:, h * 64:(h + 1) * 64], lhsT=wspb[:, h, :], rhs=zb[:, h * 64:(h + 1) * 64],
                                                        start=True, stop=True), r=["zb", "wspb"], w=["pssp"])
                s.add("dve", lambda e: e.tensor_tensor(out=t1[:].rearrange("p (h c) -> p h c", c=64), in0=pssp[:].rearrange("p (h c) -> p h c", c=64),
                                                       in1=bsp[:].unsqueeze(2).broadcast_to([128, 8, 64]), op=ALU.add),
                      r=["pssp", "bsp"], w=["t1"])
                s.add("pool", lambda e: e.tensor_tensor(out=sg_all[:, n, :], in0=t1[:], in1=gu[:], op=ALU.mult), r=["t1", ("gu", par)], w=[("sg", n)])

            load_rows(1)
            blockA(NB, True)
            blockA(NB + 1, True)
            load_rows(0)
            blockA(0, False, 1)
            for n in range(1, NB):
                blockA(n, False, 1)
                blockA(n - 1, False, 2)
            blockA(NB - 1, False, 2)
            s.emit()
        with ExitStack() as es:
            T = lambda n, sh, dt: es.enter_context(nc.sbuf_tensor(U(n), sh, dt))
            P = lambda n, sh, dt: es.enter_context(nc.psum_tensor(U(n), sh, dt))
            s = Sched(nc, "l0b")
            mrow = _load_modrows(s, C, T, 0, {"g1": 2, "sh2": 3, "sc2": 4}, 0)
            _make_G(s, T, C, 0, "gn2", mrow["sc2"], ("mrow", 0, 0, "sc2"), "b0")
            tl = _tail_alloc(C, T, P, s, 0, NF, mrow)
            tl.update(OHf=OHf, OHb=OHb, gates=gates)
            s.add("dve", lambda e: e.tensor_copy(out=tl["sc"][:, 15:16], in_=tl["sc"][:, 15:16]), r=[("mrow", 0, 0, "sc2")], w=["G2"])
            s.add("pool", lambda e: e.tensor_copy(out=tl["sc"][:, 14:15], in_=tl["sc"][:, 14:15]), r=[("mrow", 0, 0, "sh2")], w=["SH2"])
            stg = [T("b_stg0", [128, 8, 256], F32)] * 2
            w_out = T("b_wout", [128, 8, D], BF16)
            wres = _load_w_bf16(s, T, "w_out_e", w_out, Dm["w_out_e"], D, stg, "wout")
            mkf = T("b_mkf", [128, 2, 512], F32)
            mkb = T("b_mkb", [128, 2, 512], BF16)
            s.add("sync", lambda e: e.dma_start(out=mkf[:, 0, :], in_=Dm["maskP"]), w=["mkf0"], dma="mkf0")
            s.add("sync", lambda e: e.dma_start(out=mkf[:, 1, :], in_=Dm["maskN"]), w=["mkf1"], dma="mkf1")
            s.add("dve", lambda e: e.tensor_copy(out=mkb[:], in_=mkf[:]), r=["mkf0", "mkf1"], w=["mkb"])
            snk = T("b_snk", [128, 8], F32)
            exps = T("b_exps", [128, 8], F32)
            s.add("sync", lambda e: e.dma_start(out=snk[:], in_=Dm["sink"].broadcast_to([128, 8])), w=["snk"], dma="snk")
            s.add("act", lambda e: e.activation(out=exps[:], in_=snk[:], func=AF.Exp), r=["snk"], w=["exps"])
            PT = [T(f"b_PT{i}", [128, 512], BF16) for i in range(5)]
            mix2 = [T(f"b_mix{i}", [128, 512], BF16) for i in range(2)]
            mixT = T("b_mixT", [128, 8, 128], BF16)
            dd2 = [T(f"b_dd{i}", [128, 8], F32) for i in range(2)]
            xr = [T(f"b_x{i}", [128, D], F32) for i in range(2)]
            tt = T("b_tt", [128, D], F32)
            lat = [T(f"b_lat{i}", [128, D], F32) for i in range(2)]
            pS = [P(f"b_pS{i}", [128, 512], F32) for i in range(2)]
            pO = [P(f"b_pO{i}", [128, 512], F32) for i in range(2)]
            pT = P("b_pT", [128, 8, 128], BF16)
            pW = [tl["pT32"][0], tl["pT32"][1]]
            pWv = [p[:].rearrange("p a b -> p (a b)") for p in pW]

            def ATT(n):
                par = n % 2
                mix = mix2[par]
                dd = dd2[par]
                for h in range(2):
                    tiles = [("c", 0), ("c", 1)] + ([("p", n - 1)] if n > 0 else []) + [("l", n), ("n", n + 1)]
                    for i, (kind, j) in enumerate(tiles):
                        if kind == "c":
                            kt = kcT[h * 64:(h + 1) * 64, j * 128:(j + 1) * 128]
                            kres = ("kcT", j)
                        else:
                            kt = kT_all[h * 64:(h + 1) * 64, j * 128:(j + 1) * 128]
                            kres = ("kT", j)
                        masked = kind in ("p", "n")
                        ps_ = pS[i % 2]
                        s.add("pe", lambda e, kt=kt, ps_=ps_, masked=masked, n=n, h=h: e.matmul(
                            out=ps_[:], lhsT=kt, rhs=qT_all[h * 64:(h + 1) * 64, n, :], start=True, stop=not masked),
                            r=[kres, ("qT", n)], w=[("pS", i % 2)])
                        if masked:
                            mi = 0 if kind == "p" else 1
                            s.add("pe", lambda e, ps_=ps_, mi=mi: e.matmul(out=ps_[:], lhsT=identb[:], rhs=mkb[:, mi, :], start=False, stop=True),
                                  r=["mkb", "identb"], w=[("pS", i % 2)])
                        s.add("act", lambda e, ps_=ps_, i=i: e.activation(out=PT[i][:], in_=ps_[:], func=AF.Exp, scale=0.125),
                              r=[("pS", i % 2)], w=[("PT", i)])
                    nt = len(tiles)
                    for g in range(4):
                        for i, (kind, j) in enumerate(tiles):
                            if kind == "c":
                                vt = Vcaug[:, j, h, :]
                                vres = ("Vc", j)
                            else:
                                vt = Vaug[:, j, h, :]
                                vres = ("V", j)
                            s.add("pe", lambda e, g=g, i=i, vt=vt, h=h, nt=nt: e.matmul(
                                out=pO[h][:, g * 65:(g + 1) * 65], lhsT=PT[i][:, g * 128:(g + 1) * 128], rhs=vt,
                                start=(i == 0), stop=(i == nt - 1)), r=[("PT", i), vres], w=[("pO", h)])
                    pov = pO[h][:, 0:260].rearrange("p (g d) -> p g d", d=65)
                    s.add("dve", lambda e, pov=pov, h=h: e.tensor_tensor(out=dd[:, h * 4:(h + 1) * 4], in0=pov[:, :, 64], in1=exps[:, h * 4:(h + 1) * 4], op=ALU.add),
                          r=[("pO", h), "exps"], w=[("dd", par, h)])
                    s.add("dve", lambda e, h=h: e.reciprocal(out=dd[:, h * 4:(h + 1) * 4], in_=dd[:, h * 4:(h + 1) * 4]), r=[("dd", par, h)], w=[("dd", par, h)])
                    s.add("dve", lambda e, pov=pov, h=h: e.tensor_tensor(
                        out=mix[:, h * 256:(h + 1) * 256].rearrange("p (g d) -> p g d", d=64), in0=pov[:, :, 0:64],
                        in1=dd[:, h * 4:(h + 1) * 4].unsqueeze(2).broadcast_to([128, 4, 64]), op=ALU.mult),
                        r=[("pO", h), ("dd", par, h)], w=[("mix", par, h)])

            def OUT(n):
                par = n % 2
                mix = mix2[par]
                dd = dd2[par]
                s.add("sync", lambda e, n=n, par=par: e.dma_start(out=xr[par][:], in_=Dm["xs"][n * 128:(n + 1) * 128, :]), w=[("x", par)], dma=("x", par))
                for k in range(8):
                    if k < 4:
                        src = mix[:, k * 128:(k + 1) * 128]
                        rr = [("mix", par, 0), ("mix", par, 1)]
                    else:
                        src = sg_all[:, n, (k - 4) * 128:(k - 3) * 128]
                        rr = [("sg", n)]
                    s.add("pe", lambda e, k=k, src=src: e.transpose(out=pT[:, k, :], in_=src, identity=identb[:]), r=rr + ["identb"], w=["pT"])
                s.add("act", lambda e: e.activation(out=mixT[:], in_=pT[:], func=AF.Copy), r=["pT"], w=["mixT"])
                for jn in range(2):
                    for k in range(8):
                        s.add("pe", lambda e, jn=jn, k=k: e.matmul(out=pWv[jn], lhsT=mixT[:, k, :], rhs=w_out[:, k, jn * 512:(jn + 1) * 512],
                                                                   start=(k == 0), stop=(k == 7)),
                              r=["mixT"] + wres, w=[("pT32", jn)])
                for jn in range(2):
                    s.add("dve", lambda e, jn=jn: e.tensor_tensor(out=tt[:, jn * 512:(jn + 1) * 512], in0=pWv[jn], in1=mrow["g1"][:, jn * 512:(jn + 1) * 512], op=ALU.mult),
                          r=[("pT32", jn), ("mrow", 0, 0, "g1")], w=[("tt", jn)])
                lt = lat[par]
                s.add("dve", lambda e, lt=lt, par=par: e.tensor_tensor(out=lt[:], in0=tt[:], in1=xr[par][:], op=ALU.add),
                      r=[("tt", 0), ("tt", 1), ("x", par)], w=[("lat", par)])
                s.add("sync", lambda e, lt=lt, n=n: e.dma_start(out=Dm["L1"][n * 128:(n + 1) * 128, :], in_=lt[:]), r=[("lat", par)], w=[("L1", n)],
                      dma=("latst", par))

            def TAIL(n):
                _tail(s, n, lat[n % 2], ("lat", n % 2), tl, 0)

            ATT(0)
            for n in range(1, NF):
                ATT(n)
                OUT(n - 1)
                if n >= 2:
                    TAIL(n - 2)
            OUT(NF - 1)
            TAIL(NF - 2)
            TAIL(NF - 1)
            _route_all(s, tl, NF, T)
            s.add("sync", lambda e: e.nop(), r=[("L1", n) for n in range(NF)] + [("H2", n) for n in range(NF)])
            s.emit()


def phase_dispatch(C, pers, NT, NS):
    nc = C.nc
    Dm = C.d
    OHf, OHb, gates = pers["OHf"], pers["OHb"], pers["gates"]
    be_i = pers["be_i"]
    NSS = NS // 2
    with ExitStack() as es:
        T = lambda n, sh, dt: es.enter_context(nc.sbuf_tensor(U(n), sh, dt))
        P = lambda n, sh, dt: es.enter_context(nc.psum_tensor(U(n), sh, dt))
        s = Sched(nc, "disp")
        utf = T("d_utf", [128, 128], F32)
        utb = T("d_utb", [128, 128], BF16)
        oneb = T("d_oneb", [128, 128], BF16)
        OHbb = T("d_OHbb", [128, NF, 32], BF16)
        rank = T("d_rank", [128, NF, 32], F32)
        pr_ = [P(f"d_pr{i}", [128, 32], F32) for i in range(2)]
        pc = P("d_pc", [128, 32], F32)
        cnt = T("d_cnt", [128, 32], F32)
        rr = T("d_rr", [128, 32], F32)
        gt = T("d_gt", [128, 32], F32)
        pad = T("d_pad", [128, 32], F32)
        cs = [T(f"d_cs{i}", [128, 32], F32) for i in range(2)]
        pst = T("d_pst", [128, 32], F32)
        thr = T("d_thr", [128, NSS0], F32)
        cmp_ = T("d_cmp", [128, NSS0, 32], F32)
        bef = T("d_bef", [128, NSS0], F32)
        tokf = T("d_tokf", [128, NF], F32)
        tmp = T("d_tmp", [128, NF, 32], F32)
        prod = T("d_prod", [128, NF, 2, 32], F32)
        destf = T("d_destf", [128, NF, 2], F32)
        desti = T("d_desti", [128, NF, 2], I32)
        ris = T("d_ris", [128, NF, 2, 4], F32)
        fill = T("d_fill", [128, NS0 * 4], F32)
        s.add("sync", lambda e: e.dma_start(out=utf[:], in_=Dm["utri"]), w=["utf"], dma="utf")
        s.add("sync", lambda e: e.dma_start(out=thr[:], in_=Dm["thr"].broadcast_to([128, NSS0])), w=["thr"], dma="thr")
        s.add("sync", lambda e: e.dma_start(out=tokf[:], in_=Dm["tokf"]), w=["tokf"], dma="tokf")
        s.add("sync", lambda e: e.dma_start(out=fill[:], in_=Dm["rifill"]), w=["fill"], dma="fill")
        s.add("sync", lambda e: e.dma_start(out=Dm["RI"].rearrange("(p b) c -> p (b c)", p=128), in_=fill[:]), r=["fill"], w=["RIfill"], dma="RIfill")
        s.add("dve", lambda e: e.tensor_copy(out=utb[:], in_=utf[:]), r=["utf"], w=["utb"])
        s.add("dve", lambda e: e.memset(oneb[:], 1.0), w=["oneb"])
        s.add("dve", lambda e: e.tensor_copy(out=OHbb[:, 0:NT, :], in_=OHb[:, 0:NT, :]), w=["OHbb"])
        for n in range(NT):
            p = pr_[n % 2]
            s.add("pe", lambda e, n=n, p=p: e.matmul(out=p[:], lhsT=utb[:], rhs=OHbb[:, n, :], start=True, stop=(n == 0)), r=["utb", "OHbb"], w=[("pr", n % 2)])
            for m in range(n):
                s.add("pe", lambda e, m=m, n=n, p=p: e.matmul(out=p[:], lhsT=oneb[:], rhs=OHbb[:, m, :], start=False, stop=(m == n - 1)),
                      r=["oneb", "OHbb"], w=[("pr", n % 2)])
            s.add("dve", lambda e, n=n, p=p: e.tensor_copy(out=rank[:, n, :], in_=p[:]), r=[("pr", n % 2)], w=[("rank", n)])
        for n in range(NT):
            s.add("pe", lambda e, n=n: e.matmul(out=pc[:], lhsT=oneb[:], rhs=OHbb[:, n, :], start=(n == 0), stop=(n == NT - 1)), r=["oneb", "OHbb"], w=["pc"])
        s.add("dve", lambda e: e.tensor_copy(out=cnt[:], in_=pc[:]), r=["pc"], w=["cnt"])
        cmp2 = T("d_cmp2", [128, 32, 17], F32)
        s.add("dve", lambda e: e.tensor_tensor(out=cmp2[:], in0=cnt[:].unsqueeze(2).broadcast_to([128, 32, 17]),
                                               in1=thr[:, 0:17].unsqueeze(1).broadcast_to([128, 32, 17]), op=ALU.is_gt), r=["cnt", "thr"], w=["cmp2"])
        s.add("dve", lambda e: e.reduce_sum(out=pad[:], in_=cmp2[:], axis=AX.X), r=["cmp2"], w=["pad"])
        s.add("dve", lambda e: e.tensor_scalar(out=pad[:], in0=pad[:], scalar1=256.0, scalar2=None, op0=ALU.mult), r=["pad"], w=["pad"])
        s.add("dve", lambda e: e.tensor_copy(out=cs[0][:], in_=pad[:]), r=["pad"], w=[("cs", 0)])
        cur = 0
        for sh in (1, 2, 4, 8, 16):
            nx = 1 - cur
            s.add("dve", lambda e, cur=cur, nx=nx, sh=sh: e.tensor_copy(out=cs[nx][:, 0:sh], in_=cs[cur][:, 0:sh]), r=[("cs", cur)], w=[("csa", nx)])
            s.add("dve", lambda e, cur=cur, nx=nx, sh=sh: e.tensor_tensor(out=cs[nx][:, sh:32], in0=cs[cur][:, sh:32], in1=cs[cur][:, 0:32 - sh], op=ALU.add),
                  r=[("cs", cur), ("csa", nx)], w=[("cs", nx)])
            cur = nx
        pend = cs[cur]
        pres = ("cs", cur)
        s.add("dve", lambda e: e.tensor_tensor(out=pst[:], in0=pend[:], in1=pad[:], op=ALU.subtract), r=[pres, "pad"], w=["pst"])
        s.add("dve", lambda e: e.tensor_tensor(out=cmp_[:, 0:NSS, :], in0=pend[:].unsqueeze(1).broadcast_to([128, NSS, 32]),
                                               in1=thr[:, 0:NSS].unsqueeze(2).broadcast_to([128, NSS, 32]), op=ALU.is_le), r=[pres, "thr"], w=["cmp"])
        s.add("dve", lambda e: e.reduce_sum(out=bef[:, 0:NSS], in_=cmp_[:, 0:NSS, :], axis=AX.X), r=["cmp"], w=["bef"])
        s.add("dve", lambda e: e.tensor_scalar(out=bef[:, 0:NSS], in0=bef[:, 0:NSS], scalar1=31.0, scalar2=None, op0=ALU.min), r=["bef"], w=["bef"])
        s.add("dve", lambda e: e.tensor_scalar(out=be_i[:, 1, 0:NSS], in0=thr[:, 0:NSS], scalar1=pend[:, 31:32], scalar2=OOB, op0=ALU.is_ge, op1=ALU.mult), r=[pres, "thr"], w=["be_i2"])
        s.add("dve", lambda e: e.scalar_tensor_tensor(out=be_i[:, 0, 0:NSS], in0=bef[:, 0:NSS], scalar=128.0, in1=be_i[:, 1, 0:NSS], op0=ALU.mult, op1=ALU.add), r=["bef", "be_i2"], w=["be_i"])
        rb = es.enter_context(nc.gpsimd.register(U("rb_d")))
        s.add("pool", lambda e: e.reg_mov(rb, NS0 * 128 - 1))
        s.add("dve", lambda e: e.tensor_tensor(out=tmp[:, 0:NT, :], in0=rank[:, 0:NT, :], in1=pst[:].unsqueeze(1).broadcast_to([128, NT, 32]), op=ALU.add),
              r=[("rank", n) for n in range(NT)] + ["pst"], w=["tmp"])
        s.add("dve", lambda e: e.tensor_tensor(out=prod[:, 0:NT], in0=OHf[:, 0:NT], in1=tmp[:, 0:NT, :].unsqueeze(2).broadcast_to([128, NT, 2, 32]), op=ALU.mult),
              r=["tmp"], w=["prod"])
        s.add("dve", lambda e: e.reduce_sum(out=destf[:, 0:NT, :], in_=prod[:, 0:NT], axis=AX.X), r=["prod"], w=["destf"])
        s.add("dve", lambda e: e.tensor_copy(out=desti[:, 0:NT, :], in_=destf[:, 0:NT, :]), r=["destf"], w=["desti"])
        s.add("dve", lambda e: e.memset(ris[:], 0.0), w=["ris"])
        for k in range(2):
            s.add("dve", lambda e, k=k: e.tensor_copy(out=ris[:, 0:NT, k, 0], in_=tokf[:, 0:NT]), r=["tokf", "ris"], w=[("ris0", k)])
            s.add("dve", lambda e, k=k: e.tensor_copy(out=ris[:, 0:NT, k, 1], in_=gates[:, 0:NT, k]), r=["ris"], w=[("ris1", k)])
            s.add("dve", lambda e, k=k: e.tensor_scalar(out=ris[:, 0:NT, k, 2], in0=tokf[:, 0:NT], scalar1=2.0, scalar2=float(k), op0=ALU.mult, op1=ALU.add),
                  r=["tokf", "ris"], w=[("ris2", k)])
        rres = [("ris0", 0), ("ris0", 1), ("ris1", 0), ("ris1", 1), ("ris2", 0), ("ris2", 1)]
        allsc = []
        import os as _os
        _dcut = int(_os.environ.get("DCUT", "0"))
        _thr = int(_os.environ.get("DTHR", "8"))
        for n in range(NT if _dcut == 0 else (_dcut - 1)):
            for k in range(2):
                s.add("pool", lambda e, n=n, k=k: e.indirect_dma_start(
                    out=Dm["RI"][:, :], out_offset=bass.IndirectOffsetOnAxis(ap=desti[:, n, k:k + 1], axis=0),
                    in_=ris[:, n, k, :], in_offset=None, bounds_check=rb, oob_is_err=False),
                    r=rres + ["desti", "RIfill"] + (allsc[-_thr:-_thr + 1] if len(allsc) >= _thr else []), w=[("RIsc", n, k)], dma="RIsc")
                allsc.append(("RIsc", n, k))
        s.add("sync", lambda e: e.nop(), r=allsc + ["be_i", "be_i2"])
        s.emit()


def phase_moe(C, pers, l, NT, NS):
    nc = C.nc
    Dm = C.d
    be_i = pers["be_i"]
    import os as _os
    NSx = min(NS, int(_os.environ.get("MCUT", "1000")))
    with ExitStack() as es:
        T = lambda n, sh, dt: es.enter_context(nc.sbuf_tensor(U(n), sh, dt))
        P = lambda n, sh, dt: es.enter_context(nc.psum_tensor(U(n), sh, dt))
        s = Sched(nc, f"moe{l}")
        identf = T("e_identf", [128, 128], F32)
        identb = T("e_identb", [128, 128], BF16)
        s.add("sync", lambda e: e.dma_start(out=identf[:], in_=Dm["ident"]), w=["identf"], dma="identf")
        s.add("dve", lambda e: e.tensor_copy(out=identb[:], in_=identf[:]), r=["identf"], w=["identb"])
        sg_ = [T(f"e_sgf{i}", [128, 8, 512], F32) for i in range(2)]
        su_ = [T(f"e_suf{i}", [128, 8, 512], F32) for i in range(2)]
        sd_ = [T(f"e_sdf{i}", [128, 4, D], F32) for i in range(2)]
        wg = [T(f"e_wg{i}", [128, 8, 512], BF16) for i in range(2)]
        wu = [T(f"e_wu{i}", [128, 8, 512], BF16) for i in range(2)]
        wd = [T(f"e_wd{i}", [128, 4, D], BF16) for i in range(2)]
        ri = [T(f"e_ri{i}", [128, 4], F32) for i in range(4)]
        ii = [T(f"e_ii{i}", [128, 4], I32) for i in range(4)]
        wi = [T(f"e_wi{i}", [128, 1], I32) for i in range(3)]
        xg = [T(f"e_xg{i}", [128, D], BF16) for i in range(3)]
        xT = [T(f"e_xT{i}", [128, 8, 128], BF16) for i in range(2)]
        sgt = T("e_sg", [128, 512], F32)
        hid = T("e_hid", [128, 512], BF16)
        hidT = T("e_hidT", [128, 4, 128], BF16)
        y = [T(f"e_y{i}", [128, D], F32) for i in range(2)]
        pTx = P("e_pTx", [128, 8, 128], BF16)
        pTc = P("e_pTc", [128, 4, 128], BF16)
        pG = P("e_pG", [128, 512], F32)
        pU = P("e_pU", [128, 512], F32)
        pY = [P(f"e_pY{i}", [128, 512], F32) for i in range(2)]
        for i in range(3):
            s.add("dve", lambda e, i=i: e.memset(xg[i][:], 0.0), w=[("xg", i)])
        cst = T("e_cst", [128, 1], F32)
        s.add("sync", lambda e: e.dma_start(out=cst[:], in_=Dm["widx"]), w=["cst"], dma="cst")
        rbx = es.enter_context(nc.gpsimd.register(U("rbx")))
        rby = es.enter_context(nc.gpsimd.register(U("rby")))
        rbw1 = es.enter_context(nc.gpsimd.register(U("rbw1")))
        s.add("pool", lambda e: e.reg_mov(rbx, NT * 128 - 1))
        s.add("pool", lambda e: e.reg_mov(rby, 2 * NT * 128 - 1))
        s.add("pool", lambda e: e.reg_mov(rbw1, 2 * NE * 128 - 1))

        def wgather(st, src, nm, u):
            par = u % 2
            s.add("pool", lambda e: e.indirect_dma_start(
                out=st[par][:].rearrange("p a b -> p (a b)"), out_offset=None, in_=Dm[src],
                in_offset=bass.IndirectOffsetOnAxis(ap=wi[u % 3][:, 0:1], axis=0), bounds_check=rbw1, oob_is_err=False),
                r=[("wi", u % 3)], w=[("st" + nm, par)], dma=("st" + nm, par))

        def load_w(u):
            q = u % 3
            s.add("dve", lambda e: e.tensor_scalar(out=wi[q][:], in0=cst[:], scalar1=be_i[:, 0, u:u + 1], scalar2=float(l * NE * 128), op0=ALU.add, op1=ALU.add),
                  r=["cst"], w=[("wi", q)])
            wgather(sg_, "w_gate", "wg", u)
            wgather(su_, "w_up", "wu", u)
            wgather(sd_, "w_down", "wd", u)

        def conv_gu(u):
            par = u % 2
            s.add("act", lambda e: e.activation(out=wg[par][:], in_=sg_[par][:], func=AF.Copy), r=[("stwg", par)], w=[("wg", par)])
            s.add("dve", lambda e: e.tensor_copy(out=wu[par][:], in_=su_[par][:]), r=[("stwu", par)], w=[("wu", par)])

        def conv_d(u):
            par = u % 2
            s.add("act", lambda e: e.activation(out=wd[par][:, 0:2, :], in_=sd_[par][:, 0:2, :], func=AF.Copy), r=[("stwd", par)], w=[("wd", par)])
            s.add("dve", lambda e: e.tensor_copy(out=wd[par][:, 2:4, :], in_=sd_[par][:, 2:4, :]), r=[("stwd", par)], w=[("wd2", par)])

        def load_gu(b):
            par = b % 3
            q = b % 4
            s.add("sync", lambda e: e.dma_start(out=ri[q][:], in_=Dm["RI"][b * 128:(b + 1) * 128, :]), w=[("ri", q)], dma=("ri", q))
            s.add("pool", lambda e: e.tensor_copy(out=ii[q][:], in_=ri[q][:]), r=[("ri", q)], w=[("ii", q)])
            s.add("pool", lambda e: e.indirect_dma_start(
                out=xg[par][:, :], out_offset=None, in_=Dm["H2b"][:, :],
                in_offset=bass.IndirectOffsetOnAxis(ap=ii[q][:, 0:1], axis=0), bounds_check=rbx, oob_is_err=False),
                r=[("ii", q)], w=[("xg", par)], dma=("xg", par))

        def stA(b):
            par = b % 2
            xp = b % 3
            for k in range(8):
                s.add("pe", lambda e, k=k: e.transpose(out=pTx[:, k, :], in_=xg[xp][:, k * 128:(k + 1) * 128], identity=identb[:]),
                      r=[("xg", xp), "identb"], w=["pTx"])
            s.add("act", lambda e: e.activation(out=xT[par][:], in_=pTx[:], func=AF.Copy), r=["pTx"], w=[("xT", par)])

        def stB(b):
            par = b % 2
            wp = (b // 2) % 2
            for k in range(8):
                s.add("pe", lambda e, k=k: e.matmul(out=pG[:], lhsT=xT[par][:, k, :], rhs=wg[wp][:, k, :], start=(k == 0), stop=(k == 7)),
                      r=[("xT", par), ("wg", wp)], w=["pG"])
            for k in range(8):
                s.add("pe", lambda e, k=k: e.matmul(out=pU[:], lhsT=xT[par][:, k, :], rhs=wu[wp][:, k, :], start=(k == 0), stop=(k == 7)),
                      r=[("xT", par), ("wu", wp)], w=["pU"])
            s.add("act", lambda e: e.activation(out=sgt[:], in_=pG[:], func=AF.Silu), r=["pG"], w=["sgt"])
            s.add("dve", lambda e: e.tensor_tensor(out=hid[:], in0=pU[:], in1=sgt[:], op=ALU.mult), r=["pU", "sgt"], w=["hid"])

        def stT(b):
            for k in range(4):
                s.add("pe", lambda e, k=k: e.transpose(out=pTc[:, k, :], in_=hid[:, k * 128:(k + 1) * 128], identity=identb[:]),
                      r=["hid", "identb"], w=["pTc"])
            s.add("act", lambda e: e.activation(out=hidT[:], in_=pTc[:], func=AF.Copy), r=["pTc"], w=["hidT"])

        def stD(b):
            par = b % 2
            q = b % 4
            wp = (b // 2) % 2
            for jn in range(2):
                for k in range(4):
                    s.add("pe", lambda e, jn=jn, k=k: e.matmul(out=pY[jn][:], lhsT=hidT[:, k, :], rhs=wd[wp][:, k, jn * 512:(jn + 1) * 512],
                                                               start=(k == 0), stop=(k == 3)),
                          r=["hidT", ("wd", wp), ("wd2", wp)], w=[("pY", jn)])
            s.add("act", lambda e: e.activation(out=y[par][:, 0:512], in_=pY[0][:], func=AF.Copy, scale=ri[q][:, 1:2]),
                  r=[("pY", 0), ("ri", q)], w=[("y", par, 0)])
            s.add("dve", lambda e: e.tensor_scalar(out=y[par][:, 512:1024], in0=pY[1][:], scalar1=ri[q][:, 1:2], scalar2=None, op0=ALU.mult),
                  r=[("pY", 1), ("ri", q)], w=[("y", par, 1)])
            s.add("pool", lambda e: e.indirect_dma_start(
                out=Dm["Y"][:, :], out_offset=bass.IndirectOffsetOnAxis(ap=ii[q][:, 2:3], axis=0),
                in_=y[par][:, :], in_offset=None, bounds_check=rby, oob_is_err=False),
                r=[("y", par, 0), ("y", par, 1), ("ii", q)], w=[("Ysc", b)], dma=("ysc", par))

        NSSx = (NSx + 1) // 2
        load_w(0)
        load_gu(0)
        if NSx > 1:
            load_gu(1)
        if NSx > 2:
            load_gu(2)
        if NSSx > 1:
            load_w(1)
        conv_gu(0)
        conv_d(0)
        stA(0)
        stB(0)
        for b in range(NSx):
            if b + 3 < NSx:
                load_gu(b + 3)
            u = b // 2
            if b % 2 == 0:
                if u + 2 < NSSx:
                    load_w(u + 2)
            if b + 1 < NSx:
                stA(b + 1)
            stT(b)
            if b % 2 == 0 and u + 1 < NSSx:
                conv_gu(u + 1)
            if b % 2 == 1 and u + 1 < NSSx:
                conv_d(u + 1)
            if b + 1 < NSx:
                stB(b + 1)
            stD(b)
        s.add("sync", lambda e: e.nop(), r=[("Ysc", b) for b in range(NSx)])
        s.emit()


def phase_combine(C, l, NT, src, dst, final):
    nc = C.nc
    Dm = C.d
    with ExitStack() as es:
        T = lambda n, sh, dt: es.enter_context(nc.sbuf_tensor(U(n), sh, dt))
        s = Sched(nc, f"cmb{l}")
        mrow = _load_modrows(s, C, T, l, {"g2": 5}, 0)
        yt = [T(f"c_y{i}", [128, 2, D], F32) for i in range(2)]
        lt = [T(f"c_l{i}", [128, D], F32) for i in range(3)]
        ot = [T(f"c_o{i}", [128, D], F32) for i in range(2)]
        junk = T("c_junk", [128, D], BF16)
        ss = T("c_ss", [128, 2 * NF], F32)
        if final:
            gf = T("c_gf", [128, D], F32)
            s.add("sync", lambda e: e.dma_start(out=gf[:], in_=Dm["gfin"].broadcast_to([128, D])), w=["gf"], dma="gf")
            s.add("dve", lambda e: e.memset(ss[:], 0.0), w=[("ssc", c) for c in range(2 * NF)])
        outs = []

        def stage1(n):
            par = n % 2
            lp = n % 3
            s.add("sync", lambda e: e.dma_start(out=yt[par][:], in_=Dm["Y"][n * 256:(n + 1) * 256, :].rearrange("(p k) d -> p k d", k=2)),
                  w=[("yt", par)], dma=("yt", par))
            s.add("sync", lambda e: e.dma_start(out=lt[lp][:], in_=Dm[src][n * 128:(n + 1) * 128, :]), w=[("lt", lp)], dma=("lt", lp))
            s.add("pool", lambda e: e.tensor_tensor(out=yt[par][:, 0, :], in0=yt[par][:, 0, :], in1=yt[par][:, 1, :], op=ALU.add),
                  r=[("yt", par)], w=[("yt", par)])
            s.add("dve", lambda e: e.tensor_tensor(out=yt[par][:, 0, :], in0=yt[par][:, 0, :], in1=mrow["g2"][:], op=ALU.mult),
                  r=[("yt", par), ("mrow", l, 0, "g2")], w=[("yt", par)])
            s.add("dve", lambda e: e.tensor_tensor(out=lt[lp][:], in0=lt[lp][:], in1=yt[par][:, 0, :], op=ALU.add),
                  r=[("yt", par), ("lt", lp)], w=[("lt", lp)])

        def stage2(n):
            par = n % 2
            lp = n % 3
            if not final:
                s.add("sync", lambda e: e.dma_start(out=Dm[dst][n * 128:(n + 1) * 128, :], in_=lt[lp][:]), r=[("lt", lp)], w=[(dst, n)],
                      dma=("cst", lp))
            else:
                _rms_rstd(s, ss, 2 * n, lt[lp][:], ("lt", lp), junk, "c")
                s.add("dve", lambda e: e.scalar_tensor_tensor(out=ot[par][:], in0=lt[lp][:], scalar=ss[:, 2 * n + 1:2 * n + 2], in1=gf[:],
                                                              op0=ALU.mult, op1=ALU.mult),
                      r=[("lt", lp), ("rsc", 2 * n), "gf"], w=[("ot", par)])
                s.add("sync", lambda e: e.dma_start(out=Dm[dst][n * 128:(n + 1) * 128, :], in_=ot[par][:]), r=[("ot", par)], w=[(dst, n)],
                      dma=("cst", par))
            outs.append((dst, n))

        stage1(0)
        for n in range(NT):
            if n + 1 < NT:
                stage1(n + 1)
            stage2(n)
        s.add("sync", lambda e: e.nop(), r=outs)
        s.emit()


def phase_l1(C, pers):
    nc = C.nc
    Dm = C.d
    OHf, OHb, gates = pers["OHf"], pers["OHb"], pers["gates"]
    NW = 9
    with ExitStack() as es:
        T = lambda n, sh, dt: es.enter_context(nc.sbuf_tensor(U(n), sh, dt))
        P = lambda n, sh, dt: es.enter_context(nc.psum_tensor(U(n), sh, dt))
        s = Sched(nc, "l1")
        tt = T("f_tt", [128, D], F32)
        s.gn_tile = tt
        mrow = _load_modrows(s, C, T, 1, {"sh1": 0, "sc1": 1, "g1": 2, "sh2": 3, "sc2": 4}, 0)
        _make_G(s, T, C, 1, "gn1", mrow["sc1"], ("mrow", 1, 0, "sc1"), "c0")
        _make_G(s, T, C, 1, "gn2", mrow["sc2"], ("mrow", 1, 0, "sc2"), "c1")
        tl = _tail_alloc(C, T, P, s, 1, NO, mrow)
        tl.update(OHf=OHf, OHb=OHb, gates=gates)
        s.add("dve", lambda e: e.tensor_copy(out=tl["sc"][:, 15:16], in_=tl["sc"][:, 15:16]), r=[("mrow", 1, 0, "sc2")], w=["G2"])
        s.add("pool", lambda e: e.tensor_copy(out=tl["sc"][:, 14:15], in_=tl["sc"][:, 14:15]), r=[("mrow", 1, 0, "sh2")], w=["SH2"])
        stg = [T(f"f_stg{i}", [128, 8, 256], F32) for i in range(1)] * 2
        w_in = T("f_win", [128, 8, 3 * D], BF16)
        wres = _load_w_bf16(s, T, "w_in_o", w_in, Dm["w_in_o"], 3 * D, stg, "win")
        w_out = T("f_wout", [128, 8, D], BF16)
        wores = _load_w_bf16(s, T, "w_out_o", w_out, Dm["w_out_o"], D, stg, "wout")
        identb = T("f_identb", [128, 128], BF16)
        s.add("dve", lambda e: e.tensor_copy(out=identb[:], in_=tl["identf"][:]), r=["identf"], w=["identb"])
        cw = T("f_cw", [128, 3, 8], F32)
        s.add("sync", lambda e: e.dma_start(out=cw[:], in_=Dm["conv_c"]), w=["cw"], dma="cw")
        xr = [T(f"f_x{i}", [128, D], F32) for i in range(2)]
        junk = tl["junk"]
        ss = T("f_ss", [128, 2 * NF], F32)
        s.add("dve", lambda e: e.memset(ss[:], 0.0), w=[("ssa", c) for c in range(2 * NF)])
        hf = T("f_hf", [128, D], F32)
        hb = T("f_hb", [128, D], BF16)
        hT = [T(f"f_hT{i}", [128, 8, 512], BF16) for i in range(1)]
        Yh = [T(f"f_Yh{i}", [128, 8, 514], F32) for i in range(2)]
        bgb = [T(f"f_bg{i}", [128, 8, 512], BF16) for i in range(2)]
        cgs = T("f_cgs", [128, 512], F32)
        ca = T("f_ca", [128, 512], F32)
        gT = hT[0]
        lat = xr
        pT = P("f_pT", [128, 8, 128], BF16)
        pRot = [P(f"f_pRot{i}", [128, 512], F32) for i in range(4)]
        rot = [0]
        pW = [tl["pT32"][0], tl["pT32"][1]]
        pWv = [p[:].rearrange("p a b -> p (a b)") for p in pW]
        for i in range(2):
            s.add("dve", lambda e, i=i: e.memset(Yh[i][:], 0.0), w=[("Yh", i, c) for c in range(8)] + [("Yhl", i), ("Yhr", i)])

        def window_front(w):
            nt = 512 if w < 8 else 128
            hp = 0
            bp = w % 2
            for q in range(nt // 128):
                n = w * 4 + q
                par = n % 2
                s.add("sync", lambda e, n=n, par=par: e.dma_start(out=xr[par][:], in_=Dm["L2"][n * 128:(n + 1) * 128, :]), r=[("L2", n)], w=[("x", par)], dma=("x", par))
                _rms_rstd(s, ss, 2 * n, xr[par][:], ("x", par), junk, "a")
                s.add("dve", lambda e, n=n, par=par: e.scalar_tensor_tensor(out=hf[:], in0=xr[par][:], scalar=ss[:, 2 * n + 1:2 * n + 2], in1=mrow["sc1"][:],
                                                                            op0=ALU.mult, op1=ALU.mult),
                      r=[("x", par), ("rsa", 2 * n), ("mrow", 1, 0, "sc1")], w=["hf"])
                s.add("pool", lambda e: e.tensor_tensor(out=hb[:], in0=hf[:], in1=mrow["sh1"][:], op=ALU.add), r=["hf", ("mrow", 1, 0, "sh1")], w=["hb"])
                for k in range(8):
                    s.add("pe", lambda e, k=k: e.transpose(out=pT[:, k, :], in_=hb[:, k * 128:(k + 1) * 128], identity=identb[:]), r=["hb", "identb"], w=["pT"])
                s.add("act", lambda e, q=q, hp=hp: e.activation(out=hT[hp][:, :, q * 128:(q + 1) * 128], in_=pT[:], func=AF.Copy), r=["pT"], w=[("hT", hp, q), "HG"])
            hres = [("hT", hp, q) for q in range(nt // 128)] + ["HG"]
            yi = w % 2
            for c in range(8):
                def nxt():
                    i = rot[0] % 4
                    rot[0] += 1
                    return pRot[i], ("pRot", i)
                pC, pCn = nxt()
                pX, pXn = nxt()
                grp = [(pC, pCn, D + c * 128), (pX, pXn, 2 * D + c * 128)]
                if w < 8:
                    pB, pBn = nxt()
                    grp.append((pB, pBn, c * 128))
                for (pt, pn, c0) in grp:
                    for k in range(8):
                        s.add("pe", lambda e, pt=pt, c0=c0, k=k, nt=nt, hp=hp: e.matmul(out=pt[:, 0:nt], lhsT=w_in[:, k, c0:c0 + 128], rhs=hT[hp][:, k, 0:nt],
                                                                                         start=(k == 0), stop=(k == 7)),
                              r=hres + wres, w=[pn])
                s.add("act", lambda e, nt=nt, pC=pC: e.activation(out=cgs[:, 0:nt], in_=pC[:, 0:nt], func=AF.Copy), r=[pCn], w=["cgs"])
                s.add("dve", lambda e, c=c, nt=nt, yi=yi, pX=pX: e.tensor_tensor(out=Yh[yi][:, c, 1:1 + nt], in0=pX[:, 0:nt], in1=cgs[:, 0:nt], op=ALU.mult),
                      r=[pXn, "cgs"], w=[("Yh", yi, c)])
                if w < 8:
                    s.add("act", lambda e, c=c, bp=bp, pB=pB: e.activation(out=bgb[bp][:, c, :], in_=pB[:], func=AF.Copy), r=[pBn], w=[("bg", bp, c)])
            yall = [("Yh", yi, c) for c in range(8)]
            if w > 0:
                yp = (w - 1) % 2
                s.add("pool", lambda e, yi=yi, yp=yp: e.tensor_copy(out=Yh[yp][:, :, 513:514], in_=Yh[yi][:, :, 1:2]), r=yall, w=[("Yhr", yp)])
                s.add("pool", lambda e, yi=yi, yp=yp: e.tensor_copy(out=Yh[yi][:, :, 0:1], in_=Yh[yp][:, :, 512:513]), r=[("Yh", yp, c) for c in range(8)], w=[("Yhl", yi)])
            else:
                s.add("pool", lambda e, yi=yi: e.memset(Yh[yi][:, :, 0:1], 0.0), w=[("Yhl", yi)])

        def window_back(w):
            yi = w % 2
            hp = w % 2
            yres = [("Yhl", yi), ("Yhr", yi)]
            for c in range(8):
                s.add("dve", lambda e, c=c: e.tensor_scalar(out=ca[:], in0=Yh[yi][:, c, 1:513], scalar1=cw[:, 1, c:c + 1], scalar2=None, op0=ALU.mult),
                      r=yres + [("Yh", yi, c), "cw"], w=["ca"])
                s.add("dve", lambda e, c=c: e.scalar_tensor_tensor(out=ca[:], in0=Yh[yi][:, c, 0:512], scalar=cw[:, 0, c:c + 1], in1=ca[:], op0=ALU.mult, op1=ALU.add),
                      r=yres + [("Yh", yi, c), "cw", "ca"], w=["ca"])
                s.add("dve", lambda e, c=c: e.scalar_tensor_tensor(out=ca[:], in0=Yh[yi][:, c, 2:514], scalar=cw[:, 2, c:c + 1], in1=ca[:], op0=ALU.mult, op1=ALU.add),
                      r=yres + [("Yh", yi, c), "cw", "ca"], w=["ca"])
                s.add("pool", lambda e, c=c: e.tensor_tensor(out=gT[:, c, :], in0=ca[:], in1=bgb[hp][:, c, :], op=ALU.mult), r=["ca", ("bg", hp, c)], w=[("gT", c), "HG"])
            for q in range(4):
                n = w * 4 + q
                par = n % 2
                s.add("sync", lambda e, n=n, par=par: e.dma_start(out=xr[par][:], in_=Dm["L2"][n * 128:(n + 1) * 128, :]), r=[("L2", n)], w=[("x", par)], dma=("x", par))
                for jn in range(2):
                    for k in range(8):
                        s.add("pe", lambda e, jn=jn, k=k, q=q: e.matmul(out=pWv[jn], lhsT=gT[:, k, q * 128:(q + 1) * 128], rhs=w_out[:, k, jn * 512:(jn + 1) * 512],
                                                                        start=(k == 0), stop=(k == 7)),
                              r=[("gT", c) for c in range(8)] + ["HG"] + wores, w=[("pT32", jn)])
                for jn in range(2):
                    s.add("dve", lambda e, jn=jn: e.tensor_tensor(out=tt[:, jn * 512:(jn + 1) * 512], in0=pWv[jn], in1=mrow["g1"][:, jn * 512:(jn + 1) * 512], op=ALU.mult),
                          r=[("pT32", jn), ("mrow", 1, 0, "g1")], w=[("tt", jn)])
                lt = lat[par]
                s.add("dve", lambda e, lt=lt, par=par: e.tensor_tensor(out=lt[:], in0=tt[:], in1=xr[par][:], op=ALU.add),
                      r=[("tt", 0), ("tt", 1), ("x", par)], w=[("x", par)])
                s.add("sync", lambda e, lt=lt, n=n: e.dma_start(out=Dm["L3"][n * 128:(n + 1) * 128, :], in_=lt[:]), r=[("x", par)], w=[("L3", n)],
                      dma=("latst", par))
                if q >= 1:
                    _tail(s, n - 1, xr[(n - 1) % 2], ("x", (n - 1) % 2), tl, 1)
                if q == 3:
                    _tail(s, n, lt, ("x", par), tl, 1)

        import os as _os
        _l1w = int(_os.environ.get("L1W", "8"))
        _l1b = int(_os.environ.get("L1B", "1"))
        window_front(0)
        for w in range(_l1w):
            window_front(w + 1)
            if _l1b:
                window_back(w)
        if _l1w == 8 and _l1b:
            _route_all(s, tl, NO, T)
        s.add("sync", lambda e: e.nop(), r=[("L3", n) for n in range(NO if (_l1w == 8 and _l1b) else 0)] + [("H2", n) for n in range(NO if (_l1w == 8 and _l1b) else 0)])
        s.emit()


def build(stages=("mod", "l0", "d0", "m0", "c0", "l1", "d1", "m1", "c1"), ext_in_extra=(), ext_out_extra=()):
    nc = bass.Bass("TRN2", target_bir_lowering=False)
    ext_in = set(INPUT_SHAPES) | set(ext_in_extra)
    ext_out = {"out"} | set(ext_out_extra)
    C = Ctx(nc, ext_in, ext_out)
    for k, sh in INPUT_SHAPES.items():
        C.dram(k, sh, F32)
    C.dram("out", (NO * 128, D), F32)
    C.dram("modrow", (2, 2, 6 * D), F32)
    C.dram("L1", (NF * 128, D), F32)
    C.dram("L2", (NF * 128, D), F32)
    C.dram("L3", (NO * 128, D), F32)
    C.dram("H2", (NF * 128, D), F32)
    C.dram("H2b", (NF * 128, D), BF16)
    C.dram("RI", (NS0 * 128, 4), F32)
    C.dram("Y", (2 * NF * 128, D), F32)
    C.dram("DBG", (128, 2 * NF + NS0 + 64), F32)
    with ExitStack() as es:
        _SEMPOOL.clear()
        _SEMPOOL["es"] = es
        pers = {}
        pers["OHf"] = es.enter_context(nc.sbuf_tensor("p_OHf", [128, NF, 2, 32], F32))
        pers["OHb"] = es.enter_context(nc.sbuf_tensor("p_OHb", [128, NF, 32], F32))
        pers["gates"] = es.enter_context(nc.sbuf_tensor("p_gates", [128, NF, 2], F32))
        pers["be_i"] = es.enter_context(nc.sbuf_tensor("p_be", [128, 2, NSS0], F32))
        for st in stages:
            if st == "mod":
                phase_mod(C)
            elif st == "l0":
                phase_l0(C, pers)
            elif st == "d0":
                phase_dispatch(C, pers, NF, NS0)
            elif st == "m0":
                phase_moe(C, pers, 0, NF, NS0)
            elif st == "c0":
                phase_combine(C, 0, NF, "L1", "L2", False)
            elif st == "l1":
                phase_l1(C, pers)
            elif st == "d1":
                phase_dispatch(C, pers, NO, NS1)
            elif st == "m1":
                phase_moe(C, pers, 1, NO, NS1)
            elif st == "c1":
                phase_combine(C, 1, NO, "L3", "out", True)
    return nc


def _consts():
    c = {}
    c["ident"] = np.eye(128, dtype=np.float32)
    c["widx"] = np.arange(128, dtype=np.float32)[:, None]
    c["utri"] = np.triu(np.ones((128, 128), np.float32), 1)
    c["tokf"] = (np.arange(NF)[None, :] * 128 + np.arange(128)[:, None]).astype(np.float32)
    c["thr"] = (np.arange(NSS0, dtype=np.float32) * 256.0)[None, :]
    fill = np.zeros((128, NS0, 4), np.float32)
    fill[:, :, 0] = OOB
    fill[:, :, 2] = OOB
    c["rifill"] = fill.reshape(128, NS0 * 4)
    j = np.arange(128)[:, None]
    i = np.arange(128)[None, :]
    mp = np.where(j >= i, 0.0, -30000.0).astype(np.float32)
    mn = np.where(j <= i, 0.0, -30000.0).astype(np.float32)
    c["maskP"] = np.tile(mp, (1, 4))
    c["maskN"] = np.tile(mn, (1, 4))
    return c


def _rope_tables(pos):
    quarter = 16
    inv = (10000.0 ** (-np.arange(quarter, dtype=np.float32) / quarter)).astype(np.float32)
    row = (pos // 64).astype(np.float32)
    col = (pos % 64).astype(np.float32)
    ar = row[:, None] * inv[None, :]
    ac = col[:, None] * inv[None, :]
    cr, sr, cc_, sc_ = np.cos(ar), np.sin(ar), np.cos(ac), np.sin(ac)
    Ct = np.concatenate([cr, cr, cc_, cc_], axis=1).astype(np.float32)
    St = np.concatenate([-sr, sr, -sc_, sc_], axis=1).astype(np.float32)
    return Ct, St


def _relayout_experts(inp):
    out = {}
    out["w_gate"] = np.ascontiguousarray(np.asarray(inp["w_gate"], np.float32).reshape(2, NE, 8, 128, 512).transpose(0, 1, 3, 2, 4)).reshape(2 * NE * 128, 4096)
    out["w_up"] = np.ascontiguousarray(np.asarray(inp["w_up"], np.float32).reshape(2, NE, 8, 128, 512).transpose(0, 1, 3, 2, 4)).reshape(2 * NE * 128, 4096)
    out["w_down"] = np.ascontiguousarray(np.asarray(inp["w_down"], np.float32).reshape(2, NE, 4, 128, 1024).transpose(0, 1, 3, 2, 4)).reshape(2 * NE * 128, 4096)
    return out


def _core_inputs(inp, core, consts, consts_w):
    b, hf = core // 2, core % 2
    m = dict(consts)
    x = inp["x"][b]
    if hf == 0:
        pos = np.arange(0, NB * 128)
        xs = x[0:NB * 128]
    else:
        pos = np.arange(8191, 8191 - NB * 128, -1)
        xs = x[::-1][0:NB * 128]
    m["xs"] = np.ascontiguousarray(xs)
    m["ctxs"] = np.ascontiguousarray(inp["ctx"][b])
    cc = np.stack([inp["c"][b], inp["c_ctx"]], axis=-1)
    m["cc"] = np.ascontiguousarray(cc.reshape(8, 128, 2).transpose(1, 0, 2))
    m["w_ada"] = inp["w_ada"]
    m["b_ada"] = inp["b_ada"]
    m["gn1"] = inp["g_norm1"]
    m["gn2"] = inp["g_norm2"]
    m["gfin"] = inp["g_final"][None, :]
    w = inp["w_in_even"][0]
    qperm = np.concatenate([np.arange((h * 4 + g) * 64, (h * 4 + g) * 64 + 64) for g in range(4) for h in range(2)])
    m["w_in_e"] = np.ascontiguousarray(np.concatenate([w[:, qperm], w[:, 512:]], axis=1))
    m["sink"] = inp["attn_sink"][0][None, :]
    m["g_sgu"] = inp["g_sgu"][0][None, :]
    wsp = inp["w_spatial"][0]
    bsp = inp["b_spatial"][0]
    cw = inp["conv_w"][0]
    if hf == 1:
        wsp = wsp[:, ::-1, ::-1]
        bsp = bsp[:, ::-1]
        cw = cw[::-1]
    m["w_spT"] = np.ascontiguousarray(wsp.transpose(2, 0, 1))
    m["b_spc"] = np.ascontiguousarray(bsp.T)
    m["w_out_e"] = inp["w_out_even"][0]
    m["w_in_o"] = inp["w_in_odd"][0]
    m["conv_c"] = np.ascontiguousarray(cw.reshape(3, 8, 128).transpose(2, 0, 1))
    m["w_out_o"] = inp["w_out_odd"][0]
    m["wr"] = np.ascontiguousarray(np.concatenate([inp["w_router_group"], inp["w_router_expert"]], axis=-1))
    m["br"] = np.ascontiguousarray(np.concatenate([inp["b_router_group"], inp["b_router_expert"]], axis=-1))
    m["w_gate"] = consts_w["w_gate"]
    m["w_up"] = consts_w["w_up"]
    m["w_down"] = consts_w["w_down"]
    Ct, St = _rope_tables(pos)
    m["ropeC"] = Ct
    m["ropeS"] = St
    return {k: np.ascontiguousarray(np.asarray(v, dtype=np.float32)) for k, v in m.items()}


def kernel(**inputs):
    inp = {k: np.asarray(v) for k, v in inputs.items()}
    consts = _consts()
    nc = build()
    cw = _relayout_experts(inp)
    in_maps = [_core_inputs(inp, c, consts, cw) for c in range(8)]
    res = run_bass_kernel_spmd(nc, in_maps, core_ids=list(range(8)))
    out = np.empty((4, 8192, D), np.float32)
    for c in range(8):
        b, hf = c // 2, c % 2
        o = res.results[c]["out"]
        if hf == 0:
            out[b, 0:4096] = o
        else:
            out[b, 4096:8192] = o[::-1]
    return out
```
